# Optimizing a Trainium2 kernel written in Bass

```python
import math
import jax, jax.numpy as jnp
from jax import lax
import numpy as np

D_MODEL = 2048
BATCH = 2
SEQ = 8192
DEPTH = 1

D_MIX = D_MODEL
D_SSM = D_MIX // 2
SSM_GROUP = 16
N_SSM_GROUPS = D_SSM // SSM_GROUP
SSM_STATE = 64
D_ATTN = D_MIX - D_SSM
HEAD_DIM = 128
N_HEADS = D_ATTN // HEAD_DIM
N_KV_HEADS = 2
KV_REP = N_HEADS // N_KV_HEADS
N_IDX_HEADS = 16
IDX_DIM = 64
TOPK_MAX = 256
Q_BLOCK = 128
ROPE_FRAC = 4
ROPE_THETA = 500000.0
D_FF = 4 * D_MODEL
EPS = 1e-6
N_MOD = 6
IN_SPLITS = (D_SSM, N_HEADS * HEAD_DIM, N_KV_HEADS * HEAD_DIM, N_KV_HEADS * HEAD_DIM,
             N_IDX_HEADS * IDX_DIM, IDX_DIM, N_IDX_HEADS)
D_IN = D_SSM + N_HEADS * HEAD_DIM + 2 * N_KV_HEADS * HEAD_DIM + N_IDX_HEADS * IDX_DIM + IDX_DIM + N_IDX_HEADS

kernel_name = "hymba_s5_dsa_hybrid_layer"


def rms_norm(x, g):
    xf = x.astype(jnp.float32)
    y = xf * lax.rsqrt(jnp.mean(xf * xf, axis=-1, keepdims=True) + EPS)
    return (y * g.astype(jnp.float32)).astype(x.dtype)


def partial_rope(x, pos):
    d = x.shape[-1]
    r = d // ROPE_FRAC
    half = r // 2
    inv = ROPE_THETA ** (-jnp.arange(half, dtype=jnp.float32) / half)
    ang = pos.astype(jnp.float32)[..., None] * inv
    cos = jnp.cos(ang)[:, :, None, :]
    sin = jnp.sin(ang)[:, :, None, :]
    x1 = x[..., :half].astype(jnp.float32)
    x2 = x[..., half:r].astype(jnp.float32)
    rot = jnp.concatenate([x1 * cos - x2 * sin, x1 * sin + x2 * cos], axis=-1).astype(x.dtype)
    return jnp.concatenate([rot, x[..., r:]], axis=-1)


def s5_mixer(u, lam_re, lam_im, log_dt, b_re, b_im, c_re, c_im, d_skip, w_glu, b_glu):
    bsz, seq, _ = u.shape
    f32 = jnp.float32
    uf = u.astype(f32).reshape(bsz, seq, N_SSM_GROUPS, SSM_GROUP)
    lr = jnp.minimum(lam_re.astype(f32), -1e-4)
    li = lam_im.astype(f32)
    dt = jnp.exp(log_dt.astype(f32))[:, None]
    mag = jnp.exp(lr * dt)
    abar_r = mag * jnp.cos(li * dt)
    abar_i = mag * jnp.sin(li * dt)
    den = lr * lr + li * li
    fr = ((abar_r - 1.0) * lr + abar_i * li) / den
    fi = (abar_i * lr - (abar_r - 1.0) * li) / den
    bu_r = jnp.einsum('blgh,gph->blgp', uf, b_re.astype(f32))
    bu_i = jnp.einsum('blgh,gph->blgp', uf, b_im.astype(f32))
    xr0 = fr * bu_r - fi * bu_i
    xi0 = fr * bu_i + fi * bu_r
    a_r = jnp.broadcast_to(abar_r, xr0.shape)
    a_i = jnp.broadcast_to(abar_i, xr0.shape)

    def combine(e1, e2):
        a1r, a1i, b1r, b1i = e1
        a2r, a2i, b2r, b2i = e2
        return (a2r * a1r - a2i * a1i,
                a2r * a1i + a2i * a1r,
                a2r * b1r - a2i * b1i + b2r,
                a2r * b1i + a2i * b1r + b2i)

    _, _, s_r, s_i = lax.associative_scan(combine, (a_r, a_i, xr0, xi0), axis=1)
    y = (jnp.einsum('blgp,ghp->blgh', s_r, c_re.astype(f32))
         - jnp.einsum('blgp,ghp->blgh', s_i, c_im.astype(f32)))
    y = y.reshape(bsz, seq, D_SSM) + d_skip.astype(f32) * u.astype(f32)
    ya = jax.nn.gelu(y)
    out = ya * jax.nn.sigmoid(ya @ w_glu.astype(f32) + b_glu.astype(f32))
    return out.astype(u.dtype)


def dsa_attention(q, k, v, q_idx, k_idx, w_idx, topk):
    f32 = jnp.float32
    bsz, seq = q.shape[:2]
    n_blocks = seq // Q_BLOCK
    key_pos = jnp.arange(seq)
    bidx = jnp.arange(bsz)[:, None, None]
    k_idx_f = k_idx.astype(f32)
    idx_scale = IDX_DIM ** -0.5
    head_w_scale = N_IDX_HEADS ** -0.5
    attn_scale = HEAD_DIM ** -0.5

    def block(i):
        start = i * Q_BLOCK
        qb = lax.dynamic_slice_in_dim(q, start, Q_BLOCK, axis=1)
        qib = lax.dynamic_slice_in_dim(q_idx, start, Q_BLOCK, axis=1).astype(f32)
        wb = lax.dynamic_slice_in_dim(w_idx, start, Q_BLOCK, axis=1).astype(f32) * head_w_scale
        logits = jnp.einsum('bqhd,bkd->bqhk', qib, k_idx_f) * idx_scale
        isc = jnp.einsum('bqhk,bqh->bqk', jax.nn.relu(logits), wb)
        qpos = start + jnp.arange(Q_BLOCK)
        causal = key_pos[None, :] <= qpos[:, None]
        isc = jnp.where(causal[None], isc, -jnp.inf)
        top_val, top_idx = lax.top_k(isc, topk)
        valid = jnp.isfinite(top_val)
        ks = k[bidx, top_idx].astype(f32)
        vs = v[bidx, top_idx].astype(f32)
        qg = qb.reshape(bsz, Q_BLOCK, N_KV_HEADS, KV_REP, HEAD_DIM).astype(f32)
        sc = jnp.einsum('bqgrd,bqkgd->bqgrk', qg, ks) * attn_scale
        sc = jnp.where(valid[:, :, None, None, :], sc, -jnp.inf)
        p = jax.nn.softmax(sc, axis=-1)
        o = jnp.einsum('bqgrk,bqkgd->bqgrd', p, vs)
        return o.reshape(bsz, Q_BLOCK, N_HEADS * HEAD_DIM).astype(q.dtype)

    out = lax.map(block, jnp.arange(n_blocks))
    return out.transpose(1, 0, 2, 3).reshape(bsz, seq, N_HEADS * HEAD_DIM)


def setup_inputs(seed: int = 0) -> dict:
    key = jax.random.key(seed)
    ks = jax.random.split(key, 32)
    f32 = jnp.float32
    nrm = lambda k, shape, s: jax.random.normal(k, shape, f32) * s
    G, P, H = N_SSM_GROUPS, SSM_STATE, SSM_GROUP
    x = jax.random.normal(ks[0], (BATCH, SEQ, D_MODEL), f32)
    c = jax.random.normal(ks[1], (BATCH, D_MODEL), f32)
    offset = jax.random.randint(ks[2], (BATCH, 1), 0, 1024)
    positions = (offset + jnp.arange(SEQ, dtype=jnp.int32)[None, :]).astype(jnp.int32)
    lam_im = jnp.broadcast_to(math.pi * jnp.arange(P, dtype=f32), (DEPTH, G, P))
    return {
        "x": x,
        "c": c,
        "positions": positions,
        "w_ada": nrm(ks[3], (DEPTH, D_MODEL, N_MOD * D_MODEL), 0.5 * D_MODEL ** -0.5),
        "b_ada": nrm(ks[4], (DEPTH, N_MOD * D_MODEL), 0.02),
        "g_norm_mix": 1.0 + nrm(ks[5], (DEPTH, D_MODEL), 0.02),
        "w_in": nrm(ks[6], (DEPTH, D_MODEL, D_IN), D_MODEL ** -0.5),
        "lam_re": -0.5 + nrm(ks[7], (DEPTH, G, P), 0.01),
        "lam_im": lam_im,
        "log_dt": jax.random.uniform(ks[8], (DEPTH, G), f32, math.log(1e-3), math.log(1e-1)),
        "b_re": nrm(ks[9], (DEPTH, G, P, H), (2 * H) ** -0.5),
        "b_im": nrm(ks[10], (DEPTH, G, P, H), (2 * H) ** -0.5),
        "c_re": nrm(ks[11], (DEPTH, G, H, P), (2 * P) ** -0.5),
        "c_im": nrm(ks[12], (DEPTH, G, H, P), (2 * P) ** -0.5),
        "d_skip": nrm(ks[13], (DEPTH, D_SSM), 1.0),
        "w_glu": nrm(ks[14], (DEPTH, D_SSM, D_SSM), D_SSM ** -0.5),
        "b_glu": nrm(ks[15], (DEPTH, D_SSM), 0.02),
        "g_q": 1.0 + nrm(ks[16], (DEPTH, HEAD_DIM), 0.02),
        "g_k": 1.0 + nrm(ks[17], (DEPTH, HEAD_DIM), 0.02),
        "g_out_ssm": 1.0 + nrm(ks[18], (DEPTH, D_SSM), 0.02),
        "g_out_attn": 1.0 + nrm(ks[19], (DEPTH, D_ATTN), 0.02),
        "w_out": nrm(ks[20], (DEPTH, D_MIX, D_MODEL), D_MIX ** -0.5),
        "g_norm_mlp": 1.0 + nrm(ks[21], (DEPTH, D_MODEL), 0.02),
        "w_mlp_in": nrm(ks[22], (DEPTH, D_MODEL, D_FF), D_MODEL ** -0.5),
        "w_mlp_out": nrm(ks[23], (DEPTH, D_FF, D_MODEL), D_FF ** -0.5),
    }


def reference(x, c, positions, w_ada, b_ada, g_norm_mix, w_in, lam_re, lam_im, log_dt,
              b_re, b_im, c_re, c_im, d_skip, w_glu, b_glu, g_q, g_k, g_out_ssm, g_out_attn,
              w_out, g_norm_mlp, w_mlp_in, w_mlp_out):
    bsz, seq, _ = x.shape
    topk = min(TOPK_MAX, seq // 4)
    split_pts = [int(s) for s in np.cumsum(IN_SPLITS)[:-1]]
    c_act = jax.nn.silu(c.astype(jnp.float32))
    for l in range(DEPTH):
        mod = c_act @ w_ada[l].astype(jnp.float32) + b_ada[l].astype(jnp.float32)
        sh_a, sc_a, gt_a, sh_m, sc_m, gt_m = [m[:, None, :] for m in jnp.split(mod, N_MOD, axis=-1)]

        h = (rms_norm(x, g_norm_mix[l]).astype(jnp.float32) * (1.0 + sc_a) + sh_a).astype(x.dtype)
        proj = h @ w_in[l]
        u_ssm, q, k, v, q_i, k_i, w_i = jnp.split(proj, split_pts, axis=-1)

        y_ssm = s5_mixer(u_ssm, lam_re[l], lam_im[l], log_dt[l], b_re[l], b_im[l],
                         c_re[l], c_im[l], d_skip[l], w_glu[l], b_glu[l])

        q = partial_rope(rms_norm(q.reshape(bsz, seq, N_HEADS, HEAD_DIM), g_q[l]), positions)
        k = partial_rope(rms_norm(k.reshape(bsz, seq, N_KV_HEADS, HEAD_DIM), g_k[l]), positions)
        v = v.reshape(bsz, seq, N_KV_HEADS, HEAD_DIM)
        q_i = partial_rope(q_i.reshape(bsz, seq, N_IDX_HEADS, IDX_DIM), positions)
        k_i = partial_rope(k_i.reshape(bsz, seq, 1, IDX_DIM), positions)[:, :, 0, :]
        y_attn = dsa_attention(q, k, v, q_i, k_i, w_i, topk)

        mix = jnp.concatenate([rms_norm(y_ssm, g_out_ssm[l]), rms_norm(y_attn, g_out_attn[l])], axis=-1)
        x = (x + gt_a * (mix @ w_out[l])).astype(x.dtype)

        h2 = (rms_norm(x, g_norm_mlp[l]).astype(jnp.float32) * (1.0 + sc_m) + sh_m).astype(x.dtype)
        ff = jnp.square(jax.nn.relu(h2 @ w_mlp_in[l])) @ w_mlp_out[l]
        x = (x + gt_m * ff).astype(x.dtype)
    return x
```

```python
import numpy as np
from contextlib import ExitStack
import concourse.bass as bass
import concourse.mybir as mybir
from concourse.bass_utils import run_bass_kernel_spmd

F32 = mybir.dt.float32
BF16 = mybir.dt.bfloat16
I32 = mybir.dt.int32
AF = mybir.ActivationFunctionType
ALU = mybir.AluOpType
AX = mybir.AxisListType

D = 2048
L = 8192
NBLK = 64
NCH = 16
KT = 16
DIN = 3664
U0, Q0, K0, V0, QI0, KI0, WI0 = 0, 1024, 2048, 2304, 2560, 3584, 3648
EPS = 1e-6
TOPK = 256
NBIS = 18
NEG = -1.0e30
TWO_PI = 6.283185307179586
PI = 3.141592653589793

C_ID = 0
C_RM = 128
C_SG = 136
C_IOTA = 137
C_INVH = 265
C_INVI = 281
C_H2 = 289
C_CM = 317
C_SW = 829
C_CM8 = 957
C_ONE = 1981
C_NHALF = 2109
CW = 2110


def make_consts():
    c = np.zeros((128, CW), np.float32)
    c[:, C_ID:C_ID + 128] = np.eye(128, dtype=np.float32)
    p = np.arange(128)
    for g in range(8):
        c[:, C_RM + g] = (p // 16 == g)
    c[:, C_SG] = np.where(p < 64, 1.0, -1.0)
    c[:, C_IOTA:C_IOTA + 128] = np.arange(128, dtype=np.float32)[None, :]
    c[:, C_INVH:C_INVH + 16] = (500000.0 ** (-np.arange(16, dtype=np.float32) / 16)).astype(np.float32)[None, :]
    c[:, C_INVI:C_INVI + 8] = (500000.0 ** (-np.arange(8, dtype=np.float32) / 8)).astype(np.float32)[None, :]
    c[:, C_H2:C_H2 + 28] = (2.0 ** -(np.arange(28, dtype=np.float64) + 1)).astype(np.float32)[None, :]
    kk = np.arange(512)[None, :]
    qq = np.arange(128)[:, None]
    c[:, C_CM:C_CM + 512] = np.where(kk <= 384 + qq, 0.0, NEG)
    sw = np.zeros((128, 128), np.float32)
    for i in range(128):
        sw[i, (i + 64) % 128] = 1.0
    c[:, C_SW:C_SW + 128] = sw
    col = np.arange(128)
    for g in range(8):
        c[:, C_CM8 + g * 128:C_CM8 + (g + 1) * 128] = (col // 16 == g)[None, :]
    c[:, C_ONE:C_ONE + 128] = 1.0
    c[:, C_NHALF] = -0.5
    return c


class Buf:
    __slots__ = ("w", "r")

    def __init__(self):
        self.w = None
        self.r = {}


class Tile:
    def __init__(self, t, nsub=0):
        self.t = t
        self.b = Buf()
        self.sub = [Buf() for _ in range(nsub)]

    def __getitem__(self, k):
        return self.t[k]


class Sch:
    NSLOT = 12

    def __init__(self, nc, es):
        self.nc = nc
        self.E = {}
        self.sem = {}
        for name, h in (("pe", nc.tensor), ("act", nc.scalar), ("dve", nc.vector),
                        ("pool", nc.gpsimd), ("sp", nc.sync)):
            sem = es.enter_context(nc.semaphore("sem_" + name))
            self.E[name] = dict(h=h, sem=sem, cnt=0, waited={})
            self.sem[name] = sem
        self.slots = []
        for i in range(self.NSLOT):
            sem = es.enter_context(nc.semaphore("dq%d" % i))
            self.slots.append(dict(sem=sem, val=0, key="dq%d" % i))
            self.sem["dq%d" % i] = sem
        self.slot_i = 0
        self.n_inst = 0

    def _deps(self, eng, R, W):
        need = {}

        def req(d, raw):
            if d is None:
                return
            k, v = d
            if k == eng:
                if eng == "pe" or not raw:
                    return
            if need.get(k, 0) < v:
                need[k] = v
        for b in R:
            req(b.w, True)
        for b in W:
            req(b.w, False)
            for k, v in b.r.items():
                req((k, v), False)
        return need

    def _wait(self, eng, need):
        e = self.E[eng]
        for k, v in need.items():
            if e["waited"].get(k, 0) >= v:
                continue
            e["h"].wait_ge(self.sem[k], v)
            e["waited"][k] = v
            self.n_inst += 1

    def op(self, eng, fn, R=(), W=()):
        R = [x.b if isinstance(x, Tile) else x for x in R]
        W = [x.b if isinstance(x, Tile) else x for x in W]
        self._wait(eng, self._deps(eng, R, W))
        e = self.E[eng]
        inst = fn(e["h"])
        e["cnt"] += 1
        inst.then_inc(e["sem"], 1)
        self.n_inst += 1
        for b in R:
            b.r[eng] = e["cnt"]
        for b in W:
            b.w = (eng, e["cnt"])
            b.r = {}

    def dma(self, q, out, in_, R=(), W=()):
        R = [x.b if isinstance(x, Tile) else x for x in R]
        W = [x.b if isinstance(x, Tile) else x for x in W]
        need = self._deps(None, R, W)
        sl = self.slots[self.slot_i]
        self.slot_i = (self.slot_i + 1) % self.NSLOT
        if sl["val"] > 0:
            if need.get(sl["key"], 0) < sl["val"]:
                need[sl["key"]] = sl["val"]
        self._wait(q, need)
        e = self.E[q]
        inst = e["h"].dma_start(out=out, in_=in_)
        sl["val"] += 16
        inst.then_inc(sl["sem"], 16)
        self.n_inst += 1
        for b in R:
            b.r[sl["key"]] = sl["val"]
        for b in W:
            b.w = (sl["key"], sl["val"])
            b.r = {}

    def barrier(self):
        for name in ("pe", "act", "dve", "pool", "sp"):
            need = {}
            for n2, e2 in self.E.items():
                if n2 != name and e2["cnt"] > 0:
                    need[n2] = e2["cnt"]
            for sl in self.slots:
                if sl["val"] > 0:
                    need[sl["key"]] = sl["val"]
            self._wait(name, need)

    def finish(self, bufs):
        need = {}
        for b in bufs:
            b = b.b if isinstance(b, Tile) else b
            if b.w is not None:
                k, v = b.w
                need[k] = max(need.get(k, 0), v)
        self._wait("sp", need)


def build(stop=99, dbg=False, full=False):
    nc = bass.Bass("TRN2", target_bir_lowering=False)
    es = ExitStack()
    S = Sch(nc, es)
    FULL = full or stop >= 99

    def din(name, shape, dt=F32):
        return Tile(nc.dram_tensor(name, list(shape), dt, kind="ExternalInput").ap())

    def dscr(name, shape, dt):
        return Tile(nc.dram_tensor(name, list(shape), dt, kind="Internal").ap())

    def sb(st, name, shape, dt, nsub=0):
        return Tile(st.enter_context(nc.sbuf_tensor(name, list(shape), dt)), nsub)

    def ps(st, name, shape, dt=F32, nsub=0):
        return Tile(st.enter_context(nc.psum_tensor(name, list(shape), dt)), nsub)

    xv = din("xv", [L, D])
    validT = din("validT", [128, NBLK])
    posT = din("posT", [128, NBLK], I32)
    vecs = din("vecs", [82, 128])
    consts = din("consts", [128, CW])
    w_ada = din("w_ada", [D, 6 * D])
    b_ada = din("b_ada", [96, 128])
    w_in = din("w_in", [D, DIN])
    lam_re = din("lam_re", [64, 64])
    lam_im = din("lam_im", [64, 64])
    log_dt = din("log_dt", [1, 64])
    b_re = din("b_re", [64, 64, 16])
    b_im = din("b_im", [64, 64, 16])
    c_re = din("c_re", [1024, 64])
    c_im = din("c_im", [1024, 64])
    w_glu = din("w_glu", [1024, 1024])
    w_out = din("w_out", [D, D])
    w_mi = din("w_mlp_in", [D, 4 * D])
    w_mo = din("w_mlp_out", [4 * D, D])
    out_d = Tile(nc.dram_tensor("out", [2048, D], F32, kind="ExternalOutput").ap())
    dbg_d = None
    if dbg:
        dbg_d = Tile(nc.dram_tensor("dbg", [128, 8192], F32, kind="ExternalOutput").ap())

    uT_scr = dscr("uT_scr", [128, 8, L], BF16)
    KT_scr = dscr("KT_scr", [128, 2, L], BF16)
    V_scr = dscr("V_scr", [128, NBLK, 256], BF16)
    kiT_scr = dscr("kiT_scr", [128, L], BF16)
    hTo_scr = dscr("hTo_scr", [128, NCH, KT, 128], BF16)
    QT_scr = dscr("QT_scr", [128, NCH, 8, 128], BF16)
    qiT_scr = dscr("qiT_scr", [128, NCH, 8, 128], BF16)
    x1_scr = dscr("x1_scr", [128, NCH, D], F32)

    top = es
    cst = sb(top, "cst", [128, CW], F32)
    identb = sb(top, "identb", [128, 128], BF16)
    onesb = sb(top, "onesb", [128, 128], BF16)
    colv = sb(top, "colv", [128, 82], F32)
    modT = sb(top, "modT", [128, 96], F32)
    validc = sb(top, "validc", [128, NBLK], F32)
    posf = sb(top, "posf", [128, NBLK], F32)
    wi_all = sb(top, "wi_all", [128, NCH * 16], F32)
    gscA = sb(top, "gscA", [128, 16], F32)
    gscM = sb(top, "gscM", [128, 16], F32)

    ident = cst[:, C_ID:C_ID + 128]
    ones_f = cst[:, C_ONE:C_ONE + 128]

    S.dma("sp", cst[:, :], consts[:, :], W=[cst])
    S.dma("sp", validc[:, :], validT[:, :], W=[validc])
    S.op("dve", lambda h: h.tensor_copy(out=identb[:, :], in_=ident), R=[cst], W=[identb])
    S.op("dve", lambda h: h.tensor_copy(out=onesb[:, :], in_=ones_f), R=[cst], W=[onesb])

    with ExitStack() as p0:
        vst = sb(p0, "vst", [82, 128], F32)
        bst = sb(p0, "bst", [96, 128], F32)
        posi = sb(p0, "posi", [128, NBLK], I32)
        badaT = sb(p0, "badaT", [128, 96], F32)
        scb = sb(p0, "scb", [128, 16], BF16)
        pst = ps(p0, "pst", [128, 128], F32)
        pst2 = ps(p0, "pst2", [128, 128], F32)
        prow = [ps(p0, "prow%d" % i, [1, 512], F32) for i in range(2)]
        pmod = ps(p0, "pmod", [128, 96], F32)
        rowb = [sb(p0, "rowb%d" % i, [1, 512], F32) for i in range(2)]
        wa = [sb(p0, "wa%d" % i, [128, KT * 512], BF16) for i in range(2)]

        S.dma("sp", vst[:, :], vecs[:, :], W=[vst])
        S.dma("sp", bst[:, :], b_ada[:, :], W=[bst])
        S.dma("sp", posi[:, :], posT[:, :], W=[posi])
        S.op("dve", lambda h: h.tensor_copy(out=posf[:, :], in_=posi[:, :]), R=[posi], W=[posf])
        S.op("pe", lambda h: h.transpose(pst[:, 0:82], vst[:, :], cst[0:82, C_ID:C_ID + 82]), R=[vst, cst], W=[pst])
        S.op("dve", lambda h: h.tensor_copy(out=colv[:, :], in_=pst[:, 0:82]), R=[pst], W=[colv])
        S.op("pe", lambda h: h.transpose(pst2[:, 0:96], bst[:, :], cst[0:96, C_ID:C_ID + 96]), R=[bst, cst], W=[pst2])
        S.op("dve", lambda h: h.tensor_copy(out=badaT[:, :], in_=pst2[:, 0:96]), R=[pst2], W=[badaT])
        S.op("act", lambda h: h.activation(out=scb[:, :], in_=colv[:, 0:16], func=AF.Silu), R=[colv], W=[scb])
        w_ada_v = w_ada.t.rearrange("(kt p) n -> p kt n", p=128)
        for c in range(24):
            wt = wa[c % 2]
            S.dma("pool", wt[:, :].rearrange("p (kt n) -> p kt n", kt=KT),
                  w_ada_v[:, :, c * 512:(c + 1) * 512], W=[wt])
            pr = prow[c % 2]
            for kt in range(KT):
                S.op("pe", lambda h, kt=kt, wt=wt, pr=pr: h.matmul(
                    pr[:, :], scb[:, kt:kt + 1], wt[:, kt * 512:(kt + 1) * 512],
                    start=(kt == 0), stop=(kt == KT - 1)), R=[scb, wt], W=[pr])
            rb = rowb[c % 2]
            S.op("act", lambda h, rb=rb, pr=pr: h.copy(out=rb[:, :], in_=pr[:, :]), R=[pr], W=[rb])
            for i in range(4):
                col = 4 * c + i
                S.op("pe", lambda h, rb=rb, i=i, col=col: h.matmul(
                    pmod[:, col:col + 1], rb[0:1, i * 128:(i + 1) * 128], cst[0:1, C_ONE:C_ONE + 1],
                    start=True, stop=True), R=[rb, cst], W=[pmod])
        S.op("dve", lambda h: h.tensor_tensor(out=modT[:, :], in0=pmod[:, :], in1=badaT[:, :], op=ALU.add),
             R=[pmod, badaT], W=[modT])
        S.op("dve", lambda h: h.scalar_tensor_tensor(out=gscA[:, :], in0=modT[:, 16:32], scalar=1.0,
                                                     in1=colv[:, 16:32], op0=ALU.add, op1=ALU.mult),
             R=[modT, colv], W=[gscA])
        S.op("dve", lambda h: h.scalar_tensor_tensor(out=gscM[:, :], in0=modT[:, 64:80], scalar=1.0,
                                                     in1=colv[:, 32:48], op0=ALU.add, op1=ALU.mult),
             R=[modT, colv], W=[gscM])
        S.barrier()

    def bcast_row(st, src_tile, src_ap_cols, name, pbank):
        outt = sb(st, name, [128, D], F32)
        with ExitStack() as tmp:
            dg = [sb(tmp, name + "_dg%d" % i, [128, 128], F32) for i in range(2)]
            for kt in range(KT):
                d_ = dg[kt % 2]
                S.op("dve", lambda h, d_=d_, kt=kt: h.tensor_scalar(
                    out=d_[:, :], in0=ident, scalar1=src_ap_cols[:, kt:kt + 1], scalar2=None, op0=ALU.mult),
                    R=[cst, src_tile], W=[d_])
                pb = pbank[kt % 2]
                S.op("pe", lambda h, d_=d_, pb=pb: h.matmul(pb[:, 0:128], ones_f, d_[:, :], start=True, stop=True),
                     R=[cst, d_], W=[pb])
                S.op("act", lambda h, pb=pb, kt=kt: h.copy(out=outt[:, kt * 128:(kt + 1) * 128], in_=pb[:, 0:128]),
                     R=[pb], W=[outt])
            S.barrier()
        return outt

    if dbg and stop == 0:
        S.dma("sp", dbg_d[:, 0:96], modT[:, :], R=[modT], W=[dbg_d])
        S.dma("sp", dbg_d[:, 96:178], colv[:, :], R=[colv], W=[dbg_d])
    if stop == 0:
        S.finish([dbg_d] if dbg else [])
        es.close()
        return nc


    def sincos(st, ang, n, sin_out, cos_out, tag):
        tf = sb(st, tag + "_tf", [128, n], F32)
        ti = sb(st, tag + "_ti", [128, n], I32)
        r = sb(st, tag + "_r", [128, n], F32)
        m = sb(st, tag + "_m", [128, n], F32)
        for which, shift, outap, outtile in ((0, 0.0, sin_out[0], sin_out[1]), (1, PI / 2, cos_out[0], cos_out[1])):
            S.op("dve", lambda h, shift=shift: h.tensor_scalar(out=tf[:, :], in0=ang[:, :], scalar1=shift, scalar2=1.0 / TWO_PI,
                                                  op0=ALU.add, op1=ALU.mult), R=[ang], W=[tf])
            S.op("dve", lambda h: h.tensor_copy(out=ti[:, :], in_=tf[:, :]), R=[tf], W=[ti])
            S.op("dve", lambda h: h.tensor_copy(out=tf[:, :], in_=ti[:, :]), R=[ti], W=[tf])
            S.op("dve", lambda h: h.scalar_tensor_tensor(out=r[:, :], in0=tf[:, :], scalar=-TWO_PI, in1=ang[:, :],
                                                         op0=ALU.mult, op1=ALU.add), R=[tf, ang], W=[r])
            if shift != 0.0:
                S.op("dve", lambda h, shift=shift: h.tensor_scalar(out=r[:, :], in0=r[:, :], scalar1=shift, scalar2=None, op0=ALU.add),
                     R=[r], W=[r])
            S.op("dve", lambda h: h.tensor_scalar(out=m[:, :], in0=r[:, :], scalar1=PI, scalar2=-TWO_PI,
                                                  op0=ALU.is_gt, op1=ALU.mult), R=[r], W=[m])
            S.op("dve", lambda h: h.tensor_tensor(out=r[:, :], in0=r[:, :], in1=m[:, :], op=ALU.add), R=[r, m], W=[r])
            S.op("dve", lambda h: h.tensor_scalar(out=m[:, :], in0=r[:, :], scalar1=-PI, scalar2=TWO_PI,
                                                  op0=ALU.is_lt, op1=ALU.mult), R=[r], W=[m])
            S.op("dve", lambda h: h.tensor_tensor(out=r[:, :], in0=r[:, :], in1=m[:, :], op=ALU.add), R=[r, m], W=[r])
            S.op("dve", lambda h: h.tensor_scalar(out=r[:, :], in0=r[:, :], scalar1=-3.1415925, scalar2=3.1415925,
                                                  op0=ALU.max, op1=ALU.min), R=[r], W=[r])
            S.op("act", lambda h, outap=outap: h.activation(out=outap, in_=r[:, :], func=AF.Sin), R=[r], W=[outtile])

    rp = ExitStack()
    SINH = sb(rp, "SINH", [128, NBLK * 16], F32)
    COSH = sb(rp, "COSH", [128, NBLK * 16], F32)
    SINI = sb(rp, "SINI", [128, NBLK * 8], F32)
    COSI = sb(rp, "COSI", [128, NBLK * 8], F32)
    with ExitStack() as tmp:
        angh = sb(tmp, "angh", [128, NBLK * 16], F32)
        angi = sb(tmp, "angi", [128, NBLK * 8], F32)
        S.op("dve", lambda h: h.tensor_tensor(
            out=angh[:, :].rearrange("p (b i) -> p b i", i=16),
            in0=posf[:, :].unsqueeze(2).to_broadcast([128, NBLK, 16]),
            in1=cst[:, C_INVH:C_INVH + 16].unsqueeze(1).to_broadcast([128, NBLK, 16]), op=ALU.mult),
            R=[posf, cst], W=[angh])
        S.op("dve", lambda h: h.tensor_tensor(
            out=angi[:, :].rearrange("p (b i) -> p b i", i=8),
            in0=posf[:, :].unsqueeze(2).to_broadcast([128, NBLK, 8]),
            in1=cst[:, C_INVI:C_INVI + 8].unsqueeze(1).to_broadcast([128, NBLK, 8]), op=ALU.mult),
            R=[posf, cst], W=[angi])
        sincos(tmp, angh, NBLK * 16, (SINH[:, :], SINH), (COSH[:, :], COSH), "sch")
        sincos(tmp, angi, NBLK * 8, (SINI[:, :], SINI), (COSI[:, :], COSI), "sci")
        S.barrier()

    def rope(eng, st_tiles, x_ap3, nh, half, sin_t, cos_t, blk, out_ap3, Rb, Wb):
        ta, tb = st_tiles
        sn = sin_t[:, blk * half:(blk + 1) * half].unsqueeze(1).to_broadcast([128, nh, half])
        cs = cos_t[:, blk * half:(blk + 1) * half].unsqueeze(1).to_broadcast([128, nh, half])
        x1 = x_ap3[:, :, 0:half]
        x2 = x_ap3[:, :, half:2 * half]
        tav = ta[:, 0:nh * half].rearrange("p (a b) -> p a b", b=half)
        tbv = tb[:, 0:nh * half].rearrange("p (a b) -> p a b", b=half)
        S.op(eng, lambda h: h.tensor_tensor(out=tav, in0=x1, in1=cs, op=ALU.mult), R=Rb + [cos_t], W=[ta])
        S.op(eng, lambda h: h.tensor_tensor(out=tbv, in0=x2, in1=sn, op=ALU.mult), R=Rb + [sin_t], W=[tb])
        S.op(eng, lambda h: h.tensor_tensor(out=out_ap3[:, :, 0:half], in0=tav, in1=tbv, op=ALU.subtract),
             R=[ta, tb], W=Wb)
        S.op(eng, lambda h: h.tensor_tensor(out=tav, in0=x1, in1=sn, op=ALU.mult), R=Rb + [sin_t], W=[ta])
        S.op(eng, lambda h: h.tensor_tensor(out=tbv, in0=x2, in1=cs, op=ALU.mult), R=Rb + [cos_t], W=[tb])
        S.op(eng, lambda h: h.tensor_tensor(out=out_ap3[:, :, half:2 * half], in0=tav, in1=tbv, op=ALU.add),
             R=[ta, tb], W=Wb)

    with ExitStack() as p1:
        NA = 1600
        win_a = sb(p1, "win_a", [128, KT * NA], BF16)
        wv = win_a[:, :].rearrange("p (kt n) -> p kt n", kt=KT)
        w_in_v = w_in.t.rearrange("(kt p) n -> p kt n", p=128)
        for q4 in range(4):
            S.dma("pool", wv[:, 4 * q4:4 * q4 + 4, 0:1024], w_in_v[:, 4 * q4:4 * q4 + 4, U0:U0 + 1024], W=[win_a])
        for q4 in range(2):
            S.dma("pool", wv[:, 8 * q4:8 * q4 + 8, 1024:1536], w_in_v[:, 8 * q4:8 * q4 + 8, K0:K0 + 512], W=[win_a])
        S.dma("pool", wv[:, :, 1536:1600], w_in_v[:, :, KI0:KI0 + 64], W=[win_a])
        pu = [ps(p1, "pu%d" % i, [128, 512], F32) for i in range(2)]
        gsc_bc = bcast_row(p1, gscA, gscA[:, :], "gscA_bc", pu)
        sh_bc = bcast_row(p1, modT, modT[:, 0:16], "shA_bc", pu)
        gk_bc = sb(p1, "gk_bc", [128, 128], F32)
        dgk = sb(p1, "dgk", [128, 128], F32)
        S.op("dve", lambda h: h.tensor_scalar(out=dgk[:, :], in0=ident, scalar1=colv[:, 81:82], scalar2=None, op0=ALU.mult),
             R=[cst, colv], W=[dgk])
        S.op("pe", lambda h: h.matmul(pu[0][:, 0:128], ones_f, dgk[:, :], start=True, stop=True), R=[cst, dgk], W=[pu[0]])
        S.op("act", lambda h: h.copy(out=gk_bc[:, :], in_=pu[0][:, 0:128]), R=[pu[0]], W=[gk_bc])

        NXB = 5
        xt = [sb(p1, "xt%d" % i, [128, D], F32) for i in range(NXB)]
        junk = sb(p1, "junk", [128, D], BF16)
        tmpf = sb(p1, "tmpf", [128, D], BF16)
        hb = [sb(p1, "hb%d" % i, [128, D], BF16) for i in range(2)]
        hT = [sb(p1, "hT%d" % i, [128, KT * 512], BF16) for i in range(2)]
        ss = sb(p1, "ss", [128, NBLK], F32)
        ms = sb(p1, "ms", [128, NBLK], F32)
        rstd = sb(p1, "rstd", [128, NBLK], F32)
        rstdv = sb(p1, "rstdv", [128, NBLK], F32)
        ptr = [ps(p1, "ptr%d" % i, [128, 1024], BF16) for i in range(2)]
        pkvs = [ps(p1, "pkv%d" % i, [128, 512], F32) for i in range(2)]
        pki = ps(p1, "pki", [128, 64], F32)
        pkt = ps(p1, "pkt", [128, 384], BF16)
        ust = [sb(p1, "ust%d" % i, [128, 8 * 512], BF16) for i in range(2)]
        vstg = [sb(p1, "vstg%d" % i, [128, 4 * 256], BF16) for i in range(2)]
        kst = [sb(p1, "kst%d" % i, [128, 2 * 512], BF16) for i in range(1)] * 2
        kist = [sb(p1, "kist%d" % i, [128, 512], BF16) for i in range(1)] * 2
        ssk = sb(p1, "ssk", [128, 2 * NBLK], F32)
        msk = sb(p1, "msk", [128, 2 * NBLK], F32)
        rsk = sb(p1, "rsk", [128, 2 * NBLK], F32)
        kn = [sb(p1, "kn%d" % i, [128, 256], F32) for i in range(1)] * 2
        kr = [sb(p1, "kr%d" % i, [128, 256], BF16) for i in range(2)]
        kif = [sb(p1, "kif%d" % i, [128, 64], F32) for i in range(2)]
        kib = [sb(p1, "kib%d" % i, [128, 128], BF16) for i in range(2)]
        rta = sb(p1, "rta", [128, 32], F32)
        rtb = sb(p1, "rtb", [128, 32], F32)
        jk = sb(p1, "jk", [128, 128], BF16)

        def load_x(blk):
            x_ = xt[blk % NXB]
            S.dma("sp", x_[:, :], xv[blk * 128:(blk + 1) * 128, :], W=[x_])

        def stats(blk):
            x_ = xt[blk % NXB]
            S.op("act", lambda h: h.activation(out=junk[:, :], in_=x_[:, :], func=AF.Square,
                                               accum_out=ss[:, blk:blk + 1]), R=[x_], W=[junk, ss])
            S.op("dve", lambda h: h.tensor_scalar(out=ms[:, blk:blk + 1], in0=ss[:, blk:blk + 1], scalar1=1.0 / D,
                                                  scalar2=EPS, op0=ALU.mult, op1=ALU.add), R=[ss], W=[ms])
            S.op("pool", lambda h: h.tensor_tensor(out=rstd[:, blk:blk + 1], in0=ms[:, blk:blk + 1],
                                                   in1=cst[:, C_NHALF:C_NHALF + 1], op=ALU.pow), R=[ms, cst], W=[rstd])
            S.op("dve", lambda h: h.tensor_tensor(out=rstdv[:, blk:blk + 1], in0=rstd[:, blk:blk + 1],
                                                  in1=validc[:, blk:blk + 1], op=ALU.mult), R=[rstd, validc], W=[rstdv])

        def heavy(blk):
            c, bi = blk // 4, blk % 4
            x_ = xt[blk % NXB]
            h_ = hb[blk % 2]
            hT_ = hT[c % 2]
            S.op("dve", lambda h: h.scalar_tensor_tensor(out=tmpf[:, :], in0=x_[:, :], scalar=rstdv[:, blk:blk + 1],
                                                         in1=gsc_bc[:, :], op0=ALU.mult, op1=ALU.mult),
                 R=[x_, rstdv, gsc_bc], W=[tmpf])
            S.op("dve", lambda h: h.scalar_tensor_tensor(out=h_[:, :], in0=sh_bc[:, :], scalar=validc[:, blk:blk + 1],
                                                         in1=tmpf[:, :], op0=ALU.mult, op1=ALU.add),
                 R=[sh_bc, validc, tmpf], W=[h_])
            hTv = hT_[:, :].rearrange("p (kt n) -> p kt n", kt=KT)
            for half in range(2):
                pt = ptr[half]
                for k8 in range(8):
                    kt = half * 8 + k8
                    S.op("pe", lambda h, kt=kt, k8=k8, pt=pt: h.transpose(
                        pt[:, k8 * 128:(k8 + 1) * 128], h_[:, kt * 128:(kt + 1) * 128], identb[:, :]),
                        R=[h_, identb], W=[pt])
                S.op("act" if half == 0 else "pool" if False else "act", lambda h, half=half, pt=pt: h.copy(
                    out=hTv[:, half * 8:(half + 1) * 8, bi * 128:(bi + 1) * 128],
                    in_=pt[:, :].rearrange("p (a b) -> p a b", b=128)), R=[pt], W=[hT_])

        def upart(c, tiles):
            hT_ = hT[c % 2]
            hTv = hT_[:, :].rearrange("p (kt n) -> p kt n", kt=KT)
            us = ust[c % 2]
            for t in tiles:
                p_ = pu[t % 2]
                for kt in range(KT):
                    S.op("pe", lambda h, kt=kt, t=t, p_=p_: h.matmul(
                        p_[:, :], wv[:, kt, t * 128:(t + 1) * 128], hTv[:, kt, :],
                        start=(kt == 0), stop=(kt == KT - 1)), R=[win_a, hT_], W=[p_])
                S.op("act", lambda h, t=t, p_=p_: h.copy(out=us[:, t * 512:(t + 1) * 512], in_=p_[:, :]), R=[p_], W=[us])
            if 7 in tiles:
                S.dma("pool", uT_scr[:, :, c * 512:(c + 1) * 512], us[:, :].rearrange("p (t n) -> p t n", t=8), R=[us], W=[uT_scr])

        def kv_main(c, bi):
            hT_ = hT[c % 2]
            hTv = hT_[:, :].rearrange("p (kt n) -> p kt n", kt=KT)
            vs = vstg[c % 2]
            blk = 4 * c + bi
            pkv = pkvs[blk % 2]
            for kt in range(KT):
                S.op("pe", lambda h, kt=kt: h.matmul(
                    pkv[:, :], hTv[:, kt, bi * 128:(bi + 1) * 128], wv[:, kt, 1024:1536],
                    start=(kt == 0), stop=(kt == KT - 1)), R=[hT_, win_a], W=[pkv])
            for kt in range(KT):
                S.op("pe", lambda h, kt=kt: h.matmul(
                    pki[:, :], hTv[:, kt, bi * 128:(bi + 1) * 128], wv[:, kt, 1536:1600],
                    start=(kt == 0), stop=(kt == KT - 1)), R=[hT_, win_a], W=[pki])
            S.op("act", lambda h: h.copy(out=vs[:, bi * 256:(bi + 1) * 256], in_=pkv[:, 256:512]), R=[pkv], W=[vs])
            kn_ = kn[blk % 2]
            kr_ = kr[blk % 2]
            for g in range(2):
                S.op("act", lambda h, g=g: h.activation(out=jk[:, :], in_=pkv[:, g * 128:(g + 1) * 128], func=AF.Square,
                                                        accum_out=ssk[:, 2 * blk + g:2 * blk + g + 1]), R=[pkv], W=[jk, ssk])
            kif_ = kif[blk % 2]
            kib_ = kib[blk % 2]
            S.op("act", lambda h: h.copy(out=kif_[:, :], in_=pki[:, :]), R=[pki], W=[kif_])
            S.op("dve", lambda h: h.tensor_scalar(out=msk[:, 2 * blk:2 * blk + 2], in0=ssk[:, 2 * blk:2 * blk + 2],
                                                  scalar1=1.0 / 128, scalar2=EPS, op0=ALU.mult, op1=ALU.add), R=[ssk], W=[msk])
            S.op("pool", lambda h: h.tensor_tensor(out=rsk[:, 2 * blk:2 * blk + 2], in0=msk[:, 2 * blk:2 * blk + 2],
                                                   in1=cst[:, C_NHALF:C_NHALF + 1].to_broadcast([128, 2]), op=ALU.pow),
                 R=[msk, cst], W=[rsk])
            for g in range(2):
                S.op("dve", lambda h, g=g: h.scalar_tensor_tensor(
                    out=kn_[:, g * 128:(g + 1) * 128], in0=pkv[:, g * 128:(g + 1) * 128],
                    scalar=rsk[:, 2 * blk + g:2 * blk + g + 1], in1=gk_bc[:, :], op0=ALU.mult, op1=ALU.mult),
                    R=[pkv, rsk, gk_bc], W=[kn_])
            S.op("pool", lambda h: h.tensor_copy(out=kr_[:, :], in_=kn_[:, :]), R=[kn_], W=[kr_])
            rope("pool", (rta, rtb), kn_[:, :].rearrange("p (a b) -> p a b", b=128), 2, 16, SINH, COSH, blk,
                 kr_[:, :].rearrange("p (a b) -> p a b", b=128), [kn_], [kr_])
            S.op("pool", lambda h: h.tensor_copy(out=kib_[:, 0:64], in_=kif_[:, :]), R=[kif_], W=[kib_])
            rope("pool", (rta, rtb), kif_[:, :].rearrange("p (a b) -> p a b", a=1), 1, 8, SINI, COSI, blk,
                 kib_[:, 0:64].rearrange("p (a b) -> p a b", a=1), [kif_], [kib_])
            S.op("pool", lambda h: h.tensor_copy(out=kib_[:, 64:128], in_=kib_[:, 0:64]), R=[kib_], W=[kib_])

        def kv_tail(c, bi):
            hT_ = hT[c % 2]
            hTv = hT_[:, :].rearrange("p (kt n) -> p kt n", kt=KT)
            vs = vstg[c % 2]
            ks = kst[c % 2]
            kis = kist[c % 2]
            blk = 4 * c + bi
            kr_ = kr[blk % 2]
            kib_ = kib[blk % 2]
            for g in range(2):
                S.op("pe", lambda h, g=g: h.transpose(pkt[:, g * 128:(g + 1) * 128], kr_[:, g * 128:(g + 1) * 128], identb[:, :]),
                     R=[kr_, identb], W=[pkt])
            S.op("pe", lambda h: h.transpose(pkt[:, 256:384], kib_[:, :], identb[:, :]), R=[kib_, identb], W=[pkt])
            S.op("act", lambda h: h.copy(
                out=ks[:, :].rearrange("p (g n) -> p g n", g=2)[:, :, bi * 128:(bi + 1) * 128],
                in_=pkt[:, 0:256].rearrange("p (g n) -> p g n", g=2)), R=[pkt], W=[ks])
            S.op("act", lambda h: h.copy(out=kis[:, bi * 128:(bi + 1) * 128], in_=pkt[:, 256:384]), R=[pkt], W=[kis])
            if bi != 3:
                return
            S.dma("pool", V_scr[:, 4 * c:4 * c + 4, :], vs[:, :].rearrange("p (b n) -> p b n", b=4), R=[vs], W=[V_scr])
            S.dma("pool", KT_scr[:, :, c * 512:(c + 1) * 512], ks[:, :].rearrange("p (g n) -> p g n", g=2), R=[ks], W=[KT_scr])
            S.dma("pool", kiT_scr[:, c * 512:(c + 1) * 512], kis[:, :], R=[kis], W=[kiT_scr])
            S.dma("pool", hTo_scr[:, c, :, :], hTv[:, :, 384:512], R=[hT_], W=[hTo_scr])

        nblk_run = NBLK if FULL else 8
        for b_ in range(4):
            load_x(b_)
        for b_ in range(3):
            stats(b_)
        for blk in range(nblk_run + 5):
            if blk + 4 < nblk_run:
                load_x(blk + 4)
            if blk + 3 < nblk_run:
                stats(blk + 3)
            if blk < nblk_run:
                heavy(blk)
            if 4 <= blk < nblk_run + 4:
                b4 = blk - 4
                upart(b4 // 4, [2 * (b4 % 4), 2 * (b4 % 4) + 1])
                kv_main(b4 // 4, b4 % 4)
            if blk >= 5:
                b5 = blk - 5
                kv_tail(b5 // 4, b5 % 4)
        S.barrier()
        print("n_inst after P1a", S.n_inst)


    with ExitStack() as p1c:
        NC_ = 2064
        win_c = sb(p1c, "win_c", [128, KT * NC_], BF16)
        wc = win_c[:, :].rearrange("p (kt n) -> p kt n", kt=KT)
        w_in_v = w_in.t.rearrange("(kt p) n -> p kt n", p=128)
        for q4 in range(4):
            S.dma("pool", wc[:, 4 * q4:4 * q4 + 4, 0:1024], w_in_v[:, 4 * q4:4 * q4 + 4, Q0:Q0 + 1024], W=[win_c])
        for q4 in range(4):
            S.dma("pool", wc[:, 4 * q4:4 * q4 + 4, 1024:2048], w_in_v[:, 4 * q4:4 * q4 + 4, QI0:QI0 + 1024], W=[win_c])
        S.dma("pool", wc[:, :, 2048:2064], w_in_v[:, :, WI0:WI0 + 16], W=[win_c])
        pq = [ps(p1c, "pq%d" % i, [128, 512], F32) for i in range(2)]
        pqi = [ps(p1c, "pqi%d" % i, [128, 512], F32) for i in range(2)]
        pwi = ps(p1c, "pwi", [128, 512], F32)
        ptq = ps(p1c, "ptq", [128, 1024], BF16)
        ptqi = ps(p1c, "ptqi", [128, 1024], BF16)
        gq_bc = sb(p1c, "gq_bc", [128, 128], F32)
        dgq = sb(p1c, "dgq", [128, 128], F32)
        S.op("dve", lambda h: h.tensor_scalar(out=dgq[:, :], in0=ident, scalar1=colv[:, 80:81], scalar2=None, op0=ALU.mult),
             R=[cst, colv], W=[dgq])
        S.op("pe", lambda h: h.matmul(pwi[:, 0:128], ones_f, dgq[:, :], start=True, stop=True), R=[cst, dgq], W=[pwi])
        S.op("act", lambda h: h.copy(out=gq_bc[:, :], in_=pwi[:, 0:128]), R=[pwi], W=[gq_bc])
        hTo = [sb(p1c, "hTo%d" % i, [128, KT * 128], BF16) for i in range(2)]
        ssq = sb(p1c, "ssq", [128, 8 * NCH], F32)
        msq = sb(p1c, "msq", [128, 8 * NCH], F32)
        rsq = sb(p1c, "rsq", [128, 8 * NCH], F32)
        jq = sb(p1c, "jq", [128, 128], BF16)
        qn = sb(p1c, "qn", [128, 1024], F32)
        qr = sb(p1c, "qr", [128, 1024], BF16)
        qif = sb(p1c, "qif", [128, 1024], F32)
        qib = sb(p1c, "qib", [128, 1024], BF16)
        rta2 = sb(p1c, "rta2", [128, 128], F32)
        rtb2 = sb(p1c, "rtb2", [128, 128], F32)
        qTs = [sb(p1c, "qTs%d" % i, [128, 1024], BF16) for i in range(2)]
        qiTs = [sb(p1c, "qiTs%d" % i, [128, 1024], BF16) for i in range(2)]
        nm = NCH if FULL else 2
        def c_mm(m):
            blk = 4 * m + 3
            ho = hTo[m % 2]
            hov = ho[:, :].rearrange("p (kt n) -> p kt n", kt=KT)
            S.dma("sp", hov, hTo_scr[:, m, :, :], R=[hTo_scr], W=[ho])
            for cch in range(2):
                for kt in range(KT):
                    S.op("pe", lambda h, kt=kt, cch=cch: h.matmul(
                        pq[cch][:, :], hov[:, kt, :], wc[:, kt, cch * 512:(cch + 1) * 512],
                        start=(kt == 0), stop=(kt == KT - 1)), R=[ho, win_c], W=[pq[cch]])
            for cch in range(2):
                for kt in range(KT):
                    S.op("pe", lambda h, kt=kt, cch=cch: h.matmul(
                        pqi[cch][:, :], hov[:, kt, :], wc[:, kt, 1024 + cch * 512:1024 + (cch + 1) * 512],
                        start=(kt == 0), stop=(kt == KT - 1)), R=[ho, win_c], W=[pqi[cch]])
            for kt in range(KT):
                S.op("pe", lambda h, kt=kt: h.matmul(
                    pwi[:, 0:16], hov[:, kt, :], wc[:, kt, 2048:2064],
                    start=(kt == 0), stop=(kt == KT - 1)), R=[ho, win_c], W=[pwi])
            S.op("dve", lambda h: h.tensor_scalar(out=wi_all[:, 16 * m:16 * m + 16], in0=pwi[:, 0:16], scalar1=0.25 * 0.125,
                                                  scalar2=None, op0=ALU.mult), R=[pwi], W=[wi_all])

        def c_post(m):
            blk = 4 * m + 3
            for hh in range(8):
                S.op("act", lambda h, hh=hh: h.activation(
                    out=jq[:, :], in_=pq[hh // 4][:, (hh % 4) * 128:(hh % 4 + 1) * 128], func=AF.Square,
                    accum_out=ssq[:, 8 * m + hh:8 * m + hh + 1]), R=[pq[hh // 4]], W=[jq, ssq])
            S.op("dve", lambda h: h.tensor_scalar(out=msq[:, 8 * m:8 * m + 8], in0=ssq[:, 8 * m:8 * m + 8], scalar1=1.0 / 128,
                                                  scalar2=EPS, op0=ALU.mult, op1=ALU.add), R=[ssq], W=[msq])
            S.op("pool", lambda h: h.tensor_tensor(out=rsq[:, 8 * m:8 * m + 8], in0=msq[:, 8 * m:8 * m + 8],
                                                   in1=cst[:, C_NHALF:C_NHALF + 1].to_broadcast([128, 8]), op=ALU.pow),
                 R=[msq, cst], W=[rsq])
            for hh in range(8):
                S.op("dve", lambda h, hh=hh: h.scalar_tensor_tensor(
                    out=qn[:, hh * 128:(hh + 1) * 128], in0=pq[hh // 4][:, (hh % 4) * 128:(hh % 4 + 1) * 128],
                    scalar=rsq[:, 8 * m + hh:8 * m + hh + 1], in1=gq_bc[:, :], op0=ALU.mult, op1=ALU.mult),
                    R=[pq[hh // 4], rsq, gq_bc], W=[qn])
            S.op("pool", lambda h: h.tensor_copy(out=qr[:, :], in_=qn[:, :]), R=[qn], W=[qr])
            rope("pool", (rta2, rtb2), qn[:, :].rearrange("p (a b) -> p a b", b=128), 8, 16, SINH, COSH, blk,
                 qr[:, :].rearrange("p (a b) -> p a b", b=128), [qn], [qr])
            for cch in range(2):
                S.op("act", lambda h, cch=cch: h.copy(out=qif[:, cch * 512:(cch + 1) * 512], in_=pqi[cch][:, :]), R=[pqi[cch]], W=[qif])
            S.op("pool", lambda h: h.tensor_copy(out=qib[:, :], in_=qif[:, :]), R=[qif], W=[qib])
            rope("pool", (rta2, rtb2), qif[:, :].rearrange("p (a b) -> p a b", b=64), 16, 8, SINI, COSI, blk,
                 qib[:, :].rearrange("p (a b) -> p a b", b=64), [qif], [qib])

        def c_tail(m):
            for hh in range(8):
                S.op("pe", lambda h, hh=hh: h.transpose(ptq[:, hh * 128:(hh + 1) * 128], qr[:, hh * 128:(hh + 1) * 128], identb[:, :]),
                     R=[qr, identb], W=[ptq])
            qs = qTs[m % 2]
            S.op("act", lambda h: h.copy(out=qs[:, :], in_=ptq[:, :]), R=[ptq], W=[qs])
            S.dma("sp", QT_scr[:, m, :, :], qs[:, :].rearrange("p (a b) -> p a b", b=128), R=[qs], W=[QT_scr])
            for pr in range(8):
                S.op("pe", lambda h, pr=pr: h.transpose(ptqi[:, pr * 128:(pr + 1) * 128], qib[:, pr * 128:(pr + 1) * 128], identb[:, :]),
                     R=[qib, identb], W=[ptqi])
            qis = qiTs[m % 2]
            S.op("act", lambda h: h.copy(out=qis[:, :], in_=ptqi[:, :]), R=[ptqi], W=[qis])
            S.dma("sp", qiT_scr[:, m, :, :], qis[:, :].rearrange("p (a b) -> p a b", b=128), R=[qis], W=[qiT_scr])

        for m in range(nm + 1):
            if m < nm:
                c_mm(m)
            if m >= 1:
                c_tail(m - 1)
            if m < nm:
                c_post(m)
        S.barrier()
    rp.close()
    print("n_inst after P1c", S.n_inst)

    if stop == 1:
        if dbg:
            with ExitStack() as dd:
                d1 = sb(dd, "d1", [128, 6144], BF16)
                d2 = sb(dd, "d2", [128, 6144], F32)
                S.dma("sp", d1[:, 0:1024], uT_scr[:, 0, 0:1024], R=[uT_scr], W=[d1])
                S.dma("sp", d1[:, 1024:2048], KT_scr[:, 1, 0:1024], R=[KT_scr], W=[d1])
                S.dma("sp", d1[:, 2048:3072].rearrange("p (b n) -> p b n", b=4), V_scr[:, 0:4, :], R=[V_scr], W=[d1])
                S.dma("sp", d1[:, 3072:4096], kiT_scr[:, 0:1024], R=[kiT_scr], W=[d1])
                S.dma("sp", d1[:, 4096:5120].rearrange("p (a b) -> p a b", b=128), QT_scr[:, 1, :, :], R=[QT_scr], W=[d1])
                S.dma("sp", d1[:, 5120:6144].rearrange("p (a b) -> p a b", b=128), qiT_scr[:, 1, :, :], R=[qiT_scr], W=[d1])
                S.op("dve", lambda h: h.tensor_copy(out=d2[:, :], in_=d1[:, :]), R=[d1], W=[d2])
                S.dma("sp", dbg_d[:, 0:6144], d2[:, :], R=[d2], W=[dbg_d])
                S.dma("sp", dbg_d[:, 6144:6144 + 256], wi_all[:, :], R=[wi_all], W=[dbg_d])
                S.finish([dbg_d])
        es.close()
        return nc


    yaT_scr = dscr("yaT_scr", [128, 8, 2048], BF16)

    def sincos2(ang, n, sin_ap, sin_tile, cos_ap, cos_tile, tf, ti, r, m):
        for shift, outap, outtile in ((0.0, sin_ap, sin_tile), (PI / 2, cos_ap, cos_tile)):
            S.op("dve", lambda h, shift=shift: h.tensor_scalar(out=tf[:, 0:n], in0=ang[:, 0:n], scalar1=shift, scalar2=1.0 / TWO_PI,
                                                               op0=ALU.add, op1=ALU.mult), R=[ang], W=[tf])
            S.op("dve", lambda h: h.tensor_copy(out=ti[:, 0:n], in_=tf[:, 0:n]), R=[tf], W=[ti])
            S.op("dve", lambda h: h.tensor_copy(out=tf[:, 0:n], in_=ti[:, 0:n]), R=[ti], W=[tf])
            S.op("dve", lambda h: h.scalar_tensor_tensor(out=r[:, 0:n], in0=tf[:, 0:n], scalar=-TWO_PI, in1=ang[:, 0:n],
                                                         op0=ALU.mult, op1=ALU.add), R=[tf, ang], W=[r])
            if shift != 0.0:
                S.op("dve", lambda h, shift=shift: h.tensor_scalar(out=r[:, 0:n], in0=r[:, 0:n], scalar1=shift, scalar2=None,
                                                                   op0=ALU.add), R=[r], W=[r])
            S.op("dve", lambda h: h.tensor_scalar(out=m[:, 0:n], in0=r[:, 0:n], scalar1=PI, scalar2=-TWO_PI,
                                                  op0=ALU.is_gt, op1=ALU.mult), R=[r], W=[m])
            S.op("dve", lambda h: h.tensor_tensor(out=r[:, 0:n], in0=r[:, 0:n], in1=m[:, 0:n], op=ALU.add), R=[r, m], W=[r])
            S.op("dve", lambda h: h.tensor_scalar(out=m[:, 0:n], in0=r[:, 0:n], scalar1=-PI, scalar2=TWO_PI,
                                                  op0=ALU.is_lt, op1=ALU.mult), R=[r], W=[m])
            S.op("dve", lambda h: h.tensor_tensor(out=r[:, 0:n], in0=r[:, 0:n], in1=m[:, 0:n], op=ALU.add), R=[r, m], W=[r])
            S.op("dve", lambda h: h.tensor_scalar(out=r[:, 0:n], in0=r[:, 0:n], scalar1=-3.1415925, scalar2=3.1415925,
                                                  op0=ALU.max, op1=ALU.min), R=[r], W=[r])
            S.op("act", lambda h, outap=outap: h.activation(out=outap, in_=r[:, 0:n], func=AF.Sin), R=[r], W=[outtile])

    with ExitStack() as s5:
        Bpad = sb(s5, "Bpad", [128, 64 * 128], BF16)
        Bswp = sb(s5, "Bswp", [128, 64 * 128], BF16)
        CWc = sb(s5, "CWc", [128, 64 * 128], BF16)
        CWs = sb(s5, "CWs", [128, 64 * 128], BF16)
        COS = sb(s5, "COS", [128, 64 * 128], F32)
        SIN = sb(s5, "SIN", [128, 64 * 128], F32)
        RHO = sb(s5, "RHO", [128, 64], F32)
        CT = sb(s5, "CT", [128, 64], F32)
        STs = sb(s5, "STs", [128, 64], F32)
        TH = sb(s5, "TH", [128, 64], F32)
        Wt = sb(s5, "Wt", [128, 64 * 128], F32)
        R128 = sb(s5, "R128", [128, 64], F32)
        junkz = sb(s5, "junkz", [128, 128], F32)
        pA = [ps(s5, "pA%d" % i, [128, 512], F32) for i in range(2)]
        pB = [ps(s5, "pB%d" % i, [128, 512], F32) for i in range(2)]
        pY = [ps(s5, "pY%d" % i, [128, 512], F32) for i in range(2)]
        pS = ps(s5, "pS", [128, 512], F32)
        pS2 = ps(s5, "pS2", [128, 512], F32)
        with ExitStack() as su:
            lst_r = sb(su, "lst_r", [64, 128], F32)
            lst_i = sb(su, "lst_i", [64, 128], F32)
            ldr = sb(su, "ldr", [1, 64], F32)
            S.dma("sp", lst_r[:, 0:64], lam_re[:, :], W=[lst_r])
            S.dma("sp", lst_r[:, 64:128], lam_re[:, :], W=[lst_r])
            S.dma("sp", lst_i[:, 0:64], lam_im[:, :], W=[lst_i])
            S.dma("sp", lst_i[:, 64:128], lam_im[:, :], W=[lst_i])
            S.dma("sp", ldr[:, :], log_dt[:, :], W=[ldr])
            names = ["LR", "LI", "DT", "A_", "COST", "SINT", "ABR", "ABI", "AM1", "DEN", "FR", "FI", "T1", "T2", "TH128", "S128", "C128"]
            V = {n_: sb(su, "s5_" + n_, [128, 64], F32) for n_ in names}
            id64 = cst[0:64, C_ID:C_ID + 64]
            S.op("pe", lambda h: h.transpose(pS[:, 0:64], lst_r[:, :], id64), R=[lst_r, cst], W=[pS])
            S.op("dve", lambda h: h.tensor_scalar(out=V["LR"][:, :], in0=pS[:, 0:64], scalar1=-1e-4, scalar2=None, op0=ALU.min),
                 R=[pS], W=[V["LR"]])
            S.op("pe", lambda h: h.transpose(pS2[:, 0:64], lst_i[:, :], id64), R=[lst_i, cst], W=[pS2])
            S.op("dve", lambda h: h.tensor_copy(out=V["LI"][:, :], in_=pS2[:, 0:64]), R=[pS2], W=[V["LI"]])
            S.op("pe", lambda h: h.matmul(pS[:, 64:128], cst[0:1, C_ONE:C_ONE + 128], ldr[0:1, :], start=True, stop=True),
                 R=[cst, ldr, V["LR"]], W=[pS])
            S.op("act", lambda h: h.activation(out=V["DT"][:, :], in_=pS[:, 64:128], func=AF.Exp), R=[pS], W=[V["DT"]])
            tt = lambda o, a, b, op: S.op("dve", lambda h: h.tensor_tensor(out=V[o][:, :], in0=V[a][:, :], in1=V[b][:, :], op=op),
                                          R=[V[a], V[b]], W=[V[o]])
            tt("A_", "LR", "DT", ALU.mult)
            S.op("act", lambda h: h.activation(out=RHO[:, :], in_=V["A_"][:, :], func=AF.Exp), R=[V["A_"]], W=[RHO])
            S.op("dve", lambda h: h.tensor_tensor(out=TH[:, :], in0=V["LI"][:, :], in1=V["DT"][:, :], op=ALU.mult),
                 R=[V["LI"], V["DT"]], W=[TH])
            su1 = ExitStack()
            tfa = sb(su1, "tfa", [128, 1024], F32)
            tia = sb(su1, "tia", [128, 1024], I32)
            ra = sb(su1, "ra", [128, 1024], F32)
            ma = sb(su1, "ma", [128, 1024], F32)
            anga = sb(su1, "anga", [128, 1024], F32)
            sincos2(TH, 64, V["SINT"][:, :], V["SINT"], V["COST"][:, :], V["COST"], tfa, tia, ra, ma)
            S.op("dve", lambda h: h.tensor_tensor(out=V["ABR"][:, :], in0=RHO[:, :], in1=V["COST"][:, :], op=ALU.mult),
                 R=[RHO, V["COST"]], W=[V["ABR"]])
            S.op("dve", lambda h: h.tensor_tensor(out=V["ABI"][:, :], in0=RHO[:, :], in1=V["SINT"][:, :], op=ALU.mult),
                 R=[RHO, V["SINT"]], W=[V["ABI"]])
            S.op("dve", lambda h: h.tensor_scalar(out=V["AM1"][:, :], in0=V["ABR"][:, :], scalar1=-1.0, scalar2=None, op0=ALU.add),
                 R=[V["ABR"]], W=[V["AM1"]])
            tt("T1", "LR", "LR", ALU.mult)
            tt("T2", "LI", "LI", ALU.mult)
            tt("DEN", "T1", "T2", ALU.add)
            S.op("dve", lambda h: h.reciprocal(out=V["DEN"][:, :], in_=V["DEN"][:, :]), R=[V["DEN"]], W=[V["DEN"]])
            tt("T1", "AM1", "LR", ALU.mult)
            tt("T2", "ABI", "LI", ALU.mult)
            tt("FR", "T1", "T2", ALU.add)
            tt("FR", "FR", "DEN", ALU.mult)
            tt("T1", "ABI", "LR", ALU.mult)
            tt("T2", "AM1", "LI", ALU.mult)
            tt("FI", "T1", "T2", ALU.subtract)
            tt("FI", "FI", "DEN", ALU.mult)
            S.op("dve", lambda h: h.tensor_scalar(out=V["TH128"][:, :], in0=TH[:, :], scalar1=128.0, scalar2=None, op0=ALU.mult),
                 R=[TH], W=[V["TH128"]])
            sincos2(V["TH128"], 64, V["S128"][:, :], V["S128"], CT[:, :], CT, tfa, tia, ra, ma)
            S.op("dve", lambda h: h.tensor_scalar(out=STs[:, :], in0=V["S128"][:, :], scalar1=cst[:, C_SG:C_SG + 1], scalar2=-1.0,
                                                  op0=ALU.mult, op1=ALU.mult), R=[V["S128"], cst], W=[STs])
            for sl in range(8):
                S.op("dve", lambda h, sl=sl: h.tensor_tensor(
                    out=anga[:, :].rearrange("p (g j) -> p g j", j=128),
                    in0=TH[:, sl * 8:(sl + 1) * 8].unsqueeze(2).to_broadcast([128, 8, 128]),
                    in1=cst[:, C_IOTA:C_IOTA + 128].unsqueeze(1).to_broadcast([128, 8, 128]), op=ALU.mult),
                    R=[TH, cst], W=[anga])
                sincos2(anga, 1024, SIN[:, sl * 1024:(sl + 1) * 1024], SIN, COS[:, sl * 1024:(sl + 1) * 1024], COS, tfa, tia, ra, ma)
            rev = sb(su1, "rev", [128, 128], F32)
            S.op("dve", lambda h: h.tensor_scalar(out=rev[:, :], in0=cst[:, C_IOTA:C_IOTA + 128], scalar1=-1.0, scalar2=127.0,
                                                  op0=ALU.mult, op1=ALU.add), R=[cst], W=[rev])
            for sl in range(8):
                S.op("dve", lambda h, sl=sl: h.tensor_tensor(
                    out=anga[:, :].rearrange("p (g j) -> p g j", j=128),
                    in0=V["A_"][:, sl * 8:(sl + 1) * 8].unsqueeze(2).to_broadcast([128, 8, 128]),
                    in1=rev[:, :].unsqueeze(1).to_broadcast([128, 8, 128]), op=ALU.mult),
                    R=[V["A_"], rev], W=[anga])
                S.op("act", lambda h, sl=sl: h.activation(out=Wt[:, sl * 1024:(sl + 1) * 1024], in_=anga[:, :], func=AF.Exp),
                     R=[anga], W=[Wt])
            S.op("act", lambda h: h.activation(out=R128[:, :], in_=V["A_"][:, :], func=AF.Exp, scale=128.0), R=[V["A_"]], W=[R128])
            S.barrier()
            su1.close()
            BRE = sb(su, "BRE", [64, 1024], F32)
            BIM = sb(su, "BIM", [64, 1024], F32)
            BBR = sb(su, "BBR", [64, 1024], F32)
            BBI = sb(su, "BBI", [64, 1024], F32)
            NBR = sb(su, "NBR", [64, 1024], F32)
            BT = sb(su, "BT", [64, 1024], F32)
            S.dma("sp", BRE[:, :].rearrange("p (g h) -> p g h", h=16), b_re.t.rearrange("g p h -> p g h"), W=[BRE])
            S.dma("sp", BIM[:, :].rearrange("p (g h) -> p g h", h=16), b_im.t.rearrange("g p h -> p g h"), W=[BIM])
            frb = V["FR"][0:64, :].unsqueeze(2).to_broadcast([64, 64, 16])
            fib = V["FI"][0:64, :].unsqueeze(2).to_broadcast([64, 64, 16])
            v3 = lambda t_: t_[:, :].rearrange("p (g h) -> p g h", h=16)
            S.op("dve", lambda h: h.tensor_tensor(out=v3(BBR), in0=v3(BRE), in1=frb, op=ALU.mult), R=[BRE, V["FR"]], W=[BBR])
            S.op("dve", lambda h: h.tensor_tensor(out=v3(BT), in0=v3(BIM), in1=fib, op=ALU.mult), R=[BIM, V["FI"]], W=[BT])
            S.op("dve", lambda h: h.tensor_tensor(out=BBR[:, :], in0=BBR[:, :], in1=BT[:, :], op=ALU.subtract), R=[BBR, BT], W=[BBR])
            S.op("dve", lambda h: h.tensor_tensor(out=v3(BBI), in0=v3(BIM), in1=frb, op=ALU.mult), R=[BIM, V["FR"]], W=[BBI])
            S.op("dve", lambda h: h.tensor_tensor(out=v3(BT), in0=v3(BRE), in1=fib, op=ALU.mult), R=[BRE, V["FI"]], W=[BT])
            S.op("dve", lambda h: h.tensor_tensor(out=BBI[:, :], in0=BBI[:, :], in1=BT[:, :], op=ALU.add), R=[BBI, BT], W=[BBI])
            S.op("dve", lambda h: h.tensor_scalar(out=NBR[:, :], in0=BBR[:, :], scalar1=-1.0, scalar2=None, op0=ALU.mult), R=[BBR], W=[NBR])
            CS1 = [sb(su, "CS1_%d" % i, [128, 128], F32) for i in range(2)]
            CS2 = [sb(su, "CS2_%d" % i, [128, 128], F32) for i in range(2)]
            for t in range(8):
                sl_ = slice(t * 128, (t + 1) * 128)
                S.op("pe", lambda h: h.transpose(pS[:, 0:64], BBR[:, sl_], id64), R=[BBR, cst], W=[pS])
                S.op("pe", lambda h: h.transpose(pS[:, 64:128], BBI[:, sl_], id64), R=[BBI, cst], W=[pS])
                S.op("pe", lambda h: h.transpose(pS2[:, 0:64], BBI[:, sl_], id64), R=[BBI, cst], W=[pS2])
                S.op("pe", lambda h: h.transpose(pS2[:, 64:128], NBR[:, sl_], id64), R=[NBR, cst], W=[pS2])
                for g8 in range(8):
                    g = 8 * t + g8
                    S.op("dve", lambda h, g=g, g8=g8: h.tensor_scalar(
                        out=Bpad[:, g * 128:(g + 1) * 128], in0=pS[:, 0:128], scalar1=cst[:, C_RM + g8:C_RM + g8 + 1],
                        scalar2=None, op0=ALU.mult), R=[pS, cst], W=[Bpad])
                    S.op("act", lambda h, g=g, g8=g8: h.activation(
                        out=Bswp[:, g * 128:(g + 1) * 128], in_=pS2[:, 0:128], func=AF.Copy,
                        scale=cst[:, C_RM + g8:C_RM + g8 + 1]), R=[pS2, cst], W=[Bswp])
                c1, c2 = CS1[t % 2], CS2[t % 2]
                S.dma("sp", c1[:, 0:64], c_re[t * 128:(t + 1) * 128, :], W=[c1])
                S.dma("sp", c1[:, 64:128], c_im[t * 128:(t + 1) * 128, :], W=[c1])
                S.dma("sp", c2[:, 0:64], c_im[t * 128:(t + 1) * 128, :], W=[c2])
                S.dma("sp", c2[:, 64:128], c_re[t * 128:(t + 1) * 128, :], W=[c2])
                S.op("pe", lambda h, c1=c1: h.transpose(pS[:, 128:256], c1[:, :], ident), R=[c1, cst], W=[pS])
                S.op("pe", lambda h, c2=c2: h.transpose(pS2[:, 128:256], c2[:, :], ident), R=[c2, cst], W=[pS2])
                for g8 in range(8):
                    g = 8 * t + g8
                    S.op("dve", lambda h, g=g, g8=g8: h.scalar_tensor_tensor(
                        out=CWc[:, g * 128:(g + 1) * 128], in0=pS[:, 128:256], scalar=cst[:, C_SG:C_SG + 1],
                        in1=cst[:, C_CM8 + g8 * 128:C_CM8 + (g8 + 1) * 128], op0=ALU.mult, op1=ALU.mult), R=[pS, cst], W=[CWc])
                    S.op("dve", lambda h, g=g, g8=g8: h.scalar_tensor_tensor(
                        out=CWs[:, g * 128:(g + 1) * 128], in0=pS2[:, 128:256], scalar=-1.0,
                        in1=cst[:, C_CM8 + g8 * 128:C_CM8 + (g8 + 1) * 128], op0=ALU.mult, op1=ALU.mult), R=[pS2, cst], W=[CWs])
            S.barrier()

        swp = cst[:, C_SW:C_SW + 128]
        uTb = [sb(s5, "uTb%d" % i, [128, 8 * 128], BF16) for i in range(2)]
        t1 = [sb(s5, "t1_%d" % i, [128, 512], F32) for i in range(2)]
        t2 = [sb(s5, "t2_%d" % i, [128, 512], F32) for i in range(2)]
        xs = [sb(s5, "xs_%d" % i, [128, 512], F32) for i in range(2)]
        zt = [sb(s5, "zt_%d" % i, [128, 512], F32) for i in range(2)]
        Zc = [sb(s5, "Zc_%d" % i, [128, 512], BF16) for i in range(2)]
        Zs = [sb(s5, "Zs_%d" % i, [128, 512], BF16) for i in range(2)]
        zl = sb(s5, "zl", [128, 64], F32)
        zc = sb(s5, "zc", [128, 64], F32)
        c1t = sb(s5, "c1t", [128, 64], F32)
        c2t = sb(s5, "c2t", [128, 64], F32)
        yf = sb(s5, "yf", [128, 1024], F32)
        g1 = sb(s5, "g1", [128, 1024], F32)
        yab = [sb(s5, "yab%d" % i, [128, 1024], BF16) for i in range(1)]
        S.op("dve", lambda h: h.memset(zc[:, :], 0.0), W=[zc])
        nb5 = NBLK if FULL else 8
        NQ = nb5 * 16
        ubv_of = lambda blk: uTb[blk % 2][:, :].rearrange("p (t n) -> p t n", t=8)

        def load_ub(blk):
            S.dma("sp", ubv_of(blk), uT_scr[:, :, blk * 128:(blk + 1) * 128], R=[uT_scr], W=[uTb[blk % 2]])

        def front(i):
            blk, q = i // 16, i % 16
            ub = uTb[blk % 2]
            ubv = ubv_of(blk)
            if q == 1 and blk + 1 < nb5:
                load_ub(blk + 1)
            A, B = pA[i % 2], pB[i % 2]
            t1_, t2_, xs_ = t1[i % 2], t2[i % 2], xs[i % 2]
            for gi in range(4):
                g = 4 * q + gi
                S.op("pe", lambda h, g=g, gi=gi: h.matmul(
                    A[:, gi * 128:(gi + 1) * 128], Bpad[:, g * 128:(g + 1) * 128], ubv[:, g // 8, :], start=True, stop=True),
                    R=[Bpad, ub], W=[A])
            for gi in range(4):
                g = 4 * q + gi
                S.op("pe", lambda h, g=g, gi=gi: h.matmul(
                    B[:, gi * 128:(gi + 1) * 128], Bswp[:, g * 128:(g + 1) * 128], ubv[:, g // 8, :], start=True, stop=True),
                    R=[Bswp, ub], W=[B])
            S.op("dve", lambda h: h.tensor_tensor(out=t1_[:, :], in0=A[:, :], in1=COS[:, q * 512:(q + 1) * 512], op=ALU.mult),
                 R=[A, COS], W=[t1_])
            S.op("dve", lambda h: h.tensor_tensor(out=t2_[:, :], in0=B[:, :], in1=SIN[:, q * 512:(q + 1) * 512], op=ALU.mult),
                 R=[B, SIN], W=[t2_])
            S.op("pool", lambda h: h.tensor_tensor(out=xs_[:, :], in0=t1_[:, :], in1=t2_[:, :], op=ALU.add), R=[t1_, t2_], W=[xs_])

        def back(i):
            blk, q = i // 16, i % 16
            own = (blk % 4 == 3)
            m = blk // 4
            ub = uTb[blk % 2]
            ubv = ubv_of(blk)
            xs_, z_ = xs[i % 2], zt[i % 2]
            if not own:
                S.op("dve", lambda h: h.tensor_tensor(out=z_[:, :], in0=xs_[:, :], in1=Wt[:, q * 512:(q + 1) * 512], op=ALU.mult),
                     R=[xs_, Wt], W=[z_])
                S.op("dve", lambda h: h.tensor_reduce(out=zl[:, 4 * q:4 * q + 4], in_=z_[:, :].rearrange("p (g j) -> p g j", j=128),
                                                      axis=AX.X, op=ALU.add), R=[z_], W=[zl])
                if q != 15:
                    return
                S.op("dve", lambda h: h.tensor_tensor(out=c1t[:, :], in0=R128[:, :], in1=zc[:, :], op=ALU.mult), R=[R128, zc], W=[c1t])
                S.op("dve", lambda h: h.tensor_tensor(out=zl[:, :], in0=zl[:, :], in1=c1t[:, :], op=ALU.add), R=[zl, c1t], W=[zl])
            for gi in range(4 if own else 0):
                g = 4 * q + gi
                S.op("dve", lambda h, g=g, gi=gi: h.tensor_tensor_scan(
                    out=z_[:, gi * 128:(gi + 1) * 128], data0=RHO[:, g:g + 1].to_broadcast([128, 128]),
                    data1=xs_[:, gi * 128:(gi + 1) * 128], initial=zc[:, g:g + 1], op0=ALU.mult, op1=ALU.add),
                    R=[RHO, xs_, zc], W=[z_])
            if own:
                S.op("act", lambda h: h.copy(
                    out=zl[:, 4 * q:4 * q + 4].unsqueeze(2),
                    in_=z_[:, :].rearrange("p (g j) -> p g j", j=128)[:, :, 127:128]), R=[z_], W=[zl])
            if own:
                Zc_, Zs_ = Zc[q % 2], Zs[q % 2]
                S.op("pool", lambda h: h.tensor_tensor(out=Zc_[:, :], in0=z_[:, :], in1=COS[:, q * 512:(q + 1) * 512], op=ALU.mult),
                     R=[z_, COS], W=[Zc_])
                S.op("dve", lambda h: h.tensor_tensor(out=Zs_[:, :], in0=z_[:, :], in1=SIN[:, q * 512:(q + 1) * 512], op=ALU.mult),
                     R=[z_, SIN], W=[Zs_])
                for gi in range(4):
                    g = 4 * q + gi
                    t = g // 8
                    Y = pY[t // 4]
                    ysl = slice((t % 4) * 128, (t % 4 + 1) * 128)
                    S.op("pe", lambda h, g=g, gi=gi, Y=Y, ysl=ysl: h.matmul(
                        Y[:, ysl], CWc[:, g * 128:(g + 1) * 128], Zc_[:, gi * 128:(gi + 1) * 128], start=(g % 8 == 0), stop=False),
                        R=[CWc, Zc_], W=[Y])
                    S.op("pe", lambda h, g=g, gi=gi, Y=Y, ysl=ysl: h.matmul(
                        Y[:, ysl], CWs[:, g * 128:(g + 1) * 128], Zs_[:, gi * 128:(gi + 1) * 128], start=False, stop=(g % 8 == 7)),
                        R=[CWs, Zs_], W=[Y])
            if q != 15:
                return
            S.op("pe", lambda h: h.matmul(pS[:, 0:64], swp, zl[:, :], start=True, stop=True), R=[cst, zl], W=[pS])
            S.op("dve", lambda h: h.tensor_tensor(out=c1t[:, :], in0=zl[:, :], in1=CT[:, :], op=ALU.mult), R=[zl, CT], W=[c1t])
            S.op("dve", lambda h: h.tensor_tensor(out=c2t[:, :], in0=pS[:, 0:64], in1=STs[:, :], op=ALU.mult), R=[pS, STs], W=[c2t])
            S.op("dve", lambda h: h.tensor_tensor(out=zc[:, :], in0=c1t[:, :], in1=c2t[:, :], op=ALU.add), R=[c1t, c2t], W=[zc])
            if own:
                for t in range(8):
                    Y = pY[t // 4]
                    ysl = slice((t % 4) * 128, (t % 4 + 1) * 128)
                    S.op("dve", lambda h, t=t, Y=Y, ysl=ysl: h.scalar_tensor_tensor(
                        out=yf[:, t * 128:(t + 1) * 128], in0=ubv[:, t, :], scalar=colv[:, 48 + t:49 + t], in1=Y[:, ysl],
                        op0=ALU.mult, op1=ALU.add), R=[ub, colv, Y], W=[yf])
                S.op("pool", lambda h: h.tensor_tensor(out=g1[:, :], in0=yf[:, :], in1=yf[:, :], op=ALU.mult), R=[yf], W=[g1])
                S.op("pool", lambda h: h.tensor_scalar(out=g1[:, :], in0=g1[:, :], scalar1=0.07135481627, scalar2=1.5957691216,
                                                       op0=ALU.mult, op1=ALU.add), R=[g1], W=[g1])
                S.op("pool", lambda h: h.tensor_tensor(out=g1[:, :], in0=g1[:, :], in1=yf[:, :], op=ALU.mult), R=[g1, yf], W=[g1])
                S.op("act", lambda h: h.activation(out=g1[:, :], in_=g1[:, :], func=AF.Sigmoid), R=[g1], W=[g1])
                ya_ = yab[0]
                S.op("pool", lambda h: h.tensor_tensor(out=ya_[:, :], in0=yf[:, :], in1=g1[:, :], op=ALU.mult), R=[yf, g1], W=[ya_])
                S.dma("sp", yaT_scr[:, :, m * 128:(m + 1) * 128], ya_[:, :].rearrange("p (t n) -> p t n", t=8), R=[ya_], W=[yaT_scr])

        load_ub(0)
        for i in range(NQ + 1):
            if i < NQ:
                front(i)
            if i >= 1:
                back(i - 1)
        S.barrier()
    print("n_inst after S5", S.n_inst)

    if stop == 2:
        if dbg:
            with ExitStack() as dd:
                d1 = sb(dd, "d1", [128, 2048], BF16)
                d2 = sb(dd, "d2", [128, 2048], F32)
                S.dma("sp", d1[:, :].rearrange("p (t n) -> p t n", t=8), yaT_scr[:, :, 0:256], R=[yaT_scr], W=[d1])
                S.op("dve", lambda h: h.tensor_copy(out=d2[:, :], in_=d1[:, :]), R=[d1], W=[d2])
                S.dma("sp", dbg_d[:, 0:2048], d2[:, :], R=[d2], W=[dbg_d])
                S.finish([dbg_d])
        es.close()
        return nc


    yatT_scr = dscr("yatT_scr", [128, 8, 2048], BF16)
    h2T_scr = dscr("h2T_scr", [128, NCH, KT, 128], BF16)
    nm2 = NCH if FULL else 2

    with ExitStack() as p2:
        KTs = sb(p2, "KTs", [128, 2 * L], BF16)
        Vs = sb(p2, "Vs", [128, NBLK * 256], BF16)
        kiTs = sb(p2, "kiTs", [128, L], BF16)
        isc = sb(p2, "isc", [128, L], F32)
        MBs = [sb(p2, "MB%d" % i, [128, L], BF16) for i in range(2)]
        junkb = sb(p2, "junkb", [128, L], BF16)
        I4 = sb(p2, "I4", [128, 512], BF16)
        kb = sb(p2, "kb", [128, 512], F32)
        dgv = sb(p2, "dgv", [128, 128], F32)
        QTm = [sb(p2, "QTm%d" % i, [128, 1024], BF16) for i in range(2)]
        qiTm = [sb(p2, "qiTm%d" % i, [128, 1024], BF16) for i in range(2)]
        Tr = [sb(p2, "Tr%d" % i, [128, 512], F32) for i in range(2)]
        PT = [sb(p2, "PT%d" % i, [128, 512], BF16) for i in range(2)]
        Hk = sb(p2, "Hk", [128, 28], F32)
        rmax = sb(p2, "rmax", [128, 1], F32)
        rmin = sb(p2, "rmin", [128, 1], F32)
        w0 = sb(p2, "w0", [128, 1], F32)
        mid = sb(p2, "mid", [128, 1], F32)
        Sacc = sb(p2, "Sacc", [128, 1], F32)
        cntD = sb(p2, "cntD", [128, 1], F32)
        junkd = sb(p2, "junkd", [128, L // 2], BF16)
        aa = sb(p2, "aa", [128, 1], F32)
        lo = sb(p2, "lo", [128, 1], F32)
        rD = sb(p2, "rD", [128, 512], F32)
        yat = [sb(p2, "yat%d" % i, [128, 1024], BF16) for i in range(2)]
        pl_i = [ps(p2, "pl_i%d" % i, [128, 512], F32) for i in range(2)]
        pl_a = [ps(p2, "pl_a%d" % i, [128, 512], F32) for i in range(2)]
        pl = pl_i
        pO = [ps(p2, "pO%d" % i, [128, 512], F32) for i in range(2)]
        pD = [ps(p2, "pD%d" % i, [128, 512], F32) for i in range(2)]
        for hh in range(4):
            S.dma("sp", KTs[:, :].rearrange("p (g n) -> p g n", g=2)[:, :, hh * 2048:(hh + 1) * 2048],
                  KT_scr[:, :, hh * 2048:(hh + 1) * 2048], R=[KT_scr], W=[KTs])
            S.dma("sp", Vs[:, hh * 4096:(hh + 1) * 4096].rearrange("p (b n) -> p b n", n=256),
                  V_scr[:, hh * 16:(hh + 1) * 16, :], R=[V_scr], W=[Vs])
        S.dma("sp", kiTs[:, :], kiT_scr[:, :], R=[kiT_scr], W=[kiTs])
        for r4 in range(4):
            S.op("dve", lambda h, r4=r4: h.tensor_copy(out=I4[:, r4 * 128:(r4 + 1) * 128], in_=identb[:, :]), R=[identb], W=[I4])
        for b4 in range(4):
            S.op("dve", lambda h, b4=b4: h.tensor_scalar(out=dgv[:, :], in0=ident, scalar1=validc[:, b4:b4 + 1], scalar2=None, op0=ALU.mult),
                 R=[cst, validc], W=[dgv])
            S.op("pe", lambda h: h.matmul(pl[0][:, 0:128], ones_f, dgv[:, :], start=True, stop=True), R=[cst, dgv], W=[pl[0]])
            S.op("dve", lambda h, b4=b4: h.tensor_scalar(out=kb[:, b4 * 128:(b4 + 1) * 128], in0=pl[0][:, 0:128], scalar1=-1.0, scalar2=-NEG,
                                                         op0=ALU.add, op1=ALU.mult), R=[pl[0]], W=[kb])
        SCALE = 128.0 ** -0.5
        cnt_i = [0]
        cnt_a = [0]

        def load_q(m):
            qt = QTm[m % 2]
            qit = qiTm[m % 2]
            S.dma("sp", qt[:, :].rearrange("p (a b) -> p a b", b=128), QT_scr[:, m, :, :], R=[QT_scr], W=[qt])
            S.dma("sp", qit[:, :].rearrange("p (a b) -> p a b", b=128), qiT_scr[:, m, :, :], R=[qiT_scr], W=[qit])

        def idx_unit(m, c, hh):
            qit = qiTm[m % 2]
            pr, par = hh // 2, hh % 2
            p_ = pl_i[cnt_i[0] % 2]
            T_ = Tr[cnt_i[0] % 2]
            cnt_i[0] += 1
            S.op("pe", lambda h: h.matmul(
                p_[:, :], qit[par * 64:(par + 1) * 64, pr * 128:(pr + 1) * 128],
                kiTs[par * 64:(par + 1) * 64, c * 512:(c + 1) * 512], start=True, stop=True),
                R=[qit, kiTs], W=[p_])
            S.op("act", lambda h: h.activation(out=T_[:, :], in_=p_[:, :], func=AF.Relu), R=[p_], W=[T_])
            wcol = wi_all[:, 16 * m + hh:16 * m + hh + 1]
            if hh == 0:
                S.op("dve", lambda h: h.tensor_scalar(
                    out=isc[:, c * 512:(c + 1) * 512], in0=T_[:, :], scalar1=wcol, scalar2=None, op0=ALU.mult),
                    R=[T_, wi_all], W=[isc])
            else:
                S.op("dve", lambda h: h.scalar_tensor_tensor(
                    out=isc[:, c * 512:(c + 1) * 512], in0=T_[:, :], scalar=wcol, in1=isc[:, c * 512:(c + 1) * 512],
                    op0=ALU.mult, op1=ALU.add), R=[T_, wi_all, isc], W=[isc])

        def thresh(m):
            n = 512 * (m + 1)
            MB_ = MBs[m % 2]
            S.op("dve", lambda h: h.tensor_reduce(out=rmax[:, :], in_=isc[:, 0:n], axis=AX.X, op=ALU.max), R=[isc], W=[rmax])
            S.op("dve", lambda h: h.tensor_reduce(out=rmin[:, :], in_=isc[:, 0:n], axis=AX.X, op=ALU.min), R=[isc], W=[rmin])
            S.op("dve", lambda h: h.tensor_tensor(out=isc[:, 0:512], in0=isc[:, 0:512], in1=kb[:, :], op=ALU.add), R=[isc, kb], W=[isc])
            S.op("dve", lambda h: h.tensor_tensor(out=isc[:, n - 512:n], in0=isc[:, n - 512:n], in1=cst[:, C_CM:C_CM + 512], op=ALU.add),
                 R=[isc, cst], W=[isc])
            S.op("dve", lambda h: h.tensor_tensor(out=w0[:, :], in0=rmax[:, :], in1=rmin[:, :], op=ALU.subtract), R=[rmax, rmin], W=[w0])
            S.op("dve", lambda h: h.tensor_scalar(out=w0[:, :], in0=w0[:, :], scalar1=1.0001, scalar2=1e-6, op0=ALU.mult, op1=ALU.add),
                 R=[w0], W=[w0])
            S.op("dve", lambda h: h.tensor_scalar(out=Hk[:, :], in0=cst[:, C_H2:C_H2 + 28], scalar1=w0[:, 0:1], scalar2=None, op0=ALU.mult),
                 R=[cst, w0], W=[Hk])
            S.op("dve", lambda h: h.tensor_tensor(out=mid[:, :], in0=rmin[:, :], in1=Hk[:, 0:1], op=ALU.add), R=[rmin, Hk], W=[mid])
            nA = n // 2
            thrA = float(nA - 2 * TOPK)
            for k in range(NBIS):
                S.op("act", lambda h: h.activation(out=junkb[:, 0:nA], in_=isc[:, 0:nA], func=AF.Sign, bias=mid[:, 0:1], scale=-1.0,
                                                   accum_out=Sacc[:, 0:1]), R=[isc, mid], W=[junkb, Sacc])
                S.op("dve", lambda h: h.tensor_scalar(out=junkd[:, 0:n - nA], in0=isc[:, nA:n], scalar1=mid[:, 0:1], scalar2=0.0,
                                                      op0=ALU.is_ge, op1=ALU.add, accum_out=cntD[:, 0:1]), R=[isc, mid], W=[junkd, cntD])
                S.op("dve", lambda h: h.scalar_tensor_tensor(out=aa[:, :], in0=cntD[:, :], scalar=-2.0, in1=Sacc[:, :],
                                                             op0=ALU.mult, op1=ALU.add), R=[cntD, Sacc], W=[aa])
                S.op("dve", lambda h, k=k: h.tensor_scalar(out=aa[:, :], in0=aa[:, :], scalar1=thrA, scalar2=Hk[:, k:k + 1],
                                                           op0=ALU.is_le, op1=ALU.mult), R=[aa, Hk], W=[aa])
                S.op("dve", lambda h, k=k: h.scalar_tensor_tensor(out=mid[:, :], in0=aa[:, :], scalar=Hk[:, k + 1:k + 2], in1=mid[:, :],
                                                                  op0=ALU.subtract, op1=ALU.add), R=[aa, Hk, mid], W=[mid])
            S.op("dve", lambda h: h.tensor_tensor(out=lo[:, :], in0=mid[:, :], in1=Hk[:, NBIS:NBIS + 1], op=ALU.subtract),
                 R=[mid, Hk], W=[lo])
            S.op("dve", lambda h: h.tensor_scalar(out=MB_[:, 0:n], in0=isc[:, 0:n], scalar1=lo[:, 0:1], scalar2=-30000.0,
                                                  op0=ALU.is_lt, op1=ALU.mult), R=[isc, lo], W=[MB_])

        def att_front(m, kbk, g):
            qt = QTm[m % 2]
            MB_ = MBs[m % 2]
            p_ = pl_a[cnt_a[0] % 2]
            P_ = PT[cnt_a[0] % 2]
            cnt_a[0] += 1
            S.op("pe", lambda h: h.matmul(
                p_[:, :], KTs[:, g * L + kbk * 128:g * L + (kbk + 1) * 128], qt[:, g * 512:(g + 1) * 512],
                start=True, stop=False), R=[KTs, qt], W=[p_])
            S.op("pe", lambda h: h.matmul(
                p_[:, :], MB_[:, kbk * 128:(kbk + 1) * 128], I4[:, :], start=False, stop=True), R=[MB_, I4], W=[p_])
            S.op("act", lambda h: h.activation(out=P_[:, :], in_=p_[:, :], func=AF.Exp, scale=SCALE), R=[p_], W=[P_])
            return P_

        def att_back(m, kbk, g, P_, nkb):
            S.op("pe", lambda h: h.matmul(
                pO[g][:, :], Vs[:, kbk * 256 + g * 128:kbk * 256 + (g + 1) * 128], P_[:, :],
                start=(kbk == 0), stop=(kbk == nkb - 1)), R=[Vs, P_], W=[pO[g]])
            S.op("pe", lambda h: h.matmul(
                pD[g][:, :], onesb[:, :], P_[:, :], start=(kbk == 0), stop=(kbk == nkb - 1)), R=[onesb, P_], W=[pD[g]])

        def att_final(m):
            ya_ = yat[m % 2]
            for g in range(2):
                S.op("dve", lambda h, g=g: h.reciprocal(out=rD[:, :], in_=pD[g][:, :]), R=[pD[g]], W=[rD])
                S.op("dve", lambda h, g=g: h.tensor_tensor(out=ya_[:, g * 512:(g + 1) * 512], in0=pO[g][:, :], in1=rD[:, :], op=ALU.mult),
                     R=[pO[g], rD], W=[ya_])
            S.dma("sp", yatT_scr[:, :, m * 128:(m + 1) * 128], ya_[:, :].rearrange("p (a b) -> p a b", b=128), R=[ya_], W=[yatT_scr])

        load_q(0)
        for hh in range(16):
            idx_unit(0, 0, hh)
        thresh(0)
        for m in range(nm2):
            nkb = 4 * (m + 1)
            U = [(kbk, g) for kbk in range(nkb) for g in range(2)]
            I_ = []
            if m + 1 < nm2:
                load_q(m + 1)
                I_ = [(c, hh) for c in range(m + 2) for hh in range(16)]
            ii = 0
            prev = None
            for u in range(len(U) + 1):
                cur = None
                if u < len(U):
                    cur = (U[u], att_front(m, U[u][0], U[u][1]))
                if prev is not None:
                    (kbk_, g_), P_ = prev
                    att_back(m, kbk_, g_, P_, nkb)
                prev = cur
                target = (u + 1) * len(I_) // (len(U) + 1)
                while ii < target:
                    idx_unit(m + 1, I_[ii][0], I_[ii][1])
                    ii += 1
            while ii < len(I_):
                idx_unit(m + 1, I_[ii][0], I_[ii][1])
                ii += 1
            att_final(m)
            if m + 1 < nm2:
                thresh(m + 1)
        S.barrier()
    print("n_inst after P2", S.n_inst)

    if stop == 3:
        if dbg:
            with ExitStack() as dd:
                d1 = sb(dd, "d1", [128, 2048], BF16)
                d2 = sb(dd, "d2", [128, 2048], F32)
                S.dma("sp", d1[:, :].rearrange("p (t n) -> p t n", t=8), yatT_scr[:, :, 0:256], R=[yatT_scr], W=[d1])
                S.op("dve", lambda h: h.tensor_copy(out=d2[:, :], in_=d1[:, :]), R=[d1], W=[d2])
                S.dma("sp", dbg_d[:, 0:2048], d2[:, :], R=[d2], W=[dbg_d])
                S.finish([dbg_d])
        es.close()
        return nc

    with ExitStack() as p3:
        wg = sb(p3, "wg", [128, 8 * 1024], BF16)
        wo = sb(p3, "wo", [128, KT * D], BF16)
        w_glu_v = w_glu.t.rearrange("(kt p) n -> p kt n", p=128)
        w_out_v = w_out.t.rearrange("(kt p) n -> p kt n", p=128)
        S.dma("pool", wg[:, :].rearrange("p (kt n) -> p kt n", kt=8), w_glu_v, W=[wg])
        wov = wo[:, :].rearrange("p (kt n) -> p kt n", kt=KT)
        for q4 in range(4):
            S.dma("pool", wov[:, 4 * q4:4 * q4 + 4, :], w_out_v[:, 4 * q4:4 * q4 + 4, :], W=[wo])
        wgv = wg[:, :].rearrange("p (kt n) -> p kt n", kt=8)
        pG = [ps(p3, "pG%d" % i, [128, 512], F32) for i in range(2)]
        pR = ps(p3, "pR", [128, 512], F32)
        pW = [ps(p3, "pW%d" % i, [128, 512], F32) for i in range(4)]
        ptr3 = ps(p3, "ptr3", [128, 1024], BF16)
        gtA_bc = bcast_row(p3, modT, modT[:, 32:48], "gtA_bc", pG)
        gscM_bc = bcast_row(p3, gscM, gscM[:, :], "gscM_bc", pG)
        shM_bc = bcast_row(p3, modT, modT[:, 48:64], "shM_bc", pG)
        yam = [sb(p3, "yam%d" % i, [128, 1024], BF16) for i in range(2)]
        yatm = [sb(p3, "yatm%d" % i, [128, 1024], BF16) for i in range(2)]
        sg = sb(p3, "sg", [128, 1024], F32)
        ysf = sb(p3, "ysf", [128, 1024], F32)
        sqf = sb(p3, "sqf", [128, 1024], F32)
        rs_s = sb(p3, "rs_s", [128, 128], F32)
        rs_a = sb(p3, "rs_a", [128, 128], F32)
        mixT = [sb(p3, "mixT%d" % i, [128, KT * 128], BF16) for i in range(2)]
        xo = [sb(p3, "xo%d" % i, [128, D], F32) for i in range(2)]
        x1t = [sb(p3, "x1t%d" % i, [128, D], F32) for i in range(2)]
        tmp3 = sb(p3, "tmp3", [128, D], F32)
        junk3 = sb(p3, "junk3", [128, D], BF16)
        h2b = sb(p3, "h2b", [128, D], BF16)
        h2Ts = [sb(p3, "h2Ts%d" % i, [128, KT * 128], BF16) for i in range(2)]
        ss1 = sb(p3, "ss1", [128, NCH], F32)
        ms1 = sb(p3, "ms1", [128, NCH], F32)
        rs1 = sb(p3, "rs1", [128, NCH], F32)
        def p3A(m):
            ya_ = yam[m % 2]
            yt_ = yatm[m % 2]
            mx = mixT[m % 2]
            S.dma("sp", ya_[:, :].rearrange("p (t n) -> p t n", t=8), yaT_scr[:, :, m * 128:(m + 1) * 128], R=[yaT_scr], W=[ya_])
            S.dma("sp", yt_[:, :].rearrange("p (t n) -> p t n", t=8), yatT_scr[:, :, m * 128:(m + 1) * 128], R=[yatT_scr], W=[yt_])
            x_ = xo[m % 2]
            S.dma("sp", x_[:, :], xv[(4 * m + 3) * 128:(4 * m + 4) * 128, :], W=[x_])
            for t in range(8):
                G = pG[t // 4]
                gsl = slice((t % 4) * 128, (t % 4 + 1) * 128)
                for k in range(8):
                    S.op("pe", lambda h, t=t, k=k, G=G, gsl=gsl: h.matmul(
                        G[:, gsl], wgv[:, k, t * 128:(t + 1) * 128], ya_[:, k * 128:(k + 1) * 128],
                        start=(k == 0), stop=(k == 7)), R=[wg, ya_], W=[G])
                S.op("act", lambda h, t=t, G=G, gsl=gsl: h.activation(
                    out=sg[:, t * 128:(t + 1) * 128], in_=G[:, gsl], func=AF.Sigmoid, bias=colv[:, 56 + t:57 + t], scale=1.0),
                    R=[G, colv], W=[sg])
            S.op("dve", lambda h: h.tensor_tensor(out=ysf[:, :], in0=ya_[:, :], in1=sg[:, :], op=ALU.mult), R=[ya_, sg], W=[ysf])
            S.op("dve", lambda h: h.tensor_tensor(out=sqf[:, :], in0=ysf[:, :], in1=ysf[:, :], op=ALU.mult), R=[ysf], W=[sqf])
            for t in range(8):
                S.op("pe", lambda h, t=t: h.matmul(pR[:, 0:128], ones_f, sqf[:, t * 128:(t + 1) * 128], start=(t == 0), stop=(t == 7)),
                     R=[cst, sqf], W=[pR])
            S.op("dve", lambda h: h.tensor_scalar(out=rs_s[:, :], in0=pR[:, 0:128], scalar1=1.0 / 1024, scalar2=EPS, op0=ALU.mult, op1=ALU.add),
                 R=[pR], W=[rs_s])
            S.op("pool", lambda h: h.tensor_tensor(out=rs_s[:, :], in0=rs_s[:, :], in1=cst[:, C_NHALF:C_NHALF + 1].to_broadcast([128, 128]),
                                                   op=ALU.pow), R=[rs_s, cst], W=[rs_s])
            for t in range(8):
                S.op("dve", lambda h, t=t: h.scalar_tensor_tensor(
                    out=mx[:, t * 128:(t + 1) * 128], in0=ysf[:, t * 128:(t + 1) * 128], scalar=colv[:, 64 + t:65 + t], in1=rs_s[:, :],
                    op0=ALU.mult, op1=ALU.mult), R=[ysf, colv, rs_s], W=[mx])
            S.op("dve", lambda h: h.tensor_tensor(out=sqf[:, :], in0=yt_[:, :], in1=yt_[:, :], op=ALU.mult), R=[yt_], W=[sqf])
            for t in range(8):
                S.op("pe", lambda h, t=t: h.matmul(pR[:, 128:256], ones_f, sqf[:, t * 128:(t + 1) * 128], start=(t == 0), stop=(t == 7)),
                     R=[cst, sqf], W=[pR])
            S.op("dve", lambda h: h.tensor_scalar(out=rs_a[:, :], in0=pR[:, 128:256], scalar1=1.0 / 1024, scalar2=EPS, op0=ALU.mult, op1=ALU.add),
                 R=[pR], W=[rs_a])
            S.op("pool", lambda h: h.tensor_tensor(out=rs_a[:, :], in0=rs_a[:, :], in1=cst[:, C_NHALF:C_NHALF + 1].to_broadcast([128, 128]),
                                                   op=ALU.pow), R=[rs_a, cst], W=[rs_a])
            for t in range(8):
                S.op("dve", lambda h, t=t: h.scalar_tensor_tensor(
                    out=mx[:, (8 + t) * 128:(9 + t) * 128], in0=yt_[:, t * 128:(t + 1) * 128], scalar=colv[:, 72 + t:73 + t], in1=rs_a[:, :],
                    op0=ALU.mult, op1=ALU.mult), R=[yt_, colv, rs_a], W=[mx])

        def p3B(m):
            mx = mixT[m % 2]
            x_ = xo[m % 2]
            for dch in range(4):
                for kt in range(KT):
                    S.op("pe", lambda h, dch=dch, kt=kt: h.matmul(
                        pW[dch][:, :], mx[:, kt * 128:(kt + 1) * 128], wov[:, kt, dch * 512:(dch + 1) * 512],
                        start=(kt == 0), stop=(kt == KT - 1)), R=[mx, wo], W=[pW[dch]])
            x1_ = x1t[m % 2]
            for dch in range(4):
                dsl = slice(dch * 512, (dch + 1) * 512)
                S.op("dve", lambda h, dch=dch, dsl=dsl: h.tensor_tensor(out=tmp3[:, dsl], in0=pW[dch][:, :], in1=gtA_bc[:, dsl], op=ALU.mult),
                     R=[pW[dch], gtA_bc], W=[tmp3])
            S.op("dve", lambda h: h.tensor_tensor(out=x1_[:, :], in0=tmp3[:, :], in1=x_[:, :], op=ALU.add), R=[tmp3, x_], W=[x1_])
            S.dma("pool", x1_scr[:, m, :], x1_[:, :], R=[x1_], W=[x1_scr])
            S.op("act", lambda h: h.activation(out=junk3[:, :], in_=x1_[:, :], func=AF.Square, accum_out=ss1[:, m:m + 1]),
                 R=[x1_], W=[junk3, ss1])
            S.op("dve", lambda h: h.tensor_scalar(out=ms1[:, m:m + 1], in0=ss1[:, m:m + 1], scalar1=1.0 / D, scalar2=EPS,
                                                  op0=ALU.mult, op1=ALU.add), R=[ss1], W=[ms1])
            S.op("pool", lambda h: h.tensor_tensor(out=rs1[:, m:m + 1], in0=ms1[:, m:m + 1], in1=cst[:, C_NHALF:C_NHALF + 1], op=ALU.pow),
                 R=[ms1, cst], W=[rs1])
            S.op("dve", lambda h: h.scalar_tensor_tensor(out=tmp3[:, :], in0=x1_[:, :], scalar=rs1[:, m:m + 1], in1=gscM_bc[:, :],
                                                         op0=ALU.mult, op1=ALU.mult), R=[x1_, rs1, gscM_bc], W=[tmp3])
            S.op("dve", lambda h: h.tensor_tensor(out=h2b[:, :], in0=tmp3[:, :], in1=shM_bc[:, :], op=ALU.add), R=[tmp3, shM_bc], W=[h2b])
            hs = h2Ts[m % 2]
            for half in range(2):
                for k8 in range(8):
                    kt = half * 8 + k8
                    S.op("pe", lambda h, kt=kt, k8=k8: h.transpose(ptr3[:, k8 * 128:(k8 + 1) * 128], h2b[:, kt * 128:(kt + 1) * 128], identb[:, :]),
                         R=[h2b, identb], W=[ptr3])
                S.op("act", lambda h, half=half: h.copy(out=hs[:, half * 1024:(half + 1) * 1024], in_=ptr3[:, :]), R=[ptr3], W=[hs])
            S.dma("pool", h2T_scr[:, m, :, :], hs[:, :].rearrange("p (kt n) -> p kt n", kt=KT), R=[hs], W=[h2T_scr])

        p3A(0)
        for m in range(nm2):
            if m + 1 < nm2:
                p3A(m + 1)
            p3B(m)
        S.barrier()
    print("n_inst after P3", S.n_inst)

    with ExitStack() as p4:
        pH = [ps(p4, "pH%d" % i, [128, 512], F32) for i in range(3)]
        pOm = [ps(p4, "pOm%d" % i, [128, 512], F32) for i in range(4)]
        gtM_bc = bcast_row(p4, modT, modT[:, 80:96], "gtM_bc", pH)
        h2p = sb(p4, "h2p", [128, 8 * KT * 128], BF16)
        acc = sb(p4, "acc", [128, 8 * D], F32)
        wmi_s = [sb(p4, "wmi_s%d" % i, [128, KT * 512], BF16) for i in range(2)]
        wmo_s = [sb(p4, "wmo_s%d" % i, [128, 4 * D], BF16) for i in range(2)]
        hid = sb(p4, "hid", [128, 4 * 1024], BF16)
        rl = [sb(p4, "rl%d" % i, [128, 512], BF16) for i in range(2)]
        x1m = [sb(p4, "x1m%d" % i, [128, D], F32) for i in range(2)]
        accb = [Buf() for _ in range(8)]
        w_mi_v = w_mi.t.rearrange("(kt p) n -> p kt n", p=128)
        w_mo_v = w_mo.t.rearrange("(f p) n -> p f n", p=128)
        h2v = h2p[:, :].rearrange("p (b kt n) -> p b kt n", b=8, kt=KT)
        npass = 2 if FULL else 1
        nfg = 16
        ih = 0
        io = 0

        def load_w(g_):
            fg_ = g_ % nfg
            wi2 = wmi_s[g_ % 2]
            wo2 = wmo_s[g_ % 2]
            wiv2 = wi2[:, :].rearrange("p (kt n) -> p kt n", kt=KT)
            wov2 = wo2[:, :].rearrange("p (f n) -> p f n", f=4)
            for q2 in range(2):
                S.dma("pool", wiv2[:, 8 * q2:8 * q2 + 8, :], w_mi_v[:, 8 * q2:8 * q2 + 8, fg_ * 512:(fg_ + 1) * 512], W=[wi2])
            S.dma("pool", wov2, w_mo_v[:, 4 * fg_:4 * fg_ + 4, :], W=[wo2])

        for pp in range(npass):
            nbp = 8 if FULL else 2
            S.dma("sp", h2v[:, 0:nbp, :, :], h2T_scr[:, 8 * pp:8 * pp + nbp, :, :], R=[h2T_scr], W=[h2p])
            for fg in range(nfg):
                wi_ = wmi_s[fg % 2]
                wo_ = wmo_s[fg % 2]
                wiv = wi_[:, :].rearrange("p (kt n) -> p kt n", kt=KT)
                wo_v = wo_[:, :].rearrange("p (f n) -> p f n", f=4)
                if pp == 0 and fg == 0:
                    load_w(0)
                if pp * nfg + fg + 1 < npass * nfg:
                    load_w(pp * nfg + fg + 1)
                for f in range(4):
                    for c in range(nbp // 4 if nbp >= 4 else 1):
                        p_ = pH[ih % 3]
                        r_ = rl[ih % 2]
                        ih += 1
                        nb_ = min(4, nbp)
                        for kt in range(KT):
                            S.op("pe", lambda h, p_=p_, kt=kt, f=f, c=c, nb_=nb_: h.matmul(
                                p_[:, 0:nb_ * 128], wiv[:, kt, f * 128:(f + 1) * 128], h2v[:, 4 * c:4 * c + nb_, kt, :],
                                start=(kt == 0), stop=(kt == KT - 1)), R=[wi_, h2p], W=[p_])
                        S.op("act", lambda h, p_=p_, r_=r_, nb_=nb_: h.activation(out=r_[:, 0:nb_ * 128], in_=p_[:, 0:nb_ * 128], func=AF.Relu),
                             R=[p_], W=[r_])
                        S.op("pool", lambda h, r_=r_, f=f, c=c, nb_=nb_: h.tensor_tensor(
                            out=hid[:, f * 1024 + c * 512:f * 1024 + c * 512 + nb_ * 128], in0=r_[:, 0:nb_ * 128], in1=r_[:, 0:nb_ * 128],
                            op=ALU.mult), R=[r_], W=[hid])
                for blk in range(nbp):
                    for dch in range(4):
                        p_ = pOm[io % 4]
                        io += 1
                        for f in range(4):
                            S.op("pe", lambda h, p_=p_, f=f, blk=blk, dch=dch: h.matmul(
                                p_[:, :], hid[:, f * 1024 + blk * 128:f * 1024 + (blk + 1) * 128], wo_v[:, f, dch * 512:(dch + 1) * 512],
                                start=(f == 0), stop=(f == 3)), R=[hid, wo_], W=[p_])
                        asl = slice(blk * D + dch * 512, blk * D + (dch + 1) * 512)
                        if fg == 0:
                            S.op("dve", lambda h, p_=p_, asl=asl: h.tensor_copy(out=acc[:, asl], in_=p_[:, :]), R=[p_], W=[acc])
                        else:
                            S.op("dve", lambda h, p_=p_, asl=asl: h.tensor_tensor(out=acc[:, asl], in0=p_[:, :], in1=acc[:, asl], op=ALU.add),
                                 R=[p_, acc], W=[acc])
            for blk in range(nbp):
                mb = 8 * pp + blk
                xm = x1m[blk % 2]
                asl = slice(blk * D, (blk + 1) * D)
                S.dma("sp", xm[:, :], x1_scr[:, mb, :], R=[x1_scr], W=[xm])
                S.op("dve", lambda h, asl=asl: h.tensor_tensor(out=acc[:, asl], in0=acc[:, asl], in1=gtM_bc[:, :], op=ALU.mult),
                     R=[acc, gtM_bc], W=[accb[blk]])
                S.op("dve", lambda h, asl=asl, xm=xm: h.tensor_tensor(out=acc[:, asl], in0=acc[:, asl], in1=xm[:, :], op=ALU.add),
                     R=[accb[blk], xm], W=[accb[blk]])
                S.dma("pool", out_d[mb * 128:(mb + 1) * 128, :], acc[:, asl], R=[accb[blk], acc], W=[out_d])
        S.barrier()
    print("n_inst total", S.n_inst)
    S.finish([out_d])
    es.close()
    return nc


def _prep_core_inputs(inputs, core):
    b, j = core // 4, core % 4
    pad = 128 * (3 - j)
    x = np.asarray(inputs["x"], np.float32)
    xv = np.zeros((L, D), np.float32)
    xv[pad:] = x[b, :L - pad]
    valid = np.zeros((L,), np.float32)
    valid[pad:] = 1.0
    pos = np.zeros((L,), np.int32)
    pos[pad:] = np.asarray(inputs["positions"])[b, :L - pad]
    vecs = np.concatenate([
        np.asarray(inputs["c"], np.float32)[b].reshape(16, 128),
        np.asarray(inputs["g_norm_mix"], np.float32).reshape(16, 128),
        np.asarray(inputs["g_norm_mlp"], np.float32).reshape(16, 128),
        np.asarray(inputs["d_skip"], np.float32).reshape(8, 128),
        np.asarray(inputs["b_glu"], np.float32).reshape(8, 128),
        np.asarray(inputs["g_out_ssm"], np.float32).reshape(8, 128),
        np.asarray(inputs["g_out_attn"], np.float32).reshape(8, 128),
        np.asarray(inputs["g_q"], np.float32).reshape(1, 128),
        np.asarray(inputs["g_k"], np.float32).reshape(1, 128),
    ], axis=0)
    f = lambda k, shp: np.ascontiguousarray(np.asarray(inputs[k], np.float32).reshape(shp))
    return {
        "xv": xv,
        "validT": np.ascontiguousarray(valid.reshape(NBLK, 128).T),
        "posT": np.ascontiguousarray(pos.reshape(NBLK, 128).T),
        "vecs": np.ascontiguousarray(vecs),
        "w_ada": f("w_ada", (D, 6 * D)),
        "b_ada": f("b_ada", (96, 128)),
        "w_in": f("w_in", (D, DIN)),
        "lam_re": f("lam_re", (64, 64)),
        "lam_im": f("lam_im", (64, 64)),
        "log_dt": f("log_dt", (1, 64)),
        "b_re": f("b_re", (64, 64, 16)),
        "b_im": f("b_im", (64, 64, 16)),
        "c_re": f("c_re", (1024, 64)),
        "c_im": f("c_im", (1024, 64)),
        "w_glu": f("w_glu", (1024, 1024)),
        "w_out": f("w_out", (D, D)),
        "w_mlp_in": f("w_mlp_in", (D, 4 * D)),
        "w_mlp_out": f("w_mlp_out", (4 * D, D)),
    }


def kernel(**inputs):
    nc = build()
    consts = make_consts()
    shared = None
    in_maps = []
    for core in range(8):
        m = _prep_core_inputs(inputs, core)
        if shared is None:
            shared = {k: v for k, v in m.items() if k not in ("xv", "validT", "posT", "vecs")}
        else:
            for k in shared:
                m[k] = shared[k]
        m["consts"] = consts
        in_maps.append(m)
    res = run_bass_kernel_spmd(nc, in_maps, core_ids=list(range(8)))
    out = np.zeros((2, L, D), np.float32)
    for core in range(8):
        b, j = core // 4, core % 4
        o = np.asarray(res.results[core]["out"]).reshape(NCH, 128, D)
        for m_ in range(NCH):
            g0 = 128 * (4 * m_ + j)
            out[b, g0:g0 + 128] = o[m_]
    return out
```

```python
import numpy as np
from contextlib import ExitStack
import concourse.bass as bass
import concourse.mybir as mybir
from concourse.bass_utils import run_bass_kernel_spmd

F32 = mybir.dt.float32
BF16 = mybir.dt.bfloat16
I32 = mybir.dt.int32
AF = mybir.ActivationFunctionType
ALU = mybir.AluOpType
AX = mybir.AxisListType

D = 2048
L = 8192
NBLK = 64
NCH = 16
KT = 16
DIN = 3664
U0, Q0, K0, V0, QI0, KI0, WI0 = 0, 1024, 2048, 2304, 2560, 3584, 3648
EPS = 1e-6
TOPK = 256
NBIS = 18
NEG = -1.0e30
TWO_PI = 6.283185307179586
PI = 3.141592653589793

C_ID = 0
C_RM = 128
C_SG = 136
C_IOTA = 137
C_INVH = 265
C_INVI = 281
C_H2 = 289
C_CM = 317
C_SW = 829
C_CM8 = 957
C_ONE = 1981
C_NHALF = 2109
CW = 2110


def make_consts():
    c = np.zeros((128, CW), np.float32)
    c[:, C_ID:C_ID + 128] = np.eye(128, dtype=np.float32)
    p = np.arange(128)
    for g in range(8):
        c[:, C_RM + g] = (p // 16 == g)
    c[:, C_SG] = np.where(p < 64, 1.0, -1.0)
    c[:, C_IOTA:C_IOTA + 128] = np.arange(128, dtype=np.float32)[None, :]
    c[:, C_INVH:C_INVH + 16] = (500000.0 ** (-np.arange(16, dtype=np.float32) / 16)).astype(np.float32)[None, :]
    c[:, C_INVI:C_INVI + 8] = (500000.0 ** (-np.arange(8, dtype=np.float32) / 8)).astype(np.float32)[None, :]
    c[:, C_H2:C_H2 + 28] = (2.0 ** -(np.arange(28, dtype=np.float64) + 1)).astype(np.float32)[None, :]
    kk = np.arange(512)[None, :]
    qq = np.arange(128)[:, None]
    c[:, C_CM:C_CM + 512] = np.where(kk <= 384 + qq, 0.0, NEG)
    sw = np.zeros((128, 128), np.float32)
    for i in range(128):
        sw[i, (i + 64) % 128] = 1.0
    c[:, C_SW:C_SW + 128] = sw
    col = np.arange(128)
    for g in range(8):
        c[:, C_CM8 + g * 128:C_CM8 + (g + 1) * 128] = (col // 16 == g)[None, :]
    c[:, C_ONE:C_ONE + 128] = 1.0
    c[:, C_NHALF] = -0.5
    return c


class Buf:
    __slots__ = ("w", "r")

    def __init__(self):
        self.w = None
        self.r = {}


class Tile:
    def __init__(self, t, nsub=0):
        self.t = t
        self.b = Buf()
        self.sub = [Buf() for _ in range(nsub)]

    def __getitem__(self, k):
        return self.t[k]


class Sch:
    NSLOT = 12

    def __init__(self, nc, es):
        self.nc = nc
        self.E = {}
        self.sem = {}
        for name, h in (("pe", nc.tensor), ("act", nc.scalar), ("dve", nc.vector),
                        ("pool", nc.gpsimd), ("sp", nc.sync)):
            sem = es.enter_context(nc.semaphore("sem_" + name))
            self.E[name] = dict(h=h, sem=sem, cnt=0, waited={})
            self.sem[name] = sem
        self.slots = []
        for i in range(self.NSLOT):
            sem = es.enter_context(nc.semaphore("dq%d" % i))
            self.slots.append(dict(sem=sem, val=0, key="dq%d" % i))
            self.sem["dq%d" % i] = sem
        self.slot_i = 0
        self.n_inst = 0

    def _deps(self, eng, R, W):
        need = {}

        def req(d, raw):
            if d is None:
                return
            k, v = d
            if k == eng:
                if eng == "pe" or not raw:
                    return
            if need.get(k, 0) < v:
                need[k] = v
        for b in R:
            req(b.w, True)
        for b in W:
            req(b.w, False)
            for k, v in b.r.items():
                req((k, v), False)
        return need

    def _wait(self, eng, need):
        e = self.E[eng]
        for k, v in need.items():
            if e["waited"].get(k, 0) >= v:
                continue
            e["h"].wait_ge(self.sem[k], v)
            e["waited"][k] = v
            self.n_inst += 1

    def op(self, eng, fn, R=(), W=()):
        R = [x.b if isinstance(x, Tile) else x for x in R]
        W = [x.b if isinstance(x, Tile) else x for x in W]
        self._wait(eng, self._deps(eng, R, W))
        e = self.E[eng]
        inst = fn(e["h"])
        e["cnt"] += 1
        inst.then_inc(e["sem"], 1)
        self.n_inst += 1
        for b in R:
            b.r[eng] = e["cnt"]
        for b in W:
            b.w = (eng, e["cnt"])
            b.r = {}

    def dma(self, q, out, in_, R=(), W=()):
        R = [x.b if isinstance(x, Tile) else x for x in R]
        W = [x.b if isinstance(x, Tile) else x for x in W]
        need = self._deps(None, R, W)
        sl = self.slots[self.slot_i]
        self.slot_i = (self.slot_i + 1) % self.NSLOT
        if sl["val"] > 0:
            if need.get(sl["key"], 0) < sl["val"]:
                need[sl["key"]] = sl["val"]
        self._wait(q, need)
        e = self.E[q]
        inst = e["h"].dma_start(out=out, in_=in_)
        sl["val"] += 16
        inst.then_inc(sl["sem"], 16)
        self.n_inst += 1
        for b in R:
            b.r[sl["key"]] = sl["val"]
        for b in W:
            b.w = (sl["key"], sl["val"])
            b.r = {}

    def barrier(self):
        for name in ("pe", "act", "dve", "pool", "sp"):
            need = {}
            for n2, e2 in self.E.items():
                if n2 != name and e2["cnt"] > 0:
                    need[n2] = e2["cnt"]
            for sl in self.slots:
                if sl["val"] > 0:
                    need[sl["key"]] = sl["val"]
            self._wait(name, need)

    def finish(self, bufs):
        need = {}
        for b in bufs:
            b = b.b if isinstance(b, Tile) else b
            if b.w is not None:
                k, v = b.w
                need[k] = max(need.get(k, 0), v)
        self._wait("sp", need)


def build(stop=99, dbg=False, full=False):
    nc = bass.Bass("TRN2", target_bir_lowering=False)
    es = ExitStack()
    S = Sch(nc, es)
    FULL = full or stop >= 99

    def din(name, shape, dt=F32):
        return Tile(nc.dram_tensor(name, list(shape), dt, kind="ExternalInput").ap())

    def dscr(name, shape, dt):
        return Tile(nc.dram_tensor(name, list(shape), dt, kind="Internal").ap())

    def sb(st, name, shape, dt, nsub=0):
        return Tile(st.enter_context(nc.sbuf_tensor(name, list(shape), dt)), nsub)

    def ps(st, name, shape, dt=F32, nsub=0):
        return Tile(st.enter_context(nc.psum_tensor(name, list(shape), dt)), nsub)

    xv = din("xv", [L, D])
    validT = din("validT", [128, NBLK])
    posT = din("posT", [128, NBLK], I32)
    vecs = din("vecs", [82, 128])
    consts = din("consts", [128, CW])
    w_ada = din("w_ada", [D, 6 * D])
    b_ada = din("b_ada", [96, 128])
    w_in = din("w_in", [D, DIN])
    lam_re = din("lam_re", [64, 64])
    lam_im = din("lam_im", [64, 64])
    log_dt = din("log_dt", [1, 64])
    b_re = din("b_re", [64, 64, 16])
    b_im = din("b_im", [64, 64, 16])
    c_re = din("c_re", [1024, 64])
    c_im = din("c_im", [1024, 64])
    w_glu = din("w_glu", [1024, 1024])
    w_out = din("w_out", [D, D])
    w_mi = din("w_mlp_in", [D, 4 * D])
    w_mo = din("w_mlp_out", [4 * D, D])
    out_d = Tile(nc.dram_tensor("out", [2048, D], F32, kind="ExternalOutput").ap())
    dbg_d = None
    if dbg:
        dbg_d = Tile(nc.dram_tensor("dbg", [128, 8192], F32, kind="ExternalOutput").ap())

    uT_scr = dscr("uT_scr", [128, 8, L], BF16)
    KT_scr = dscr("KT_scr", [128, 2, L], BF16)
    V_scr = dscr("V_scr", [128, NBLK, 256], BF16)
    kiT_scr = dscr("kiT_scr", [128, L], BF16)
    hTo_scr = dscr("hTo_scr", [128, NCH, KT, 128], BF16)
    QT_scr = dscr("QT_scr", [128, NCH, 8, 128], BF16)
    qiT_scr = dscr("qiT_scr", [128, NCH, 8, 128], BF16)
    x1_scr = dscr("x1_scr", [128, NCH, D], F32)

    top = es
    cst = sb(top, "cst", [128, CW], F32)
    identb = sb(top, "identb", [128, 128], BF16)
    onesb = sb(top, "onesb", [128, 128], BF16)
    colv = sb(top, "colv", [128, 82], F32)
    modT = sb(top, "modT", [128, 96], F32)
    validc = sb(top, "validc", [128, NBLK], F32)
    posf = sb(top, "posf", [128, NBLK], F32)
    wi_all = sb(top, "wi_all", [128, NCH * 16], F32)
    gscA = sb(top, "gscA", [128, 16], F32)
    gscM = sb(top, "gscM", [128, 16], F32)

    ident = cst[:, C_ID:C_ID + 128]
    ones_f = cst[:, C_ONE:C_ONE + 128]

    S.dma("sp", cst[:, :], consts[:, :], W=[cst])
    S.dma("sp", validc[:, :], validT[:, :], W=[validc])
    S.op("dve", lambda h: h.tensor_copy(out=identb[:, :], in_=ident), R=[cst], W=[identb])
    S.op("dve", lambda h: h.tensor_copy(out=onesb[:, :], in_=ones_f), R=[cst], W=[onesb])

    with ExitStack() as p0:
        vst = sb(p0, "vst", [82, 128], F32)
        bst = sb(p0, "bst", [96, 128], F32)
        posi = sb(p0, "posi", [128, NBLK], I32)
        badaT = sb(p0, "badaT", [128, 96], F32)
        scb = sb(p0, "scb", [128, 16], BF16)
        pst = ps(p0, "pst", [128, 128], F32)
        pst2 = ps(p0, "pst2", [128, 128], F32)
        prow = [ps(p0, "prow%d" % i, [1, 512], F32) for i in range(2)]
        pmod = ps(p0, "pmod", [128, 96], F32)
        rowb = [sb(p0, "rowb%d" % i, [1, 512], F32) for i in range(2)]
        wa = [sb(p0, "wa%d" % i, [128, KT * 512], BF16) for i in range(2)]

        S.dma("sp", vst[:, :], vecs[:, :], W=[vst])
        S.dma("sp", bst[:, :], b_ada[:, :], W=[bst])
        S.dma("sp", posi[:, :], posT[:, :], W=[posi])
        S.op("dve", lambda h: h.tensor_copy(out=posf[:, :], in_=posi[:, :]), R=[posi], W=[posf])
        S.op("pe", lambda h: h.transpose(pst[:, 0:82], vst[:, :], cst[0:82, C_ID:C_ID + 82]), R=[vst, cst], W=[pst])
        S.op("dve", lambda h: h.tensor_copy(out=colv[:, :], in_=pst[:, 0:82]), R=[pst], W=[colv])
        S.op("pe", lambda h: h.transpose(pst2[:, 0:96], bst[:, :], cst[0:96, C_ID:C_ID + 96]), R=[bst, cst], W=[pst2])
        S.op("dve", lambda h: h.tensor_copy(out=badaT[:, :], in_=pst2[:, 0:96]), R=[pst2], W=[badaT])
        S.op("act", lambda h: h.activation(out=scb[:, :], in_=colv[:, 0:16], func=AF.Silu), R=[colv], W=[scb])
        w_ada_v = w_ada.t.rearrange("(kt p) n -> p kt n", p=128)
        for c in range(24):
            wt = wa[c % 2]
            S.dma("pool", wt[:, :].rearrange("p (kt n) -> p kt n", kt=KT),
                  w_ada_v[:, :, c * 512:(c + 1) * 512], W=[wt])
            pr = prow[c % 2]
            for kt in range(KT):
                S.op("pe", lambda h, kt=kt, wt=wt, pr=pr: h.matmul(
                    pr[:, :], scb[:, kt:kt + 1], wt[:, kt * 512:(kt + 1) * 512],
                    start=(kt == 0), stop=(kt == KT - 1)), R=[scb, wt], W=[pr])
            rb = rowb[c % 2]
            S.op("act", lambda h, rb=rb, pr=pr: h.copy(out=rb[:, :], in_=pr[:, :]), R=[pr], W=[rb])
            for i in range(4):
                col = 4 * c + i
                S.op("pe", lambda h, rb=rb, i=i, col=col: h.matmul(
                    pmod[:, col:col + 1], rb[0:1, i * 128:(i + 1) * 128], cst[0:1, C_ONE:C_ONE + 1],
                    start=True, stop=True), R=[rb, cst], W=[pmod])
        S.op("dve", lambda h: h.tensor_tensor(out=modT[:, :], in0=pmod[:, :], in1=badaT[:, :], op=ALU.add),
             R=[pmod, badaT], W=[modT])
        S.op("dve", lambda h: h.scalar_tensor_tensor(out=gscA[:, :], in0=modT[:, 16:32], scalar=1.0,
                                                     in1=colv[:, 16:32], op0=ALU.add, op1=ALU.mult),
             R=[modT, colv], W=[gscA])
        S.op("dve", lambda h: h.scalar_tensor_tensor(out=gscM[:, :], in0=modT[:, 64:80], scalar=1.0,
                                                     in1=colv[:, 32:48], op0=ALU.add, op1=ALU.mult),
             R=[modT, colv], W=[gscM])
        S.barrier()

    def bcast_row(st, src_tile, src_ap_cols, name, pbank):
        outt = sb(st, name, [128, D], F32)
        with ExitStack() as tmp:
            dg = [sb(tmp, name + "_dg%d" % i, [128, 128], F32) for i in range(2)]
            for kt in range(KT):
                d_ = dg[kt % 2]
                S.op("dve", lambda h, d_=d_, kt=kt: h.tensor_scalar(
                    out=d_[:, :], in0=ident, scalar1=src_ap_cols[:, kt:kt + 1], scalar2=None, op0=ALU.mult),
                    R=[cst, src_tile], W=[d_])
                pb = pbank[kt % 2]
                S.op("pe", lambda h, d_=d_, pb=pb: h.matmul(pb[:, 0:128], ones_f, d_[:, :], start=True, stop=True),
                     R=[cst, d_], W=[pb])
                S.op("act", lambda h, pb=pb, kt=kt: h.copy(out=outt[:, kt * 128:(kt + 1) * 128], in_=pb[:, 0:128]),
                     R=[pb], W=[outt])
            S.barrier()
        return outt

    if dbg and stop == 0:
        S.dma("sp", dbg_d[:, 0:96], modT[:, :], R=[modT], W=[dbg_d])
        S.dma("sp", dbg_d[:, 96:178], colv[:, :], R=[colv], W=[dbg_d])
    if stop == 0:
        S.finish([dbg_d] if dbg else [])
        es.close()
        return nc


    def sincos(st, ang, n, sin_out, cos_out, tag):
        tf = sb(st, tag + "_tf", [128, n], F32)
        ti = sb(st, tag + "_ti", [128, n], I32)
        r = sb(st, tag + "_r", [128, n], F32)
        m = sb(st, tag + "_m", [128, n], F32)
        for which, shift, outap, outtile in ((0, 0.0, sin_out[0], sin_out[1]), (1, PI / 2, cos_out[0], cos_out[1])):
            S.op("dve", lambda h, shift=shift: h.tensor_scalar(out=tf[:, :], in0=ang[:, :], scalar1=shift, scalar2=1.0 / TWO_PI,
                                                  op0=ALU.add, op1=ALU.mult), R=[ang], W=[tf])
            S.op("dve", lambda h: h.tensor_copy(out=ti[:, :], in_=tf[:, :]), R=[tf], W=[ti])
            S.op("dve", lambda h: h.tensor_copy(out=tf[:, :], in_=ti[:, :]), R=[ti], W=[tf])
            S.op("dve", lambda h: h.scalar_tensor_tensor(out=r[:, :], in0=tf[:, :], scalar=-TWO_PI, in1=ang[:, :],
                                                         op0=ALU.mult, op1=ALU.add), R=[tf, ang], W=[r])
            if shift != 0.0:
                S.op("dve", lambda h, shift=shift: h.tensor_scalar(out=r[:, :], in0=r[:, :], scalar1=shift, scalar2=None, op0=ALU.add),
                     R=[r], W=[r])
            S.op("dve", lambda h: h.tensor_scalar(out=m[:, :], in0=r[:, :], scalar1=PI, scalar2=-TWO_PI,
                                                  op0=ALU.is_gt, op1=ALU.mult), R=[r], W=[m])
            S.op("dve", lambda h: h.tensor_tensor(out=r[:, :], in0=r[:, :], in1=m[:, :], op=ALU.add), R=[r, m], W=[r])
            S.op("dve", lambda h: h.tensor_scalar(out=m[:, :], in0=r[:, :], scalar1=-PI, scalar2=TWO_PI,
                                                  op0=ALU.is_lt, op1=ALU.mult), R=[r], W=[m])
            S.op("dve", lambda h: h.tensor_tensor(out=r[:, :], in0=r[:, :], in1=m[:, :], op=ALU.add), R=[r, m], W=[r])
            S.op("dve", lambda h: h.tensor_scalar(out=r[:, :], in0=r[:, :], scalar1=-3.1415925, scalar2=3.1415925,
                                                  op0=ALU.max, op1=ALU.min), R=[r], W=[r])
            S.op("act", lambda h, outap=outap: h.activation(out=outap, in_=r[:, :], func=AF.Sin), R=[r], W=[outtile])

    rp = ExitStack()
    SINH = sb(rp, "SINH", [128, NBLK * 16], F32)
    COSH = sb(rp, "COSH", [128, NBLK * 16], F32)
    SINI = sb(rp, "SINI", [128, NBLK * 8], F32)
    COSI = sb(rp, "COSI", [128, NBLK * 8], F32)
    with ExitStack() as tmp:
        angh = sb(tmp, "angh", [128, NBLK * 16], F32)
        angi = sb(tmp, "angi", [128, NBLK * 8], F32)
        S.op("dve", lambda h: h.tensor_tensor(
            out=angh[:, :].rearrange("p (b i) -> p b i", i=16),
            in0=posf[:, :].unsqueeze(2).to_broadcast([128, NBLK, 16]),
            in1=cst[:, C_INVH:C_INVH + 16].unsqueeze(1).to_broadcast([128, NBLK, 16]), op=ALU.mult),
            R=[posf, cst], W=[angh])
        S.op("dve", lambda h: h.tensor_tensor(
            out=angi[:, :].rearrange("p (b i) -> p b i", i=8),
            in0=posf[:, :].unsqueeze(2).to_broadcast([128, NBLK, 8]),
            in1=cst[:, C_INVI:C_INVI + 8].unsqueeze(1).to_broadcast([128, NBLK, 8]), op=ALU.mult),
            R=[posf, cst], W=[angi])
        sincos(tmp, angh, NBLK * 16, (SINH[:, :], SINH), (COSH[:, :], COSH), "sch")
        sincos(tmp, angi, NBLK * 8, (SINI[:, :], SINI), (COSI[:, :], COSI), "sci")
        S.barrier()

    def rope(eng, st_tiles, x_ap3, nh, half, sin_t, cos_t, blk, out_ap3, Rb, Wb):
        ta, tb = st_tiles
        sn = sin_t[:, blk * half:(blk + 1) * half].unsqueeze(1).to_broadcast([128, nh, half])
        cs = cos_t[:, blk * half:(blk + 1) * half].unsqueeze(1).to_broadcast([128, nh, half])
        x1 = x_ap3[:, :, 0:half]
        x2 = x_ap3[:, :, half:2 * half]
        tav = ta[:, 0:nh * half].rearrange("p (a b) -> p a b", b=half)
        tbv = tb[:, 0:nh * half].rearrange("p (a b) -> p a b", b=half)
        S.op(eng, lambda h: h.tensor_tensor(out=tav, in0=x1, in1=cs, op=ALU.mult), R=Rb + [cos_t], W=[ta])
        S.op(eng, lambda h: h.tensor_tensor(out=tbv, in0=x2, in1=sn, op=ALU.mult), R=Rb + [sin_t], W=[tb])
        S.op(eng, lambda h: h.tensor_tensor(out=out_ap3[:, :, 0:half], in0=tav, in1=tbv, op=ALU.subtract),
             R=[ta, tb], W=Wb)
        S.op(eng, lambda h: h.tensor_tensor(out=tav, in0=x1, in1=sn, op=ALU.mult), R=Rb + [sin_t], W=[ta])
        S.op(eng, lambda h: h.tensor_tensor(out=tbv, in0=x2, in1=cs, op=ALU.mult), R=Rb + [cos_t], W=[tb])
        S.op(eng, lambda h: h.tensor_tensor(out=out_ap3[:, :, half:2 * half], in0=tav, in1=tbv, op=ALU.add),
             R=[ta, tb], W=Wb)

    with ExitStack() as p1:
        NA = 1600
        win_a = sb(p1, "win_a", [128, KT * NA], BF16)
        wv = win_a[:, :].rearrange("p (kt n) -> p kt n", kt=KT)
        w_in_v = w_in.t.rearrange("(kt p) n -> p kt n", p=128)
        for q4 in range(4):
            S.dma("pool", wv[:, 4 * q4:4 * q4 + 4, 0:1024], w_in_v[:, 4 * q4:4 * q4 + 4, U0:U0 + 1024], W=[win_a])
        for q4 in range(2):
            S.dma("pool", wv[:, 8 * q4:8 * q4 + 8, 1024:1536], w_in_v[:, 8 * q4:8 * q4 + 8, K0:K0 + 512], W=[win_a])
        S.dma("pool", wv[:, :, 1536:1600], w_in_v[:, :, KI0:KI0 + 64], W=[win_a])
        pu = [ps(p1, "pu%d" % i, [128, 512], F32) for i in range(2)]
        gsc_bc = bcast_row(p1, gscA, gscA[:, :], "gscA_bc", pu)
        sh_bc = bcast_row(p1, modT, modT[:, 0:16], "shA_bc", pu)
        gk_bc = sb(p1, "gk_bc", [128, 128], F32)
        dgk = sb(p1, "dgk", [128, 128], F32)
        S.op("dve", lambda h: h.tensor_scalar(out=dgk[:, :], in0=ident, scalar1=colv[:, 81:82], scalar2=None, op0=ALU.mult),
             R=[cst, colv], W=[dgk])
        S.op("pe", lambda h: h.matmul(pu[0][:, 0:128], ones_f, dgk[:, :], start=True, stop=True), R=[cst, dgk], W=[pu[0]])
        S.op("act", lambda h: h.copy(out=gk_bc[:, :], in_=pu[0][:, 0:128]), R=[pu[0]], W=[gk_bc])

        NXB = 5
        xt = [sb(p1, "xt%d" % i, [128, D], F32) for i in range(NXB)]
        junk = sb(p1, "junk", [128, D], BF16)
        tmpf = sb(p1, "tmpf", [128, D], BF16)
        hb = [sb(p1, "hb%d" % i, [128, D], BF16) for i in range(2)]
        hT = [sb(p1, "hT%d" % i, [128, KT * 512], BF16) for i in range(2)]
        ss = sb(p1, "ss", [128, NBLK], F32)
        ms = sb(p1, "ms", [128, NBLK], F32)
        rstd = sb(p1, "rstd", [128, NBLK], F32)
        rstdv = sb(p1, "rstdv", [128, NBLK], F32)
        ptr = [ps(p1, "ptr%d" % i, [128, 1024], BF16) for i in range(2)]
        pkvs = [ps(p1, "pkv%d" % i, [128, 512], F32) for i in range(2)]
        pki = ps(p1, "pki", [128, 64], F32)
        pkt = ps(p1, "pkt", [128, 384], BF16)
        ust = [sb(p1, "ust%d" % i, [128, 8 * 512], BF16) for i in range(2)]
        vstg = [sb(p1, "vstg%d" % i, [128, 4 * 256], BF16) for i in range(2)]
        kst = [sb(p1, "kst%d" % i, [128, 2 * 512], BF16) for i in range(1)] * 2
        kist = [sb(p1, "kist%d" % i, [128, 512], BF16) for i in range(1)] * 2
        ssk = sb(p1, "ssk", [128, 2 * NBLK], F32)
        msk = sb(p1, "msk", [128, 2 * NBLK], F32)
        rsk = sb(p1, "rsk", [128, 2 * NBLK], F32)
        kn = [sb(p1, "kn%d" % i, [128, 256], F32) for i in range(1)] * 2
        kr = [sb(p1, "kr%d" % i, [128, 256], BF16) for i in range(2)]
        kif = [sb(p1, "kif%d" % i, [128, 64], F32) for i in range(2)]
        kib = [sb(p1, "kib%d" % i, [128, 128], BF16) for i in range(2)]
        rta = sb(p1, "rta", [128, 32], F32)
        rtb = sb(p1, "rtb", [128, 32], F32)
        jk = sb(p1, "jk", [128, 128], BF16)

        def load_x(blk):
            x_ = xt[blk % NXB]
            S.dma("sp", x_[:, :], xv[blk * 128:(blk + 1) * 128, :], W=[x_])

        def stats(blk):
            x_ = xt[blk % NXB]
            S.op("act", lambda h: h.activation(out=junk[:, :], in_=x_[:, :], func=AF.Square,
                                               accum_out=ss[:, blk:blk + 1]), R=[x_], W=[junk, ss])
            S.op("dve", lambda h: h.tensor_scalar(out=ms[:, blk:blk + 1], in0=ss[:, blk:blk + 1], scalar1=1.0 / D,
                                                  scalar2=EPS, op0=ALU.mult, op1=ALU.add), R=[ss], W=[ms])
            S.op("pool", lambda h: h.tensor_tensor(out=rstd[:, blk:blk + 1], in0=ms[:, blk:blk + 1],
                                                   in1=cst[:, C_NHALF:C_NHALF + 1], op=ALU.pow), R=[ms, cst], W=[rstd])
            S.op("dve", lambda h: h.tensor_tensor(out=rstdv[:, blk:blk + 1], in0=rstd[:, blk:blk + 1],
                                                  in1=validc[:, blk:blk + 1], op=ALU.mult), R=[rstd, validc], W=[rstdv])

        def heavy(blk):
            c, bi = blk // 4, blk % 4
            x_ = xt[blk % NXB]
            h_ = hb[blk % 2]
            hT_ = hT[c % 2]
            S.op("dve", lambda h: h.scalar_tensor_tensor(out=tmpf[:, :], in0=x_[:, :], scalar=rstdv[:, blk:blk + 1],
                                                         in1=gsc_bc[:, :], op0=ALU.mult, op1=ALU.mult),
                 R=[x_, rstdv, gsc_bc], W=[tmpf])
            S.op("dve", lambda h: h.scalar_tensor_tensor(out=h_[:, :], in0=sh_bc[:, :], scalar=validc[:, blk:blk + 1],
                                                         in1=tmpf[:, :], op0=ALU.mult, op1=ALU.add),
                 R=[sh_bc, validc, tmpf], W=[h_])
            hTv = hT_[:, :].rearrange("p (kt n) -> p kt n", kt=KT)
            for half in range(2):
                pt = ptr[half]
                for k8 in range(8):
                    kt = half * 8 + k8
                    S.op("pe", lambda h, kt=kt, k8=k8, pt=pt: h.transpose(
                        pt[:, k8 * 128:(k8 + 1) * 128], h_[:, kt * 128:(kt + 1) * 128], identb[:, :]),
                        R=[h_, identb], W=[pt])
                S.op("act" if half == 0 else "pool" if False else "act", lambda h, half=half, pt=pt: h.copy(
                    out=hTv[:, half * 8:(half + 1) * 8, bi * 128:(bi + 1) * 128],
                    in_=pt[:, :].rearrange("p (a b) -> p a b", b=128)), R=[pt], W=[hT_])

        def upart(c, tiles):
            hT_ = hT[c % 2]
            hTv = hT_[:, :].rearrange("p (kt n) -> p kt n", kt=KT)
            us = ust[c % 2]
            for t in tiles:
                p_ = pu[t % 2]
                for kt in range(KT):
                    S.op("pe", lambda h, kt=kt, t=t, p_=p_: h.matmul(
                        p_[:, :], wv[:, kt, t * 128:(t + 1) * 128], hTv[:, kt, :],
                        start=(kt == 0), stop=(kt == KT - 1)), R=[win_a, hT_], W=[p_])
                S.op("act", lambda h, t=t, p_=p_: h.copy(out=us[:, t * 512:(t + 1) * 512], in_=p_[:, :]), R=[p_], W=[us])
            if 7 in tiles:
                S.dma("pool", uT_scr[:, :, c * 512:(c + 1) * 512], us[:, :].rearrange("p (t n) -> p t n", t=8), R=[us], W=[uT_scr])

        def kv_main(c, bi):
            hT_ = hT[c % 2]
            hTv = hT_[:, :].rearrange("p (kt n) -> p kt n", kt=KT)
            vs = vstg[c % 2]
            blk = 4 * c + bi
            pkv = pkvs[blk % 2]
            for kt in range(KT):
                S.op("pe", lambda h, kt=kt: h.matmul(
                    pkv[:, :], hTv[:, kt, bi * 128:(bi + 1) * 128], wv[:, kt, 1024:1536],
                    start=(kt == 0), stop=(kt == KT - 1)), R=[hT_, win_a], W=[pkv])
            for kt in range(KT):
                S.op("pe", lambda h, kt=kt: h.matmul(
                    pki[:, :], hTv[:, kt, bi * 128:(bi + 1) * 128], wv[:, kt, 1536:1600],
                    start=(kt == 0), stop=(kt == KT - 1)), R=[hT_, win_a], W=[pki])
            S.op("act", lambda h: h.copy(out=vs[:, bi * 256:(bi + 1) * 256], in_=pkv[:, 256:512]), R=[pkv], W=[vs])
            kn_ = kn[blk % 2]
            kr_ = kr[blk % 2]
            for g in range(2):
                S.op("act", lambda h, g=g: h.activation(out=jk[:, :], in_=pkv[:, g * 128:(g + 1) * 128], func=AF.Square,
                                                        accum_out=ssk[:, 2 * blk + g:2 * blk + g + 1]), R=[pkv], W=[jk, ssk])
            kif_ = kif[blk % 2]
            kib_ = kib[blk % 2]
            S.op("act", lambda h: h.copy(out=kif_[:, :], in_=pki[:, :]), R=[pki], W=[kif_])
            S.op("dve", lambda h: h.tensor_scalar(out=msk[:, 2 * blk:2 * blk + 2], in0=ssk[:, 2 * blk:2 * blk + 2],
                                                  scalar1=1.0 / 128, scalar2=EPS, op0=ALU.mult, op1=ALU.add), R=[ssk], W=[msk])
            S.op("pool", lambda h: h.tensor_tensor(out=rsk[:, 2 * blk:2 * blk + 2], in0=msk[:, 2 * blk:2 * blk + 2],
                                                   in1=cst[:, C_NHALF:C_NHALF + 1].to_broadcast([128, 2]), op=ALU.pow),
                 R=[msk, cst], W=[rsk])
            for g in range(2):
                S.op("dve", lambda h, g=g: h.scalar_tensor_tensor(
                    out=kn_[:, g * 128:(g + 1) * 128], in0=pkv[:, g * 128:(g + 1) * 128],
                    scalar=rsk[:, 2 * blk + g:2 * blk + g + 1], in1=gk_bc[:, :], op0=ALU.mult, op1=ALU.mult),
                    R=[pkv, rsk, gk_bc], W=[kn_])
            S.op("pool", lambda h: h.tensor_copy(out=kr_[:, :], in_=kn_[:, :]), R=[kn_], W=[kr_])
            rope("pool", (rta, rtb), kn_[:, :].rearrange("p (a b) -> p a b", b=128), 2, 16, SINH, COSH, blk,
                 kr_[:, :].rearrange("p (a b) -> p a b", b=128), [kn_], [kr_])
            S.op("pool", lambda h: h.tensor_copy(out=kib_[:, 0:64], in_=kif_[:, :]), R=[kif_], W=[kib_])
            rope("pool", (rta, rtb), kif_[:, :].rearrange("p (a b) -> p a b", a=1), 1, 8, SINI, COSI, blk,
                 kib_[:, 0:64].rearrange("p (a b) -> p a b", a=1), [kif_], [kib_])
            S.op("pool", lambda h: h.tensor_copy(out=kib_[:, 64:128], in_=kib_[:, 0:64]), R=[kib_], W=[kib_])

        def kv_tail(c, bi):
            hT_ = hT[c % 2]
            hTv = hT_[:, :].rearrange("p (kt n) -> p kt n", kt=KT)
            vs = vstg[c % 2]
            ks = kst[c % 2]
            kis = kist[c % 2]
            blk = 4 * c + bi
            kr_ = kr[blk % 2]
            kib_ = kib[blk % 2]
            for g in range(2):
                S.op("pe", lambda h, g=g: h.transpose(pkt[:, g * 128:(g + 1) * 128], kr_[:, g * 128:(g + 1) * 128], identb[:, :]),
                     R=[kr_, identb], W=[pkt])
            S.op("pe", lambda h: h.transpose(pkt[:, 256:384], kib_[:, :], identb[:, :]), R=[kib_, identb], W=[pkt])
            S.op("act", lambda h: h.copy(
                out=ks[:, :].rearrange("p (g n) -> p g n", g=2)[:, :, bi * 128:(bi + 1) * 128],
                in_=pkt[:, 0:256].rearrange("p (g n) -> p g n", g=2)), R=[pkt], W=[ks])
            S.op("act", lambda h: h.copy(out=kis[:, bi * 128:(bi + 1) * 128], in_=pkt[:, 256:384]), R=[pkt], W=[kis])
            if bi != 3:
                return
            S.dma("pool", V_scr[:, 4 * c:4 * c + 4, :], vs[:, :].rearrange("p (b n) -> p b n", b=4), R=[vs], W=[V_scr])
            S.dma("pool", KT_scr[:, :, c * 512:(c + 1) * 512], ks[:, :].rearrange("p (g n) -> p g n", g=2), R=[ks], W=[KT_scr])
            S.dma("pool", kiT_scr[:, c * 512:(c + 1) * 512], kis[:, :], R=[kis], W=[kiT_scr])
            S.dma("pool", hTo_scr[:, c, :, :], hTv[:, :, 384:512], R=[hT_], W=[hTo_scr])

        nblk_run = NBLK if FULL else 8
        for b_ in range(4):
            load_x(b_)
        for b_ in range(3):
            stats(b_)
        for blk in range(nblk_run + 5):
            if blk + 4 < nblk_run:
                load_x(blk + 4)
            if blk + 3 < nblk_run:
                stats(blk + 3)
            if blk < nblk_run:
                heavy(blk)
            if 4 <= blk < nblk_run + 4:
                b4 = blk - 4
                upart(b4 // 4, [2 * (b4 % 4), 2 * (b4 % 4) + 1])
                kv_main(b4 // 4, b4 % 4)
            if blk >= 5:
                b5 = blk - 5
                kv_tail(b5 // 4, b5 % 4)
        S.barrier()
        print("n_inst after P1a", S.n_inst)


    with ExitStack() as p1c:
        NC_ = 2064
        win_c = sb(p1c, "win_c", [128, KT * NC_], BF16)
        wc = win_c[:, :].rearrange("p (kt n) -> p kt n", kt=KT)
        w_in_v = w_in.t.rearrange("(kt p) n -> p kt n", p=128)
        for q4 in range(4):
            S.dma("pool", wc[:, 4 * q4:4 * q4 + 4, 0:1024], w_in_v[:, 4 * q4:4 * q4 + 4, Q0:Q0 + 1024], W=[win_c])
        for q4 in range(4):
            S.dma("pool", wc[:, 4 * q4:4 * q4 + 4, 1024:2048], w_in_v[:, 4 * q4:4 * q4 + 4, QI0:QI0 + 1024], W=[win_c])
        S.dma("pool", wc[:, :, 2048:2064], w_in_v[:, :, WI0:WI0 + 16], W=[win_c])
        pq = [ps(p1c, "pq%d" % i, [128, 512], F32) for i in range(2)]
        pqi = [ps(p1c, "pqi%d" % i, [128, 512], F32) for i in range(2)]
        pwi = ps(p1c, "pwi", [128, 512], F32)
        ptq = ps(p1c, "ptq", [128, 1024], BF16)
        ptqi = ps(p1c, "ptqi", [128, 1024], BF16)
        gq_bc = sb(p1c, "gq_bc", [128, 128], F32)
        dgq = sb(p1c, "dgq", [128, 128], F32)
        S.op("dve", lambda h: h.tensor_scalar(out=dgq[:, :], in0=ident, scalar1=colv[:, 80:81], scalar2=None, op0=ALU.mult),
             R=[cst, colv], W=[dgq])
        S.op("pe", lambda h: h.matmul(pwi[:, 0:128], ones_f, dgq[:, :], start=True, stop=True), R=[cst, dgq], W=[pwi])
        S.op("act", lambda h: h.copy(out=gq_bc[:, :], in_=pwi[:, 0:128]), R=[pwi], W=[gq_bc])
        hTo = [sb(p1c, "hTo%d" % i, [128, KT * 128], BF16) for i in range(2)]
        ssq = sb(p1c, "ssq", [128, 8 * NCH], F32)
        msq = sb(p1c, "msq", [128, 8 * NCH], F32)
        rsq = sb(p1c, "rsq", [128, 8 * NCH], F32)
        jq = sb(p1c, "jq", [128, 128], BF16)
        qn = sb(p1c, "qn", [128, 1024], F32)
        qr = sb(p1c, "qr", [128, 1024], BF16)
        qif = sb(p1c, "qif", [128, 1024], F32)
        qib = sb(p1c, "qib", [128, 1024], BF16)
        rta2 = sb(p1c, "rta2", [128, 128], F32)
        rtb2 = sb(p1c, "rtb2", [128, 128], F32)
        qTs = [sb(p1c, "qTs%d" % i, [128, 1024], BF16) for i in range(2)]
        qiTs = [sb(p1c, "qiTs%d" % i, [128, 1024], BF16) for i in range(2)]
        nm = NCH if FULL else 2
        def c_mm(m):
            blk = 4 * m + 3
            ho = hTo[m % 2]
            hov = ho[:, :].rearrange("p (kt n) -> p kt n", kt=KT)
            S.dma("sp", hov, hTo_scr[:, m, :, :], R=[hTo_scr], W=[ho])
            for cch in range(2):
                for kt in range(KT):
                    S.op("pe", lambda h, kt=kt, cch=cch: h.matmul(
                        pq[cch][:, :], hov[:, kt, :], wc[:, kt, cch * 512:(cch + 1) * 512],
                        start=(kt == 0), stop=(kt == KT - 1)), R=[ho, win_c], W=[pq[cch]])
            for cch in range(2):
                for kt in range(KT):
                    S.op("pe", lambda h, kt=kt, cch=cch: h.matmul(
                        pqi[cch][:, :], hov[:, kt, :], wc[:, kt, 1024 + cch * 512:1024 + (cch + 1) * 512],
                        start=(kt == 0), stop=(kt == KT - 1)), R=[ho, win_c], W=[pqi[cch]])
            for kt in range(KT):
                S.op("pe", lambda h, kt=kt: h.matmul(
                    pwi[:, 0:16], hov[:, kt, :], wc[:, kt, 2048:2064],
                    start=(kt == 0), stop=(kt == KT - 1)), R=[ho, win_c], W=[pwi])
            S.op("dve", lambda h: h.tensor_scalar(out=wi_all[:, 16 * m:16 * m + 16], in0=pwi[:, 0:16], scalar1=0.25 * 0.125,
                                                  scalar2=None, op0=ALU.mult), R=[pwi], W=[wi_all])

        def c_post(m):
            blk = 4 * m + 3
            for hh in range(8):
                S.op("act", lambda h, hh=hh: h.activation(
                    out=jq[:, :], in_=pq[hh // 4][:, (hh % 4) * 128:(hh % 4 + 1) * 128], func=AF.Square,
                    accum_out=ssq[:, 8 * m + hh:8 * m + hh + 1]), R=[pq[hh // 4]], W=[jq, ssq])
            S.op("dve", lambda h: h.tensor_scalar(out=msq[:, 8 * m:8 * m + 8], in0=ssq[:, 8 * m:8 * m + 8], scalar1=1.0 / 128,
                                                  scalar2=EPS, op0=ALU.mult, op1=ALU.add), R=[ssq], W=[msq])
            S.op("pool", lambda h: h.tensor_tensor(out=rsq[:, 8 * m:8 * m + 8], in0=msq[:, 8 * m:8 * m + 8],
                                                   in1=cst[:, C_NHALF:C_NHALF + 1].to_broadcast([128, 8]), op=ALU.pow),
                 R=[msq, cst], W=[rsq])
            for hh in range(8):
                S.op("dve", lambda h, hh=hh: h.scalar_tensor_tensor(
                    out=qn[:, hh * 128:(hh + 1) * 128], in0=pq[hh // 4][:, (hh % 4) * 128:(hh % 4 + 1) * 128],
                    scalar=rsq[:, 8 * m + hh:8 * m + hh + 1], in1=gq_bc[:, :], op0=ALU.mult, op1=ALU.mult),
                    R=[pq[hh // 4], rsq, gq_bc], W=[qn])
            S.op("pool", lambda h: h.tensor_copy(out=qr[:, :], in_=qn[:, :]), R=[qn], W=[qr])
            rope("pool", (rta2, rtb2), qn[:, :].rearrange("p (a b) -> p a b", b=128), 8, 16, SINH, COSH, blk,
                 qr[:, :].rearrange("p (a b) -> p a b", b=128), [qn], [qr])
            for cch in range(2):
                S.op("act", lambda h, cch=cch: h.copy(out=qif[:, cch * 512:(cch + 1) * 512], in_=pqi[cch][:, :]), R=[pqi[cch]], W=[qif])
            S.op("pool", lambda h: h.tensor_copy(out=qib[:, :], in_=qif[:, :]), R=[qif], W=[qib])
            rope("pool", (rta2, rtb2), qif[:, :].rearrange("p (a b) -> p a b", b=64), 16, 8, SINI, COSI, blk,
                 qib[:, :].rearrange("p (a b) -> p a b", b=64), [qif], [qib])

        def c_tail(m):
            for hh in range(8):
                S.op("pe", lambda h, hh=hh: h.transpose(ptq[:, hh * 128:(hh + 1) * 128], qr[:, hh * 128:(hh + 1) * 128], identb[:, :]),
                     R=[qr, identb], W=[ptq])
            qs = qTs[m % 2]
            S.op("act", lambda h: h.copy(out=qs[:, :], in_=ptq[:, :]), R=[ptq], W=[qs])
            S.dma("sp", QT_scr[:, m, :, :], qs[:, :].rearrange("p (a b) -> p a b", b=128), R=[qs], W=[QT_scr])
            for pr in range(8):
                S.op("pe", lambda h, pr=pr: h.transpose(ptqi[:, pr * 128:(pr + 1) * 128], qib[:, pr * 128:(pr + 1) * 128], identb[:, :]),
                     R=[qib, identb], W=[ptqi])
            qis = qiTs[m % 2]
            S.op("act", lambda h: h.copy(out=qis[:, :], in_=ptqi[:, :]), R=[ptqi], W=[qis])
            S.dma("sp", qiT_scr[:, m, :, :], qis[:, :].rearrange("p (a b) -> p a b", b=128), R=[qis], W=[qiT_scr])

        for m in range(nm + 1):
            if m < nm:
                c_mm(m)
            if m >= 1:
                c_tail(m - 1)
            if m < nm:
                c_post(m)
        S.barrier()
    rp.close()
    print("n_inst after P1c", S.n_inst)

    if stop == 1:
        if dbg:
            with ExitStack() as dd:
                d1 = sb(dd, "d1", [128, 6144], BF16)
                d2 = sb(dd, "d2", [128, 6144], F32)
                S.dma("sp", d1[:, 0:1024], uT_scr[:, 0, 0:1024], R=[uT_scr], W=[d1])
                S.dma("sp", d1[:, 1024:2048], KT_scr[:, 1, 0:1024], R=[KT_scr], W=[d1])
                S.dma("sp", d1[:, 2048:3072].rearrange("p (b n) -> p b n", b=4), V_scr[:, 0:4, :], R=[V_scr], W=[d1])
                S.dma("sp", d1[:, 3072:4096], kiT_scr[:, 0:1024], R=[kiT_scr], W=[d1])
                S.dma("sp", d1[:, 4096:5120].rearrange("p (a b) -> p a b", b=128), QT_scr[:, 1, :, :], R=[QT_scr], W=[d1])
                S.dma("sp", d1[:, 5120:6144].rearrange("p (a b) -> p a b", b=128), qiT_scr[:, 1, :, :], R=[qiT_scr], W=[d1])
                S.op("dve", lambda h: h.tensor_copy(out=d2[:, :], in_=d1[:, :]), R=[d1], W=[d2])
                S.dma("sp", dbg_d[:, 0:6144], d2[:, :], R=[d2], W=[dbg_d])
                S.dma("sp", dbg_d[:, 6144:6144 + 256], wi_all[:, :], R=[wi_all], W=[dbg_d])
                S.finish([dbg_d])
        es.close()
        return nc


    yaT_scr = dscr("yaT_scr", [128, 8, 2048], BF16)

    def sincos2(ang, n, sin_ap, sin_tile, cos_ap, cos_tile, tf, ti, r, m):
        for shift, outap, outtile in ((0.0, sin_ap, sin_tile), (PI / 2, cos_ap, cos_tile)):
            S.op("dve", lambda h, shift=shift: h.tensor_scalar(out=tf[:, 0:n], in0=ang[:, 0:n], scalar1=shift, scalar2=1.0 / TWO_PI,
                                                               op0=ALU.add, op1=ALU.mult), R=[ang], W=[tf])
            S.op("dve", lambda h: h.tensor_copy(out=ti[:, 0:n], in_=tf[:, 0:n]), R=[tf], W=[ti])
            S.op("dve", lambda h: h.tensor_copy(out=tf[:, 0:n], in_=ti[:, 0:n]), R=[ti], W=[tf])
            S.op("dve", lambda h: h.scalar_tensor_tensor(out=r[:, 0:n], in0=tf[:, 0:n], scalar=-TWO_PI, in1=ang[:, 0:n],
                                                         op0=ALU.mult, op1=ALU.add), R=[tf, ang], W=[r])
            if shift != 0.0:
                S.op("dve", lambda h, shift=shift: h.tensor_scalar(out=r[:, 0:n], in0=r[:, 0:n], scalar1=shift, scalar2=None,
                                                                   op0=ALU.add), R=[r], W=[r])
            S.op("dve", lambda h: h.tensor_scalar(out=m[:, 0:n], in0=r[:, 0:n], scalar1=PI, scalar2=-TWO_PI,
                                                  op0=ALU.is_gt, op1=ALU.mult), R=[r], W=[m])
            S.op("dve", lambda h: h.tensor_tensor(out=r[:, 0:n], in0=r[:, 0:n], in1=m[:, 0:n], op=ALU.add), R=[r, m], W=[r])
            S.op("dve", lambda h: h.tensor_scalar(out=m[:, 0:n], in0=r[:, 0:n], scalar1=-PI, scalar2=TWO_PI,
                                                  op0=ALU.is_lt, op1=ALU.mult), R=[r], W=[m])
            S.op("dve", lambda h: h.tensor_tensor(out=r[:, 0:n], in0=r[:, 0:n], in1=m[:, 0:n], op=ALU.add), R=[r, m], W=[r])
            S.op("dve", lambda h: h.tensor_scalar(out=r[:, 0:n], in0=r[:, 0:n], scalar1=-3.1415925, scalar2=3.1415925,
                                                  op0=ALU.max, op1=ALU.min), R=[r], W=[r])
            S.op("act", lambda h, outap=outap: h.activation(out=outap, in_=r[:, 0:n], func=AF.Sin), R=[r], W=[outtile])

    with ExitStack() as s5:
        Bpad = sb(s5, "Bpad", [128, 64 * 128], BF16)
        Bswp = sb(s5, "Bswp", [128, 64 * 128], BF16)
        CWc = sb(s5, "CWc", [128, 64 * 128], BF16)
        CWs = sb(s5, "CWs", [128, 64 * 128], BF16)
        COS = sb(s5, "COS", [128, 64 * 128], F32)
        SIN = sb(s5, "SIN", [128, 64 * 128], F32)
        RHO = sb(s5, "RHO", [128, 64], F32)
        CT = sb(s5, "CT", [128, 64], F32)
        STs = sb(s5, "STs", [128, 64], F32)
        TH = sb(s5, "TH", [128, 64], F32)
        Wt = sb(s5, "Wt", [128, 64 * 128], F32)
        R128 = sb(s5, "R128", [128, 64], F32)
        junkz = sb(s5, "junkz", [128, 128], F32)
        pA = [ps(s5, "pA%d" % i, [128, 512], F32) for i in range(2)]
        pB = [ps(s5, "pB%d" % i, [128, 512], F32) for i in range(2)]
        pY = [ps(s5, "pY%d" % i, [128, 512], F32) for i in range(2)]
        pS = ps(s5, "pS", [128, 512], F32)
        pS2 = ps(s5, "pS2", [128, 512], F32)
        with ExitStack() as su:
            lst_r = sb(su, "lst_r", [64, 128], F32)
            lst_i = sb(su, "lst_i", [64, 128], F32)
            ldr = sb(su, "ldr", [1, 64], F32)
            S.dma("sp", lst_r[:, 0:64], lam_re[:, :], W=[lst_r])
            S.dma("sp", lst_r[:, 64:128], lam_re[:, :], W=[lst_r])
            S.dma("sp", lst_i[:, 0:64], lam_im[:, :], W=[lst_i])
            S.dma("sp", lst_i[:, 64:128], lam_im[:, :], W=[lst_i])
            S.dma("sp", ldr[:, :], log_dt[:, :], W=[ldr])
            names = ["LR", "LI", "DT", "A_", "COST", "SINT", "ABR", "ABI", "AM1", "DEN", "FR", "FI", "T1", "T2", "TH128", "S128", "C128"]
            V = {n_: sb(su, "s5_" + n_, [128, 64], F32) for n_ in names}
            id64 = cst[0:64, C_ID:C_ID + 64]
            S.op("pe", lambda h: h.transpose(pS[:, 0:64], lst_r[:, :], id64), R=[lst_r, cst], W=[pS])
            S.op("dve", lambda h: h.tensor_scalar(out=V["LR"][:, :], in0=pS[:, 0:64], scalar1=-1e-4, scalar2=None, op0=ALU.min),
                 R=[pS], W=[V["LR"]])
            S.op("pe", lambda h: h.transpose(pS2[:, 0:64], lst_i[:, :], id64), R=[lst_i, cst], W=[pS2])
            S.op("dve", lambda h: h.tensor_copy(out=V["LI"][:, :], in_=pS2[:, 0:64]), R=[pS2], W=[V["LI"]])
            S.op("pe", lambda h: h.matmul(pS[:, 64:128], cst[0:1, C_ONE:C_ONE + 128], ldr[0:1, :], start=True, stop=True),
                 R=[cst, ldr, V["LR"]], W=[pS])
            S.op("act", lambda h: h.activation(out=V["DT"][:, :], in_=pS[:, 64:128], func=AF.Exp), R=[pS], W=[V["DT"]])
            tt = lambda o, a, b, op: S.op("dve", lambda h: h.tensor_tensor(out=V[o][:, :], in0=V[a][:, :], in1=V[b][:, :], op=op),
                                          R=[V[a], V[b]], W=[V[o]])
            tt("A_", "LR", "DT", ALU.mult)
            S.op("act", lambda h: h.activation(out=RHO[:, :], in_=V["A_"][:, :], func=AF.Exp), R=[V["A_"]], W=[RHO])
            S.op("dve", lambda h: h.tensor_tensor(out=TH[:, :], in0=V["LI"][:, :], in1=V["DT"][:, :], op=ALU.mult),
                 R=[V["LI"], V["DT"]], W=[TH])
            su1 = ExitStack()
            tfa = sb(su1, "tfa", [128, 1024], F32)
            tia = sb(su1, "tia", [128, 1024], I32)
            ra = sb(su1, "ra", [128, 1024], F32)
            ma = sb(su1, "ma", [128, 1024], F32)
            anga = sb(su1, "anga", [128, 1024], F32)
            sincos2(TH, 64, V["SINT"][:, :], V["SINT"], V["COST"][:, :], V["COST"], tfa, tia, ra, ma)
            S.op("dve", lambda h: h.tensor_tensor(out=V["ABR"][:, :], in0=RHO[:, :], in1=V["COST"][:, :], op=ALU.mult),
                 R=[RHO, V["COST"]], W=[V["ABR"]])
            S.op("dve", lambda h: h.tensor_tensor(out=V["ABI"][:, :], in0=RHO[:, :], in1=V["SINT"][:, :], op=ALU.mult),
                 R=[RHO, V["SINT"]], W=[V["ABI"]])
            S.op("dve", lambda h: h.tensor_scalar(out=V["AM1"][:, :], in0=V["ABR"][:, :], scalar1=-1.0, scalar2=None, op0=ALU.add),
                 R=[V["ABR"]], W=[V["AM1"]])
            tt("T1", "LR", "LR", ALU.mult)
            tt("T2", "LI", "LI", ALU.mult)
            tt("DEN", "T1", "T2", ALU.add)
            S.op("dve", lambda h: h.reciprocal(out=V["DEN"][:, :], in_=V["DEN"][:, :]), R=[V["DEN"]], W=[V["DEN"]])
            tt("T1", "AM1", "LR", ALU.mult)
            tt("T2", "ABI", "LI", ALU.mult)
            tt("FR", "T1", "T2", ALU.add)
            tt("FR", "FR", "DEN", ALU.mult)
            tt("T1", "ABI", "LR", ALU.mult)
            tt("T2", "AM1", "LI", ALU.mult)
            tt("FI", "T1", "T2", ALU.subtract)
            tt("FI", "FI", "DEN", ALU.mult)
            S.op("dve", lambda h: h.tensor_scalar(out=V["TH128"][:, :], in0=TH[:, :], scalar1=128.0, scalar2=None, op0=ALU.mult),
                 R=[TH], W=[V["TH128"]])
            sincos2(V["TH128"], 64, V["S128"][:, :], V["S128"], CT[:, :], CT, tfa, tia, ra, ma)
            S.op("dve", lambda h: h.tensor_scalar(out=STs[:, :], in0=V["S128"][:, :], scalar1=cst[:, C_SG:C_SG + 1], scalar2=-1.0,
                                                  op0=ALU.mult, op1=ALU.mult), R=[V["S128"], cst], W=[STs])
            for sl in range(8):
                S.op("dve", lambda h, sl=sl: h.tensor_tensor(
                    out=anga[:, :].rearrange("p (g j) -> p g j", j=128),
                    in0=TH[:, sl * 8:(sl + 1) * 8].unsqueeze(2).to_broadcast([128, 8, 128]),
                    in1=cst[:, C_IOTA:C_IOTA + 128].unsqueeze(1).to_broadcast([128, 8, 128]), op=ALU.mult),
                    R=[TH, cst], W=[anga])
                sincos2(anga, 1024, SIN[:, sl * 1024:(sl + 1) * 1024], SIN, COS[:, sl * 1024:(sl + 1) * 1024], COS, tfa, tia, ra, ma)
            rev = sb(su1, "rev", [128, 128], F32)
            S.op("dve", lambda h: h.tensor_scalar(out=rev[:, :], in0=cst[:, C_IOTA:C_IOTA + 128], scalar1=-1.0, scalar2=127.0,
                                                  op0=ALU.mult, op1=ALU.add), R=[cst], W=[rev])
            for sl in range(8):
                S.op("dve", lambda h, sl=sl: h.tensor_tensor(
                    out=anga[:, :].rearrange("p (g j) -> p g j", j=128),
                    in0=V["A_"][:, sl * 8:(sl + 1) * 8].unsqueeze(2).to_broadcast([128, 8, 128]),
                    in1=rev[:, :].unsqueeze(1).to_broadcast([128, 8, 128]), op=ALU.mult),
                    R=[V["A_"], rev], W=[anga])
                S.op("act", lambda h, sl=sl: h.activation(out=Wt[:, sl * 1024:(sl + 1) * 1024], in_=anga[:, :], func=AF.Exp),
                     R=[anga], W=[Wt])
            S.op("act", lambda h: h.activation(out=R128[:, :], in_=V["A_"][:, :], func=AF.Exp, scale=128.0), R=[V["A_"]], W=[R128])
            S.barrier()
            su1.close()
            BRE = sb(su, "BRE", [64, 1024], F32)
            BIM = sb(su, "BIM", [64, 1024], F32)
            BBR = sb(su, "BBR", [64, 1024], F32)
            BBI = sb(su, "BBI", [64, 1024], F32)
            NBR = sb(su, "NBR", [64, 1024], F32)
            BT = sb(su, "BT", [64, 1024], F32)
            S.dma("sp", BRE[:, :].rearrange("p (g h) -> p g h", h=16), b_re.t.rearrange("g p h -> p g h"), W=[BRE])
            S.dma("sp", BIM[:, :].rearrange("p (g h) -> p g h", h=16), b_im.t.rearrange("g p h -> p g h"), W=[BIM])
            frb = V["FR"][0:64, :].unsqueeze(2).to_broadcast([64, 64, 16])
            fib = V["FI"][0:64, :].unsqueeze(2).to_broadcast([64, 64, 16])
            v3 = lambda t_: t_[:, :].rearrange("p (g h) -> p g h", h=16)
            S.op("dve", lambda h: h.tensor_tensor(out=v3(BBR), in0=v3(BRE), in1=frb, op=ALU.mult), R=[BRE, V["FR"]], W=[BBR])
            S.op("dve", lambda h: h.tensor_tensor(out=v3(BT), in0=v3(BIM), in1=fib, op=ALU.mult), R=[BIM, V["FI"]], W=[BT])
            S.op("dve", lambda h: h.tensor_tensor(out=BBR[:, :], in0=BBR[:, :], in1=BT[:, :], op=ALU.subtract), R=[BBR, BT], W=[BBR])
            S.op("dve", lambda h: h.tensor_tensor(out=v3(BBI), in0=v3(BIM), in1=frb, op=ALU.mult), R=[BIM, V["FR"]], W=[BBI])
            S.op("dve", lambda h: h.tensor_tensor(out=v3(BT), in0=v3(BRE), in1=fib, op=ALU.mult), R=[BRE, V["FI"]], W=[BT])
            S.op("dve", lambda h: h.tensor_tensor(out=BBI[:, :], in0=BBI[:, :], in1=BT[:, :], op=ALU.add), R=[BBI, BT], W=[BBI])
            S.op("dve", lambda h: h.tensor_scalar(out=NBR[:, :], in0=BBR[:, :], scalar1=-1.0, scalar2=None, op0=ALU.mult), R=[BBR], W=[NBR])
            CS1 = [sb(su, "CS1_%d" % i, [128, 128], F32) for i in range(2)]
            CS2 = [sb(su, "CS2_%d" % i, [128, 128], F32) for i in range(2)]
            for t in range(8):
                sl_ = slice(t * 128, (t + 1) * 128)
                S.op("pe", lambda h: h.transpose(pS[:, 0:64], BBR[:, sl_], id64), R=[BBR, cst], W=[pS])
                S.op("pe", lambda h: h.transpose(pS[:, 64:128], BBI[:, sl_], id64), R=[BBI, cst], W=[pS])
                S.op("pe", lambda h: h.transpose(pS2[:, 0:64], BBI[:, sl_], id64), R=[BBI, cst], W=[pS2])
                S.op("pe", lambda h: h.transpose(pS2[:, 64:128], NBR[:, sl_], id64), R=[NBR, cst], W=[pS2])
                for g8 in range(8):
                    g = 8 * t + g8
                    S.op("dve", lambda h, g=g, g8=g8: h.tensor_scalar(
                        out=Bpad[:, g * 128:(g + 1) * 128], in0=pS[:, 0:128], scalar1=cst[:, C_RM + g8:C_RM + g8 + 1],
                        scalar2=None, op0=ALU.mult), R=[pS, cst], W=[Bpad])
                    S.op("act", lambda h, g=g, g8=g8: h.activation(
                        out=Bswp[:, g * 128:(g + 1) * 128], in_=pS2[:, 0:128], func=AF.Copy,
                        scale=cst[:, C_RM + g8:C_RM + g8 + 1]), R=[pS2, cst], W=[Bswp])
                c1, c2 = CS1[t % 2], CS2[t % 2]
                S.dma("sp", c1[:, 0:64], c_re[t * 128:(t + 1) * 128, :], W=[c1])
                S.dma("sp", c1[:, 64:128], c_im[t * 128:(t + 1) * 128, :], W=[c1])
                S.dma("sp", c2[:, 0:64], c_im[t * 128:(t + 1) * 128, :], W=[c2])
                S.dma("sp", c2[:, 64:128], c_re[t * 128:(t + 1) * 128, :], W=[c2])
                S.op("pe", lambda h, c1=c1: h.transpose(pS[:, 128:256], c1[:, :], ident), R=[c1, cst], W=[pS])
                S.op("pe", lambda h, c2=c2: h.transpose(pS2[:, 128:256], c2[:, :], ident), R=[c2, cst], W=[pS2])
                for g8 in range(8):
                    g = 8 * t + g8
                    S.op("dve", lambda h, g=g, g8=g8: h.scalar_tensor_tensor(
                        out=CWc[:, g * 128:(g + 1) * 128], in0=pS[:, 128:256], scalar=cst[:, C_SG:C_SG + 1],
                        in1=cst[:, C_CM8 + g8 * 128:C_CM8 + (g8 + 1) * 128], op0=ALU.mult, op1=ALU.mult), R=[pS, cst], W=[CWc])
                    S.op("dve", lambda h, g=g, g8=g8: h.scalar_tensor_tensor(
                        out=CWs[:, g * 128:(g + 1) * 128], in0=pS2[:, 128:256], scalar=-1.0,
                        in1=cst[:, C_CM8 + g8 * 128:C_CM8 + (g8 + 1) * 128], op0=ALU.mult, op1=ALU.mult), R=[pS2, cst], W=[CWs])
            S.barrier()

        swp = cst[:, C_SW:C_SW + 128]
        uTb = [sb(s5, "uTb%d" % i, [128, 8 * 128], BF16) for i in range(2)]
        t1 = [sb(s5, "t1_%d" % i, [128, 512], F32) for i in range(2)]
        t2 = [sb(s5, "t2_%d" % i, [128, 512], F32) for i in range(2)]
        xs = [sb(s5, "xs_%d" % i, [128, 512], F32) for i in range(2)]
        zt = [sb(s5, "zt_%d" % i, [128, 512], F32) for i in range(2)]
        Zc = [sb(s5, "Zc_%d" % i, [128, 512], BF16) for i in range(2)]
        Zs = [sb(s5, "Zs_%d" % i, [128, 512], BF16) for i in range(2)]
        zl = sb(s5, "zl", [128, 64], F32)
        zc = sb(s5, "zc", [128, 64], F32)
        c1t = sb(s5, "c1t", [128, 64], F32)
        c2t = sb(s5, "c2t", [128, 64], F32)
        yf = sb(s5, "yf", [128, 1024], F32)
        g1 = sb(s5, "g1", [128, 1024], F32)
        yab = [sb(s5, "yab%d" % i, [128, 1024], BF16) for i in range(1)]
        S.op("dve", lambda h: h.memset(zc[:, :], 0.0), W=[zc])
        nb5 = NBLK if FULL else 8
        NQ = nb5 * 16
        ubv_of = lambda blk: uTb[blk % 2][:, :].rearrange("p (t n) -> p t n", t=8)

        def load_ub(blk):
            S.dma("sp", ubv_of(blk), uT_scr[:, :, blk * 128:(blk + 1) * 128], R=[uT_scr], W=[uTb[blk % 2]])

        def front(i):
            blk, q = i // 16, i % 16
            ub = uTb[blk % 2]
            ubv = ubv_of(blk)
            if q == 1 and blk + 1 < nb5:
                load_ub(blk + 1)
            A, B = pA[i % 2], pB[i % 2]
            t1_, t2_, xs_ = t1[i % 2], t2[i % 2], xs[i % 2]
            for gi in range(4):
                g = 4 * q + gi
                S.op("pe", lambda h, g=g, gi=gi: h.matmul(
                    A[:, gi * 128:(gi + 1) * 128], Bpad[:, g * 128:(g + 1) * 128], ubv[:, g // 8, :], start=True, stop=True),
                    R=[Bpad, ub], W=[A])
            for gi in range(4):
                g = 4 * q + gi
                S.op("pe", lambda h, g=g, gi=gi: h.matmul(
                    B[:, gi * 128:(gi + 1) * 128], Bswp[:, g * 128:(g + 1) * 128], ubv[:, g // 8, :], start=True, stop=True),
                    R=[Bswp, ub], W=[B])
            S.op("dve", lambda h: h.tensor_tensor(out=t1_[:, :], in0=A[:, :], in1=COS[:, q * 512:(q + 1) * 512], op=ALU.mult),
                 R=[A, COS], W=[t1_])
            S.op("dve", lambda h: h.tensor_tensor(out=t2_[:, :], in0=B[:, :], in1=SIN[:, q * 512:(q + 1) * 512], op=ALU.mult),
                 R=[B, SIN], W=[t2_])
            S.op("pool", lambda h: h.tensor_tensor(out=xs_[:, :], in0=t1_[:, :], in1=t2_[:, :], op=ALU.add), R=[t1_, t2_], W=[xs_])

        def back(i):
            blk, q = i // 16, i % 16
            own = (blk % 4 == 3)
            m = blk // 4
            ub = uTb[blk % 2]
            ubv = ubv_of(blk)
            xs_, z_ = xs[i % 2], zt[i % 2]
            if not own:
                S.op("dve", lambda h: h.tensor_tensor(out=z_[:, :], in0=xs_[:, :], in1=Wt[:, q * 512:(q + 1) * 512], op=ALU.mult),
                     R=[xs_, Wt], W=[z_])
                S.op("dve", lambda h: h.tensor_reduce(out=zl[:, 4 * q:4 * q + 4], in_=z_[:, :].rearrange("p (g j) -> p g j", j=128),
                                                      axis=AX.X, op=ALU.add), R=[z_], W=[zl])
                if q != 15:
                    return
                S.op("dve", lambda h: h.tensor_tensor(out=c1t[:, :], in0=R128[:, :], in1=zc[:, :], op=ALU.mult), R=[R128, zc], W=[c1t])
                S.op("dve", lambda h: h.tensor_tensor(out=zl[:, :], in0=zl[:, :], in1=c1t[:, :], op=ALU.add), R=[zl, c1t], W=[zl])
            for gi in range(4 if own else 0):
                g = 4 * q + gi
                S.op("dve", lambda h, g=g, gi=gi: h.tensor_tensor_scan(
                    out=z_[:, gi * 128:(gi + 1) * 128], data0=RHO[:, g:g + 1].to_broadcast([128, 128]),
                    data1=xs_[:, gi * 128:(gi + 1) * 128], initial=zc[:, g:g + 1], op0=ALU.mult, op1=ALU.add),
                    R=[RHO, xs_, zc], W=[z_])
            if own:
                S.op("act", lambda h: h.copy(
                    out=zl[:, 4 * q:4 * q + 4].unsqueeze(2),
                    in_=z_[:, :].rearrange("p (g j) -> p g j", j=128)[:, :, 127:128]), R=[z_], W=[zl])
            if own:
                Zc_, Zs_ = Zc[q % 2], Zs[q % 2]
                S.op("pool", lambda h: h.tensor_tensor(out=Zc_[:, :], in0=z_[:, :], in1=COS[:, q * 512:(q + 1) * 512], op=ALU.mult),
                     R=[z_, COS], W=[Zc_])
                S.op("dve", lambda h: h.tensor_tensor(out=Zs_[:, :], in0=z_[:, :], in1=SIN[:, q * 512:(q + 1) * 512], op=ALU.mult),
                     R=[z_, SIN], W=[Zs_])
                for gi in range(4):
                    g = 4 * q + gi
                    t = g // 8
                    Y = pY[t // 4]
                    ysl = slice((t % 4) * 128, (t % 4 + 1) * 128)
                    S.op("pe", lambda h, g=g, gi=gi, Y=Y, ysl=ysl: h.matmul(
                        Y[:, ysl], CWc[:, g * 128:(g + 1) * 128], Zc_[:, gi * 128:(gi + 1) * 128], start=(g % 8 == 0), stop=False),
                        R=[CWc, Zc_], W=[Y])
                    S.op("pe", lambda h, g=g, gi=gi, Y=Y, ysl=ysl: h.matmul(
                        Y[:, ysl], CWs[:, g * 128:(g + 1) * 128], Zs_[:, gi * 128:(gi + 1) * 128], start=False, stop=(g % 8 == 7)),
                        R=[CWs, Zs_], W=[Y])
            if q != 15:
                return
            S.op("pe", lambda h: h.matmul(pS[:, 0:64], swp, zl[:, :], start=True, stop=True), R=[cst, zl], W=[pS])
            S.op("dve", lambda h: h.tensor_tensor(out=c1t[:, :], in0=zl[:, :], in1=CT[:, :], op=ALU.mult), R=[zl, CT], W=[c1t])
            S.op("dve", lambda h: h.tensor_tensor(out=c2t[:, :], in0=pS[:, 0:64], in1=STs[:, :], op=ALU.mult), R=[pS, STs], W=[c2t])
            S.op("dve", lambda h: h.tensor_tensor(out=zc[:, :], in0=c1t[:, :], in1=c2t[:, :], op=ALU.add), R=[c1t, c2t], W=[zc])
            if own:
                for t in range(8):
                    Y = pY[t // 4]
                    ysl = slice((t % 4) * 128, (t % 4 + 1) * 128)
                    S.op("dve", lambda h, t=t, Y=Y, ysl=ysl: h.scalar_tensor_tensor(
                        out=yf[:, t * 128:(t + 1) * 128], in0=ubv[:, t, :], scalar=colv[:, 48 + t:49 + t], in1=Y[:, ysl],
                        op0=ALU.mult, op1=ALU.add), R=[ub, colv, Y], W=[yf])
                S.op("pool", lambda h: h.tensor_tensor(out=g1[:, :], in0=yf[:, :], in1=yf[:, :], op=ALU.mult), R=[yf], W=[g1])
                S.op("pool", lambda h: h.tensor_scalar(out=g1[:, :], in0=g1[:, :], scalar1=0.07135481627, scalar2=1.5957691216,
                                                       op0=ALU.mult, op1=ALU.add), R=[g1], W=[g1])
                S.op("pool", lambda h: h.tensor_tensor(out=g1[:, :], in0=g1[:, :], in1=yf[:, :], op=ALU.mult), R=[g1, yf], W=[g1])
                S.op("act", lambda h: h.activation(out=g1[:, :], in_=g1[:, :], func=AF.Sigmoid), R=[g1], W=[g1])
                ya_ = yab[0]
                S.op("pool", lambda h: h.tensor_tensor(out=ya_[:, :], in0=yf[:, :], in1=g1[:, :], op=ALU.mult), R=[yf, g1], W=[ya_])
                S.dma("sp", yaT_scr[:, :, m * 128:(m + 1) * 128], ya_[:, :].rearrange("p (t n) -> p t n", t=8), R=[ya_], W=[yaT_scr])

        load_ub(0)
        for i in range(NQ + 1):
            if i < NQ:
                front(i)
            if i >= 1:
                back(i - 1)
        S.barrier()
    print("n_inst after S5", S.n_inst)

    if stop == 2:
        if dbg:
            with ExitStack() as dd:
                d1 = sb(dd, "d1", [128, 2048], BF16)
                d2 = sb(dd, "d2", [128, 2048], F32)
                S.dma("sp", d1[:, :].rearrange("p (t n) -> p t n", t=8), yaT_scr[:, :, 0:256], R=[yaT_scr], W=[d1])
                S.op("dve", lambda h: h.tensor_copy(out=d2[:, :], in_=d1[:, :]), R=[d1], W=[d2])
                S.dma("sp", dbg_d[:, 0:2048], d2[:, :], R=[d2], W=[dbg_d])
                S.finish([dbg_d])
        es.close()
        return nc


    yatT_scr = dscr("yatT_scr", [128, 8, 2048], BF16)
    h2T_scr = dscr("h2T_scr", [128, NCH, KT, 128], BF16)
    nm2 = NCH if FULL else 2

    with ExitStack() as p2:
        KTs = sb(p2, "KTs", [128, 2 * L], BF16)
        Vs = sb(p2, "Vs", [128, NBLK * 256], BF16)
        kiTs = sb(p2, "kiTs", [128, L], BF16)
        isc = sb(p2, "isc", [128, L], F32)
        MBs = [sb(p2, "MB%d" % i, [128, L], BF16) for i in range(2)]
        junkb = sb(p2, "junkb", [128, L], BF16)
        I4 = sb(p2, "I4", [128, 512], BF16)
        kb = sb(p2, "kb", [128, 512], F32)
        dgv = sb(p2, "dgv", [128, 128], F32)
        QTm = [sb(p2, "QTm%d" % i, [128, 1024], BF16) for i in range(2)]
        qiTm = [sb(p2, "qiTm%d" % i, [128, 1024], BF16) for i in range(2)]
        Tr = [sb(p2, "Tr%d" % i, [128, 512], F32) for i in range(2)]
        PT = [sb(p2, "PT%d" % i, [128, 512], BF16) for i in range(2)]
        Hk = sb(p2, "Hk", [128, 28], F32)
        rmax = sb(p2, "rmax", [128, 1], F32)
        rmin = sb(p2, "rmin", [128, 1], F32)
        w0 = sb(p2, "w0", [128, 1], F32)
        mid = sb(p2, "mid", [128, 1], F32)
        Sacc = sb(p2, "Sacc", [128, 1], F32)
        cntD = sb(p2, "cntD", [128, 1], F32)
        junkd = sb(p2, "junkd", [128, L // 2], BF16)
        aa = sb(p2, "aa", [128, 1], F32)
        lo = sb(p2, "lo", [128, 1], F32)
        rD = sb(p2, "rD", [128, 512], F32)
        yat = [sb(p2, "yat%d" % i, [128, 1024], BF16) for i in range(2)]
        pl_i = [ps(p2, "pl_i%d" % i, [128, 512], F32) for i in range(2)]
        pl_a = [ps(p2, "pl_a%d" % i, [128, 512], F32) for i in range(2)]
        pl = pl_i
        pO = [ps(p2, "pO%d" % i, [128, 512], F32) for i in range(2)]
        pD = [ps(p2, "pD%d" % i, [128, 512], F32) for i in range(2)]
        for hh in range(4):
            S.dma("sp", KTs[:, :].rearrange("p (g n) -> p g n", g=2)[:, :, hh * 2048:(hh + 1) * 2048],
                  KT_scr[:, :, hh * 2048:(hh + 1) * 2048], R=[KT_scr], W=[KTs])
            S.dma("sp", Vs[:, hh * 4096:(hh + 1) * 4096].rearrange("p (b n) -> p b n", n=256),
                  V_scr[:, hh * 16:(hh + 1) * 16, :], R=[V_scr], W=[Vs])
        S.dma("sp", kiTs[:, :], kiT_scr[:, :], R=[kiT_scr], W=[kiTs])
        for r4 in range(4):
            S.op("dve", lambda h, r4=r4: h.tensor_copy(out=I4[:, r4 * 128:(r4 + 1) * 128], in_=identb[:, :]), R=[identb], W=[I4])
        for b4 in range(4):
            S.op("dve", lambda h, b4=b4: h.tensor_scalar(out=dgv[:, :], in0=ident, scalar1=validc[:, b4:b4 + 1], scalar2=None, op0=ALU.mult),
                 R=[cst, validc], W=[dgv])
            S.op("pe", lambda h: h.matmul(pl[0][:, 0:128], ones_f, dgv[:, :], start=True, stop=True), R=[cst, dgv], W=[pl[0]])
            S.op("dve", lambda h, b4=b4: h.tensor_scalar(out=kb[:, b4 * 128:(b4 + 1) * 128], in0=pl[0][:, 0:128], scalar1=-1.0, scalar2=-NEG,
                                                         op0=ALU.add, op1=ALU.mult), R=[pl[0]], W=[kb])
        SCALE = 128.0 ** -0.5
        cnt_i = [0]
        cnt_a = [0]

        def load_q(m):
            qt = QTm[m % 2]
            qit = qiTm[m % 2]
            S.dma("sp", qt[:, :].rearrange("p (a b) -> p a b", b=128), QT_scr[:, m, :, :], R=[QT_scr], W=[qt])
            S.dma("sp", qit[:, :].rearrange("p (a b) -> p a b", b=128), qiT_scr[:, m, :, :], R=[qiT_scr], W=[qit])

        def idx_unit(m, c, hh):
            qit = qiTm[m % 2]
            pr, par = hh // 2, hh % 2
            p_ = pl_i[cnt_i[0] % 2]
            T_ = Tr[cnt_i[0] % 2]
            cnt_i[0] += 1
            S.op("pe", lambda h: h.matmul(
                p_[:, :], qit[par * 64:(par + 1) * 64, pr * 128:(pr + 1) * 128],
                kiTs[par * 64:(par + 1) * 64, c * 512:(c + 1) * 512], start=True, stop=True),
                R=[qit, kiTs], W=[p_])
            S.op("act", lambda h: h.activation(out=T_[:, :], in_=p_[:, :], func=AF.Relu), R=[p_], W=[T_])
            wcol = wi_all[:, 16 * m + hh:16 * m + hh + 1]
            if hh == 0:
                S.op("dve", lambda h: h.tensor_scalar(
                    out=isc[:, c * 512:(c + 1) * 512], in0=T_[:, :], scalar1=wcol, scalar2=None, op0=ALU.mult),
                    R=[T_, wi_all], W=[isc])
            else:
                S.op("dve", lambda h: h.scalar_tensor_tensor(
                    out=isc[:, c * 512:(c + 1) * 512], in0=T_[:, :], scalar=wcol, in1=isc[:, c * 512:(c + 1) * 512],
                    op0=ALU.mult, op1=ALU.add), R=[T_, wi_all, isc], W=[isc])

        def thresh(m):
            n = 512 * (m + 1)
            MB_ = MBs[m % 2]
            S.op("dve", lambda h: h.tensor_reduce(out=rmax[:, :], in_=isc[:, 0:n], axis=AX.X, op=ALU.max), R=[isc], W=[rmax])
            S.op("dve", lambda h: h.tensor_reduce(out=rmin[:, :], in_=isc[:, 0:n], axis=AX.X, op=ALU.min), R=[isc], W=[rmin])
            S.op("dve", lambda h: h.tensor_tensor(out=isc[:, 0:512], in0=isc[:, 0:512], in1=kb[:, :], op=ALU.add), R=[isc, kb], W=[isc])
            S.op("dve", lambda h: h.tensor_tensor(out=isc[:, n - 512:n], in0=isc[:, n - 512:n], in1=cst[:, C_CM:C_CM + 512], op=ALU.add),
                 R=[isc, cst], W=[isc])
            S.op("dve", lambda h: h.tensor_tensor(out=w0[:, :], in0=rmax[:, :], in1=rmin[:, :], op=ALU.subtract), R=[rmax, rmin], W=[w0])
            S.op("dve", lambda h: h.tensor_scalar(out=w0[:, :], in0=w0[:, :], scalar1=1.0001, scalar2=1e-6, op0=ALU.mult, op1=ALU.add),
                 R=[w0], W=[w0])
            S.op("dve", lambda h: h.tensor_scalar(out=Hk[:, :], in0=cst[:, C_H2:C_H2 + 28], scalar1=w0[:, 0:1], scalar2=None, op0=ALU.mult),
                 R=[cst, w0], W=[Hk])
            S.op("dve", lambda h: h.tensor_tensor(out=mid[:, :], in0=rmin[:, :], in1=Hk[:, 0:1], op=ALU.add), R=[rmin, Hk], W=[mid])
            nA = n // 2
            thrA = float(nA - 2 * TOPK)
            for k in range(NBIS):
                S.op("act", lambda h: h.activation(out=junkb[:, 0:nA], in_=isc[:, 0:nA], func=AF.Sign, bias=mid[:, 0:1], scale=-1.0,
                                                   accum_out=Sacc[:, 0:1]), R=[isc, mid], W=[junkb, Sacc])
                S.op("dve", lambda h: h.tensor_scalar(out=junkd[:, 0:n - nA], in0=isc[:, nA:n], scalar1=mid[:, 0:1], scalar2=0.0,
                                                      op0=ALU.is_ge, op1=ALU.add, accum_out=cntD[:, 0:1]), R=[isc, mid], W=[junkd, cntD])
                S.op("dve", lambda h: h.scalar_tensor_tensor(out=aa[:, :], in0=cntD[:, :], scalar=-2.0, in1=Sacc[:, :],
                                                             op0=ALU.mult, op1=ALU.add), R=[cntD, Sacc], W=[aa])
                S.op("dve", lambda h, k=k: h.tensor_scalar(out=aa[:, :], in0=aa[:, :], scalar1=thrA, scalar2=Hk[:, k:k + 1],
                                                           op0=ALU.is_le, op1=ALU.mult), R=[aa, Hk], W=[aa])
                S.op("dve", lambda h, k=k: h.scalar_tensor_tensor(out=mid[:, :], in0=aa[:, :], scalar=Hk[:, k + 1:k + 2], in1=mid[:, :],
                                                                  op0=ALU.subtract, op1=ALU.add), R=[aa, Hk, mid], W=[mid])
            S.op("dve", lambda h: h.tensor_tensor(out=lo[:, :], in0=mid[:, :], in1=Hk[:, NBIS:NBIS + 1], op=ALU.subtract),
                 R=[mid, Hk], W=[lo])
            S.op("dve", lambda h: h.tensor_scalar(out=MB_[:, 0:n], in0=isc[:, 0:n], scalar1=lo[:, 0:1], scalar2=-30000.0,
                                                  op0=ALU.is_lt, op1=ALU.mult), R=[isc, lo], W=[MB_])

        def att_front(m, kbk, g):
            qt = QTm[m % 2]
            MB_ = MBs[m % 2]
            p_ = pl_a[cnt_a[0] % 2]
            P_ = PT[cnt_a[0] % 2]
            cnt_a[0] += 1
            S.op("pe", lambda h: h.matmul(
                p_[:, :], KTs[:, g * L + kbk * 128:g * L + (kbk + 1) * 128], qt[:, g * 512:(g + 1) * 512],
                start=True, stop=False), R=[KTs, qt], W=[p_])
            S.op("pe", lambda h: h.matmul(
                p_[:, :], MB_[:, kbk * 128:(kbk + 1) * 128], I4[:, :], start=False, stop=True), R=[MB_, I4], W=[p_])
            S.op("act", lambda h: h.activation(out=P_[:, :], in_=p_[:, :], func=AF.Exp, scale=SCALE), R=[p_], W=[P_])
            return P_

        def att_back(m, kbk, g, P_, nkb):
            S.op("pe", lambda h: h.matmul(
                pO[g][:, :], Vs[:, kbk * 256 + g * 128:kbk * 256 + (g + 1) * 128], P_[:, :],
                start=(kbk == 0), stop=(kbk == nkb - 1)), R=[Vs, P_], W=[pO[g]])
            S.op("pe", lambda h: h.matmul(
                pD[g][:, :], onesb[:, :], P_[:, :], start=(kbk == 0), stop=(kbk == nkb - 1)), R=[onesb, P_], W=[pD[g]])

        def att_final(m):
            ya_ = yat[m % 2]
            for g in range(2):
                S.op("dve", lambda h, g=g: h.reciprocal(out=rD[:, :], in_=pD[g][:, :]), R=[pD[g]], W=[rD])
                S.op("dve", lambda h, g=g: h.tensor_tensor(out=ya_[:, g * 512:(g + 1) * 512], in0=pO[g][:, :], in1=rD[:, :], op=ALU.mult),
                     R=[pO[g], rD], W=[ya_])
            S.dma("sp", yatT_scr[:, :, m * 128:(m + 1) * 128], ya_[:, :].rearrange("p (a b) -> p a b", b=128), R=[ya_], W=[yatT_scr])

        load_q(0)
        for hh in range(16):
            idx_unit(0, 0, hh)
        thresh(0)
        for m in range(nm2):
            nkb = 4 * (m + 1)
            U = [(kbk, g) for kbk in range(nkb) for g in range(2)]
            I_ = []
            if m + 1 < nm2:
                load_q(m + 1)
                I_ = [(c, hh) for c in range(m + 2) for hh in range(16)]
            ii = 0
            prev = None
            for u in range(len(U) + 1):
                cur = None
                if u < len(U):
                    cur = (U[u], att_front(m, U[u][0], U[u][1]))
                if prev is not None:
                    (kbk_, g_), P_ = prev
                    att_back(m, kbk_, g_, P_, nkb)
                prev = cur
                target = (u + 1) * len(I_) // (len(U) + 1)
                while ii < target:
                    idx_unit(m + 1, I_[ii][0], I_[ii][1])
                    ii += 1
            while ii < len(I_):
                idx_unit(m + 1, I_[ii][0], I_[ii][1])
                ii += 1
            att_final(m)
            if m + 1 < nm2:
                thresh(m + 1)
        S.barrier()
    print("n_inst after P2", S.n_inst)

    if stop == 3:
        if dbg:
            with ExitStack() as dd:
                d1 = sb(dd, "d1", [128, 2048], BF16)
                d2 = sb(dd, "d2", [128, 2048], F32)
                S.dma("sp", d1[:, :].rearrange("p (t n) -> p t n", t=8), yatT_scr[:, :, 0:256], R=[yatT_scr], W=[d1])
                S.op("dve", lambda h: h.tensor_copy(out=d2[:, :], in_=d1[:, :]), R=[d1], W=[d2])
                S.dma("sp", dbg_d[:, 0:2048], d2[:, :], R=[d2], W=[dbg_d])
                S.finish([dbg_d])
        es.close()
        return nc

    with ExitStack() as p3:
        wg = sb(p3, "wg", [128, 8 * 1024], BF16)
        wo = sb(p3, "wo", [128, KT * D], BF16)
        w_glu_v = w_glu.t.rearrange("(kt p) n -> p kt n", p=128)
        w_out_v = w_out.t.rearrange("(kt p) n -> p kt n", p=128)
        S.dma("pool", wg[:, :].rearrange("p (kt n) -> p kt n", kt=8), w_glu_v, W=[wg])
        wov = wo[:, :].rearrange("p (kt n) -> p kt n", kt=KT)
        for q4 in range(4):
            S.dma("pool", wov[:, 4 * q4:4 * q4 + 4, :], w_out_v[:, 4 * q4:4 * q4 + 4, :], W=[wo])
        wgv = wg[:, :].rearrange("p (kt n) -> p kt n", kt=8)
        pG = [ps(p3, "pG%d" % i, [128, 512], F32) for i in range(2)]
        pR = ps(p3, "pR", [128, 512], F32)
        pW = [ps(p3, "pW%d" % i, [128, 512], F32) for i in range(4)]
        ptr3 = ps(p3, "ptr3", [128, 1024], BF16)
        gtA_bc = bcast_row(p3, modT, modT[:, 32:48], "gtA_bc", pG)
        gscM_bc = bcast_row(p3, gscM, gscM[:, :], "gscM_bc", pG)
        shM_bc = bcast_row(p3, modT, modT[:, 48:64], "shM_bc", pG)
        yam = [sb(p3, "yam%d" % i, [128, 1024], BF16) for i in range(3)]
        yatm = [sb(p3, "yatm%d" % i, [128, 1024], BF16) for i in range(3)]
        sg = sb(p3, "sg", [128, 1024], F32)
        ysf = sb(p3, "ysf", [128, 1024], F32)
        sqf = sb(p3, "sqf", [128, 1024], F32)
        rs_s = sb(p3, "rs_s", [128, 128], F32)
        rs_a = sb(p3, "rs_a", [128, 128], F32)
        mixT = [sb(p3, "mixT%d" % i, [128, KT * 128], BF16) for i in range(3)]
        xo = [sb(p3, "xo%d" % i, [128, D], F32) for i in range(2)]
        x1t = [sb(p3, "x1t%d" % i, [128, D], F32) for i in range(2)]
        tmp3 = sb(p3, "tmp3", [128, D], F32)
        h2b = sb(p3, "h2b", [128, D], BF16)
        junk3 = h2b
        h2Ts = [sb(p3, "h2Ts%d" % i, [128, KT * 128], BF16) for i in range(1)] * 2
        ss1 = sb(p3, "ss1", [128, NCH], F32)
        ms1 = sb(p3, "ms1", [128, NCH], F32)
        rs1 = sb(p3, "rs1", [128, NCH], F32)
        def p3A(m):
            ya_ = yam[m % 3]
            yt_ = yatm[m % 3]
            mx = mixT[m % 3]
            S.dma("sp", ya_[:, :].rearrange("p (t n) -> p t n", t=8), yaT_scr[:, :, m * 128:(m + 1) * 128], R=[yaT_scr], W=[ya_])
            S.dma("sp", yt_[:, :].rearrange("p (t n) -> p t n", t=8), yatT_scr[:, :, m * 128:(m + 1) * 128], R=[yatT_scr], W=[yt_])
            for t in range(8):
                G = pG[t // 4]
                gsl = slice((t % 4) * 128, (t % 4 + 1) * 128)
                for k in range(8):
                    S.op("pe", lambda h, t=t, k=k, G=G, gsl=gsl: h.matmul(
                        G[:, gsl], wgv[:, k, t * 128:(t + 1) * 128], ya_[:, k * 128:(k + 1) * 128],
                        start=(k == 0), stop=(k == 7)), R=[wg, ya_], W=[G])
                S.op("act", lambda h, t=t, G=G, gsl=gsl: h.activation(
                    out=sg[:, t * 128:(t + 1) * 128], in_=G[:, gsl], func=AF.Sigmoid, bias=colv[:, 56 + t:57 + t], scale=1.0),
                    R=[G, colv], W=[sg])
            S.op("dve", lambda h: h.tensor_tensor(out=ysf[:, :], in0=ya_[:, :], in1=sg[:, :], op=ALU.mult), R=[ya_, sg], W=[ysf])
            S.op("dve", lambda h: h.tensor_tensor(out=sqf[:, :], in0=ysf[:, :], in1=ysf[:, :], op=ALU.mult), R=[ysf], W=[sqf])
            for t in range(8):
                S.op("pe", lambda h, t=t: h.matmul(pR[:, 0:128], ones_f, sqf[:, t * 128:(t + 1) * 128], start=(t == 0), stop=(t == 7)),
                     R=[cst, sqf], W=[pR])
            S.op("dve", lambda h: h.tensor_scalar(out=rs_s[:, :], in0=pR[:, 0:128], scalar1=1.0 / 1024, scalar2=EPS, op0=ALU.mult, op1=ALU.add),
                 R=[pR], W=[rs_s])
            S.op("pool", lambda h: h.tensor_tensor(out=rs_s[:, :], in0=rs_s[:, :], in1=cst[:, C_NHALF:C_NHALF + 1].to_broadcast([128, 128]),
                                                   op=ALU.pow), R=[rs_s, cst], W=[rs_s])
            for t in range(8):
                S.op("dve", lambda h, t=t: h.scalar_tensor_tensor(
                    out=mx[:, t * 128:(t + 1) * 128], in0=ysf[:, t * 128:(t + 1) * 128], scalar=colv[:, 64 + t:65 + t], in1=rs_s[:, :],
                    op0=ALU.mult, op1=ALU.mult), R=[ysf, colv, rs_s], W=[mx])
            S.op("dve", lambda h: h.tensor_tensor(out=sqf[:, :], in0=yt_[:, :], in1=yt_[:, :], op=ALU.mult), R=[yt_], W=[sqf])
            for t in range(8):
                S.op("pe", lambda h, t=t: h.matmul(pR[:, 128:256], ones_f, sqf[:, t * 128:(t + 1) * 128], start=(t == 0), stop=(t == 7)),
                     R=[cst, sqf], W=[pR])
            S.op("dve", lambda h: h.tensor_scalar(out=rs_a[:, :], in0=pR[:, 128:256], scalar1=1.0 / 1024, scalar2=EPS, op0=ALU.mult, op1=ALU.add),
                 R=[pR], W=[rs_a])
            S.op("pool", lambda h: h.tensor_tensor(out=rs_a[:, :], in0=rs_a[:, :], in1=cst[:, C_NHALF:C_NHALF + 1].to_broadcast([128, 128]),
                                                   op=ALU.pow), R=[rs_a, cst], W=[rs_a])
            for t in range(8):
                S.op("dve", lambda h, t=t: h.scalar_tensor_tensor(
                    out=mx[:, (8 + t) * 128:(9 + t) * 128], in0=yt_[:, t * 128:(t + 1) * 128], scalar=colv[:, 72 + t:73 + t], in1=rs_a[:, :],
                    op0=ALU.mult, op1=ALU.mult), R=[yt_, colv, rs_a], W=[mx])

        def load_xo(m):
            S.dma("sp", xo[m % 2][:, :], xv[(4 * m + 3) * 128:(4 * m + 4) * 128, :], W=[xo[m % 2]])

        def p3B(m):
            mx = mixT[m % 3]
            x_ = xo[m % 2]
            if m + 1 < nm2:
                load_xo(m + 1)
            for dch in range(4):
                for kt in range(KT):
                    S.op("pe", lambda h, dch=dch, kt=kt: h.matmul(
                        pW[dch][:, :], mx[:, kt * 128:(kt + 1) * 128], wov[:, kt, dch * 512:(dch + 1) * 512],
                        start=(kt == 0), stop=(kt == KT - 1)), R=[mx, wo], W=[pW[dch]])
            x1_ = x1t[m % 2]
            for dch in range(4):
                dsl = slice(dch * 512, (dch + 1) * 512)
                S.op("dve", lambda h, dch=dch, dsl=dsl: h.tensor_tensor(out=tmp3[:, dsl], in0=pW[dch][:, :], in1=gtA_bc[:, dsl], op=ALU.mult),
                     R=[pW[dch], gtA_bc], W=[tmp3])
            S.op("dve", lambda h: h.tensor_tensor(out=x1_[:, :], in0=tmp3[:, :], in1=x_[:, :], op=ALU.add), R=[tmp3, x_], W=[x1_])
            S.dma("pool", x1_scr[:, m, :], x1_[:, :], R=[x1_], W=[x1_scr])
            S.op("act", lambda h: h.activation(out=junk3[:, :], in_=x1_[:, :], func=AF.Square, accum_out=ss1[:, m:m + 1]),
                 R=[x1_], W=[junk3, ss1])
            S.op("dve", lambda h: h.tensor_scalar(out=ms1[:, m:m + 1], in0=ss1[:, m:m + 1], scalar1=1.0 / D, scalar2=EPS,
                                                  op0=ALU.mult, op1=ALU.add), R=[ss1], W=[ms1])
            S.op("pool", lambda h: h.tensor_tensor(out=rs1[:, m:m + 1], in0=ms1[:, m:m + 1], in1=cst[:, C_NHALF:C_NHALF + 1], op=ALU.pow),
                 R=[ms1, cst], W=[rs1])
            S.op("dve", lambda h: h.scalar_tensor_tensor(out=tmp3[:, :], in0=x1_[:, :], scalar=rs1[:, m:m + 1], in1=gscM_bc[:, :],
                                                         op0=ALU.mult, op1=ALU.mult), R=[x1_, rs1, gscM_bc], W=[tmp3])
            S.op("dve", lambda h: h.tensor_tensor(out=h2b[:, :], in0=tmp3[:, :], in1=shM_bc[:, :], op=ALU.add), R=[tmp3, shM_bc], W=[h2b])
            hs = h2Ts[m % 2]
            for half in range(2):
                for k8 in range(8):
                    kt = half * 8 + k8
                    S.op("pe", lambda h, kt=kt, k8=k8: h.transpose(ptr3[:, k8 * 128:(k8 + 1) * 128], h2b[:, kt * 128:(kt + 1) * 128], identb[:, :]),
                         R=[h2b, identb], W=[ptr3])
                S.op("act", lambda h, half=half: h.copy(out=hs[:, half * 1024:(half + 1) * 1024], in_=ptr3[:, :]), R=[ptr3], W=[hs])
            S.dma("pool", h2T_scr[:, m, :, :], hs[:, :].rearrange("p (kt n) -> p kt n", kt=KT), R=[hs], W=[h2T_scr])

        load_xo(0)
        p3A(0)
        if nm2 > 1:
            p3A(1)
        for m in range(nm2):
            if m + 2 < nm2:
                p3A(m + 2)
            p3B(m)
        S.barrier()
    print("n_inst after P3", S.n_inst)

    with ExitStack() as p4:
        pH = [ps(p4, "pH%d" % i, [128, 512], F32) for i in range(3)]
        pOm = [ps(p4, "pOm%d" % i, [128, 512], F32) for i in range(4)]
        gtM_bc = bcast_row(p4, modT, modT[:, 80:96], "gtM_bc", pH)
        h2p = sb(p4, "h2p", [128, 8 * KT * 128], BF16)
        acc = sb(p4, "acc", [128, 8 * D], F32)
        wmi_s = [sb(p4, "wmi_s%d" % i, [128, KT * 512], BF16) for i in range(2)]
        wmo_s = [sb(p4, "wmo_s%d" % i, [128, 4 * D], BF16) for i in range(2)]
        hid = sb(p4, "hid", [128, 4 * 1024], BF16)
        rl = [sb(p4, "rl%d" % i, [128, 512], BF16) for i in range(2)]
        x1m = [sb(p4, "x1m%d" % i, [128, D], F32) for i in range(2)]
        accb = [Buf() for _ in range(8)]
        w_mi_v = w_mi.t.rearrange("(kt p) n -> p kt n", p=128)
        w_mo_v = w_mo.t.rearrange("(f p) n -> p f n", p=128)
        h2v = h2p[:, :].rearrange("p (b kt n) -> p b kt n", b=8, kt=KT)
        npass = 2 if FULL else 1
        nfg = 16
        ih = 0
        io = 0

        def load_w(g_):
            fg_ = g_ % nfg
            wi2 = wmi_s[g_ % 2]
            wo2 = wmo_s[g_ % 2]
            wiv2 = wi2[:, :].rearrange("p (kt n) -> p kt n", kt=KT)
            wov2 = wo2[:, :].rearrange("p (f n) -> p f n", f=4)
            for q2 in range(2):
                S.dma("pool", wiv2[:, 8 * q2:8 * q2 + 8, :], w_mi_v[:, 8 * q2:8 * q2 + 8, fg_ * 512:(fg_ + 1) * 512], W=[wi2])
            S.dma("pool", wov2, w_mo_v[:, 4 * fg_:4 * fg_ + 4, :], W=[wo2])

        for pp in range(npass):
            nbp = 8 if FULL else 2
            S.dma("sp", h2v[:, 0:nbp, :, :], h2T_scr[:, 8 * pp:8 * pp + nbp, :, :], R=[h2T_scr], W=[h2p])
            for fg in range(nfg):
                wi_ = wmi_s[fg % 2]
                wo_ = wmo_s[fg % 2]
                wiv = wi_[:, :].rearrange("p (kt n) -> p kt n", kt=KT)
                wo_v = wo_[:, :].rearrange("p (f n) -> p f n", f=4)
                if pp == 0 and fg == 0:
                    load_w(0)
                if pp * nfg + fg + 1 < npass * nfg:
                    load_w(pp * nfg + fg + 1)
                for f in range(4):
                    for c in range(nbp // 4 if nbp >= 4 else 1):
                        p_ = pH[ih % 3]
                        r_ = rl[ih % 2]
                        ih += 1
                        nb_ = min(4, nbp)
                        for kt in range(KT):
                            S.op("pe", lambda h, p_=p_, kt=kt, f=f, c=c, nb_=nb_: h.matmul(
                                p_[:, 0:nb_ * 128], wiv[:, kt, f * 128:(f + 1) * 128], h2v[:, 4 * c:4 * c + nb_, kt, :],
                                start=(kt == 0), stop=(kt == KT - 1)), R=[wi_, h2p], W=[p_])
                        S.op("act", lambda h, p_=p_, r_=r_, nb_=nb_: h.activation(out=r_[:, 0:nb_ * 128], in_=p_[:, 0:nb_ * 128], func=AF.Relu),
                             R=[p_], W=[r_])
                        S.op("pool", lambda h, r_=r_, f=f, c=c, nb_=nb_: h.tensor_tensor(
                            out=hid[:, f * 1024 + c * 512:f * 1024 + c * 512 + nb_ * 128], in0=r_[:, 0:nb_ * 128], in1=r_[:, 0:nb_ * 128],
                            op=ALU.mult), R=[r_], W=[hid])
                for blk in range(nbp):
                    for dch in range(4):
                        p_ = pOm[io % 4]
                        io += 1
                        for f in range(4):
                            S.op("pe", lambda h, p_=p_, f=f, blk=blk, dch=dch: h.matmul(
                                p_[:, :], hid[:, f * 1024 + blk * 128:f * 1024 + (blk + 1) * 128], wo_v[:, f, dch * 512:(dch + 1) * 512],
                                start=(f == 0), stop=(f == 3)), R=[hid, wo_], W=[p_])
                        asl = slice(blk * D + dch * 512, blk * D + (dch + 1) * 512)
                        if fg == 0:
                            S.op("dve", lambda h, p_=p_, asl=asl: h.tensor_copy(out=acc[:, asl], in_=p_[:, :]), R=[p_], W=[acc])
                        else:
                            S.op("dve", lambda h, p_=p_, asl=asl: h.tensor_tensor(out=acc[:, asl], in0=p_[:, :], in1=acc[:, asl], op=ALU.add),
                                 R=[p_, acc], W=[acc])
            for blk in range(nbp):
                mb = 8 * pp + blk
                xm = x1m[blk % 2]
                asl = slice(blk * D, (blk + 1) * D)
                S.dma("sp", xm[:, :], x1_scr[:, mb, :], R=[x1_scr], W=[xm])
                S.op("dve", lambda h, asl=asl: h.tensor_tensor(out=acc[:, asl], in0=acc[:, asl], in1=gtM_bc[:, :], op=ALU.mult),
                     R=[acc, gtM_bc], W=[accb[blk]])
                S.op("dve", lambda h, asl=asl, xm=xm: h.tensor_tensor(out=acc[:, asl], in0=acc[:, asl], in1=xm[:, :], op=ALU.add),
                     R=[accb[blk], xm], W=[accb[blk]])
                S.dma("pool", out_d[mb * 128:(mb + 1) * 128, :], acc[:, asl], R=[accb[blk], acc], W=[out_d])
        S.barrier()
    print("n_inst total", S.n_inst)
    S.finish([out_d])
    es.close()
    return nc


def _prep_core_inputs(inputs, core):
    b, j = core // 4, core % 4
    pad = 128 * (3 - j)
    x = np.asarray(inputs["x"], np.float32)
    xv = np.zeros((L, D), np.float32)
    xv[pad:] = x[b, :L - pad]
    valid = np.zeros((L,), np.float32)
    valid[pad:] = 1.0
    pos = np.zeros((L,), np.int32)
    pos[pad:] = np.asarray(inputs["positions"])[b, :L - pad]
    vecs = np.concatenate([
        np.asarray(inputs["c"], np.float32)[b].reshape(16, 128),
        np.asarray(inputs["g_norm_mix"], np.float32).reshape(16, 128),
        np.asarray(inputs["g_norm_mlp"], np.float32).reshape(16, 128),
        np.asarray(inputs["d_skip"], np.float32).reshape(8, 128),
        np.asarray(inputs["b_glu"], np.float32).reshape(8, 128),
        np.asarray(inputs["g_out_ssm"], np.float32).reshape(8, 128),
        np.asarray(inputs["g_out_attn"], np.float32).reshape(8, 128),
        np.asarray(inputs["g_q"], np.float32).reshape(1, 128),
        np.asarray(inputs["g_k"], np.float32).reshape(1, 128),
    ], axis=0)
    f = lambda k, shp: np.ascontiguousarray(np.asarray(inputs[k], np.float32).reshape(shp))
    return {
        "xv": xv,
        "validT": np.ascontiguousarray(valid.reshape(NBLK, 128).T),
        "posT": np.ascontiguousarray(pos.reshape(NBLK, 128).T),
        "vecs": np.ascontiguousarray(vecs),
        "w_ada": f("w_ada", (D, 6 * D)),
        "b_ada": f("b_ada", (96, 128)),
        "w_in": f("w_in", (D, DIN)),
        "lam_re": f("lam_re", (64, 64)),
        "lam_im": f("lam_im", (64, 64)),
        "log_dt": f("log_dt", (1, 64)),
        "b_re": f("b_re", (64, 64, 16)),
        "b_im": f("b_im", (64, 64, 16)),
        "c_re": f("c_re", (1024, 64)),
        "c_im": f("c_im", (1024, 64)),
        "w_glu": f("w_glu", (1024, 1024)),
        "w_out": f("w_out", (D, D)),
        "w_mlp_in": f("w_mlp_in", (D, 4 * D)),
        "w_mlp_out": f("w_mlp_out", (4 * D, D)),
    }


def kernel(**inputs):
    nc = build()
    consts = make_consts()
    shared = None
    in_maps = []
    for core in range(8):
        m = _prep_core_inputs(inputs, core)
        if shared is None:
            shared = {k: v for k, v in m.items() if k not in ("xv", "validT", "posT", "vecs")}
        else:
            for k in shared:
                m[k] = shared[k]
        m["consts"] = consts
        in_maps.append(m)
    res = run_bass_kernel_spmd(nc, in_maps, core_ids=list(range(8)))
    out = np.zeros((2, L, D), np.float32)
    for core in range(8):
        b, j = core // 4, core % 4
        o = np.asarray(res.results[core]["out"]).reshape(NCH, 128, D)
        for m_ in range(NCH):
            g0 = 128 * (4 * m_ + j)
            out[b, g0:g0 + 128] = o[m_]
    return out
```
